# Optimizing a Trainium2 kernel written in Bass

```python
import jax, jax.numpy as jnp
from jax import lax
import numpy as np

D_MODEL = 1024
BATCH = 8
SEQ = 2048
DEPTH = 1

ATTN_HEADS = 8
ATTN_HEAD_DIM = 64
ATTN_WIDTH = ATTN_HEADS * ATTN_HEAD_DIM
KV_LATENT = 128
IDX_HEADS = 8
IDX_HEAD_DIM = 64
TOPK_MAX = 256
Q_BLOCK = 128
POOL_WINDOWS = (2, 4, 8, 16)
POOL_GROUP = 128
POOL_WIDTH = len(POOL_WINDOWS) * POOL_GROUP
N_EXPERTS = 32
TOP_K_EXPERTS = 4
D_FF = 1024
SWIGLU_LIMIT = 7.0
SWIGLU_ALPHA = 1.702
MOE_BLOCK = 128
DEEPNORM_ALPHA = (2.0 * DEPTH) ** 0.25
DEEPNORM_BETA = (8.0 * DEPTH) ** -0.25
LN_EPS = 1e-5
N_MOD = 6
IN_SIZES = (ATTN_WIDTH, KV_LATENT, IDX_HEADS * IDX_HEAD_DIM, IDX_HEAD_DIM, IDX_HEADS, POOL_WIDTH, 2 * D_MODEL)
IN_COLS = sum(IN_SIZES)

kernel_name = 'hybrid_dsa_pool_moe_deepnorm_block'


def layer_norm(x, g, b):
    xf = x.astype(jnp.float32)
    mu = jnp.mean(xf, axis=-1, keepdims=True)
    var = jnp.mean(jnp.square(xf - mu), axis=-1, keepdims=True)
    y = (xf - mu) * lax.rsqrt(var + LN_EPS)
    return (y * g.astype(jnp.float32) + b.astype(jnp.float32)).astype(x.dtype)


def rms_norm(x, g):
    xf = x.astype(jnp.float32)
    y = xf * lax.rsqrt(jnp.mean(jnp.square(xf), axis=-1, keepdims=True) + LN_EPS)
    return (y * g.astype(jnp.float32)).astype(x.dtype)


def split_cols(a):
    offs, acc = [], 0
    for s in IN_SIZES[:-1]:
        acc += s
        offs.append(acc)
    return jnp.split(a, offs, axis=-1)


def dsa_attention(q_lat, c_kv, q_idx, k_idx, w_idx):
    B, S = c_kv.shape[0], c_kv.shape[1]
    topk = min(TOPK_MAX, S // 4)
    nb = S // Q_BLOCK
    slopes = 2.0 ** (-8.0 * jnp.arange(1, ATTN_HEADS + 1, dtype=jnp.float32) / ATTN_HEADS)
    key_pos = jnp.arange(S)

    def to_blocks(a):
        return jnp.moveaxis(a.reshape((B, nb, Q_BLOCK) + a.shape[2:]), 1, 0)

    def one_block(args):
        ql, qi, wi, start = args
        qpos = start + jnp.arange(Q_BLOCK)
        causal = key_pos[None, :] <= qpos[:, None]
        dots = jnp.einsum('bqhd,bsd->bqhs', qi, k_idx).astype(jnp.float32)
        score = jnp.einsum('bqh,bqhs->bqs', wi.astype(jnp.float32), jax.nn.relu(dots))
        score = jnp.where(causal[None], score, -jnp.inf)
        _, sel = lax.top_k(score, topk)
        c_sel = jax.vmap(lambda c, i: c[i])(c_kv, sel)
        logits = jnp.einsum('bqhr,bqkr->bqhk', ql, c_sel).astype(jnp.float32)
        dist = (qpos[None, :, None] - sel).astype(jnp.float32)
        logits = logits - slopes[None, None, :, None] * dist[:, :, None, :]
        valid = sel <= qpos[None, :, None]
        logits = jnp.where(valid[:, :, None, :], logits, -jnp.inf)
        p = jax.nn.softmax(logits, axis=-1).astype(c_sel.dtype)
        return jnp.einsum('bqhk,bqkr->bqhr', p, c_sel)

    starts = jnp.arange(nb) * Q_BLOCK
    out = lax.map(one_block, (to_blocks(q_lat), to_blocks(q_idx), to_blocks(w_idx), starts))
    return jnp.moveaxis(out, 0, 1).reshape(B, S, ATTN_HEADS, KV_LATENT)


def multiscale_pool(u):
    B, S, C = u.shape
    uf = u.astype(jnp.float32)
    cs = jnp.concatenate([jnp.zeros((B, 1, C), jnp.float32), jnp.cumsum(uf, axis=1)], axis=1)
    upper = jnp.arange(1, S + 1)
    outs = []
    for g, w in enumerate(POOL_WINDOWS):
        lo, hi = g * POOL_GROUP, (g + 1) * POOL_GROUP
        lower = jnp.maximum(upper - w, 0)
        csg = cs[..., lo:hi]
        count = (upper - lower).astype(jnp.float32)[None, :, None]
        outs.append((csg[:, upper] - csg[:, lower]) / count - uf[..., lo:hi])
    return jnp.concatenate(outs, axis=-1).astype(u.dtype)


def moe(h, w_router, b_router, w_gate_up, b_gate_up, w_down, b_down):
    B, S, D = h.shape
    T = B * S
    hf = h.reshape(T, D)
    logits = (hf @ w_router).astype(jnp.float32) + b_router.astype(jnp.float32)
    top_logit, top_e = lax.top_k(logits, TOP_K_EXPERTS)
    gate = jax.nn.softmax(top_logit, axis=-1)
    A = T * TOP_K_EXPERTS
    flat_e = top_e.reshape(A)
    order = jnp.argsort(flat_e)
    sorted_e = flat_e[order]
    tok = order // TOP_K_EXPERTS
    sizes = jnp.bincount(flat_e, length=N_EXPERTS)
    starts = jnp.cumsum(sizes) - sizes
    padded = (sizes + MOE_BLOCK - 1) // MOE_BLOCK * MOE_BLOCK
    pad_end = jnp.cumsum(padded)
    pad_start = pad_end - padded
    dest = pad_start[sorted_e] + jnp.arange(A) - starts[sorted_e]
    n_blocks = -(-A // MOE_BLOCK) + N_EXPERTS
    x_pad = jnp.zeros((n_blocks * MOE_BLOCK, D), h.dtype).at[dest].set(hf[tok])
    block_e = jnp.minimum(jnp.searchsorted(pad_end, jnp.arange(n_blocks) * MOE_BLOCK, side='right'), N_EXPERTS - 1)

    def expert_block(args):
        xb, e = args
        gu = xb @ w_gate_up[e] + b_gate_up[e]
        x_glu, x_lin = jnp.split(gu, 2, axis=-1)
        x_glu = jnp.minimum(x_glu, SWIGLU_LIMIT)
        x_lin = jnp.clip(x_lin, -SWIGLU_LIMIT, SWIGLU_LIMIT)
        act = x_glu * jax.nn.sigmoid(SWIGLU_ALPHA * x_glu) * (x_lin + 1.0)
        return act @ w_down[e] + b_down[e]

    y_pad = lax.map(expert_block, (x_pad.reshape(n_blocks, MOE_BLOCK, D), block_e)).reshape(-1, D)
    y = y_pad[dest] * gate.reshape(A)[order][:, None].astype(h.dtype)
    return jax.ops.segment_sum(y, tok, num_segments=T).reshape(B, S, D)


def setup_inputs(seed: int = 0) -> dict:
    key = jax.random.key(seed)
    ks = jax.random.split(key, 24)
    f32 = jnp.float32
    L, D, E, F, R = DEPTH, D_MODEL, N_EXPERTS, D_FF, KV_LATENT
    nrm = lambda k, shape, s: jax.random.normal(k, shape, f32) * s
    return {
        'x': nrm(ks[0], (BATCH, SEQ, D), 1.0),
        'c': nrm(ks[1], (BATCH, D), 1.0),
        'w_ada': nrm(ks[2], (L, D, N_MOD * D), 0.5 * D ** -0.5),
        'b_ada': nrm(ks[3], (L, N_MOD * D), 0.01),
        'w_in': nrm(ks[4], (L, D, IN_COLS), D ** -0.5),
        'kv_norm_g': 1.0 + nrm(ks[5], (L, R), 0.02),
        'w_uk': nrm(ks[6], (L, R, ATTN_HEADS, ATTN_HEAD_DIM), R ** -0.5),
        'w_uv': nrm(ks[7], (L, R, ATTN_HEADS, ATTN_HEAD_DIM), R ** -0.5),
        'w_pool_group': nrm(ks[8], (L, len(POOL_WINDOWS), POOL_GROUP, POOL_GROUP), POOL_GROUP ** -0.5),
        'pool_scale': 1.0 + nrm(ks[9], (L, POOL_WIDTH), 0.02),
        'w_branch_attn': nrm(ks[10], (L, ATTN_WIDTH, D), ATTN_WIDTH ** -0.5),
        'w_branch_pool': nrm(ks[11], (L, POOL_WIDTH, D), POOL_WIDTH ** -0.5),
        'w_out': nrm(ks[12], (L, D, D), D ** -0.5 * DEEPNORM_BETA),
        'ln1_g': 1.0 + nrm(ks[13], (L, D), 0.02),
        'ln1_b': nrm(ks[14], (L, D), 0.02),
        'w_router': nrm(ks[15], (L, D, E), D ** -0.5),
        'b_router': nrm(ks[16], (L, E), 0.01),
        'w_gate_up': nrm(ks[17], (L, E, D, 2 * F), D ** -0.5),
        'b_gate_up': nrm(ks[18], (L, E, 2 * F), 0.01),
        'w_down': nrm(ks[19], (L, E, F, D), F ** -0.5 * DEEPNORM_BETA),
        'b_down': nrm(ks[20], (L, E, D), 0.01),
        'ln2_g': 1.0 + nrm(ks[21], (L, D), 0.02),
        'ln2_b': nrm(ks[22], (L, D), 0.02),
    }


def reference(x, c, w_ada, b_ada, w_in, kv_norm_g, w_uk, w_uv, w_pool_group, pool_scale,
              w_branch_attn, w_branch_pool, w_out, ln1_g, ln1_b, w_router, b_router,
              w_gate_up, b_gate_up, w_down, b_down, ln2_g, ln2_b):
    B, S, D = x.shape
    cond = jax.nn.silu(c)
    for l in range(DEPTH):
        mod = (cond @ w_ada[l] + b_ada[l]).reshape(B, N_MOD, 1, D)
        shift1, scale1, gate1, shift2, scale2, gate2 = (mod[:, 0], mod[:, 1], mod[:, 2], mod[:, 3], mod[:, 4], mod[:, 5])

        h = x * (1.0 + scale1) + shift1
        q, ckv, qi, ki, wi, u, g = split_cols(h @ w_in[l])
        q = q.reshape(B, S, ATTN_HEADS, ATTN_HEAD_DIM)
        q_lat = jnp.einsum('bshd,rhd->bshr', q, w_uk[l]) * (ATTN_HEAD_DIM ** -0.5)
        ckv = rms_norm(ckv, kv_norm_g[l])
        qi = qi.reshape(B, S, IDX_HEADS, IDX_HEAD_DIM) * (IDX_HEAD_DIM ** -0.5)
        wi = wi * (IDX_HEADS ** -0.5)
        o_lat = dsa_attention(q_lat, ckv, qi, ki, wi)
        y_attn = jnp.einsum('bshr,rhd->bshd', o_lat, w_uv[l]).reshape(B, S, ATTN_WIDTH)
        pooled = multiscale_pool(u).reshape(B, S, len(POOL_WINDOWS), POOL_GROUP)
        y_pool = jnp.einsum('bsgc,gcd->bsgd', pooled, w_pool_group[l]).reshape(B, S, POOL_WIDTH) * pool_scale[l]
        g_attn, g_pool = g[..., :D], g[..., D:]
        mix = jax.nn.sigmoid(g_attn) * (y_attn @ w_branch_attn[l]) + jax.nn.sigmoid(g_pool) * (y_pool @ w_branch_pool[l])
        x = layer_norm(DEEPNORM_ALPHA * x + gate1 * (mix @ w_out[l]), ln1_g[l], ln1_b[l])

        h2 = x * (1.0 + scale2) + shift2
        y_moe = moe(h2, w_router[l], b_router[l], w_gate_up[l], b_gate_up[l], w_down[l], b_down[l])
        x = layer_norm(DEEPNORM_ALPHA * x + gate2 * y_moe, ln2_g[l], ln2_b[l])
    return x
```

```python
import numpy as np
from contextlib import ExitStack
import concourse.bass as bass
import concourse.mybir as mybir
from concourse.bass_utils import run_bass_kernel_spmd

F32 = mybir.dt.float32
BF16 = mybir.dt.bfloat16
U32 = mybir.dt.uint32
AF = mybir.ActivationFunctionType
ALU = mybir.AluOpType
AX = mybir.AxisListType

S_LEN = 2048
D = 1024
NT = 16
CAP = 1024
SG = 256
NG = CAP // SG
NE = 32
KBIS = 12
ALPHA = 2.0 ** 0.25
EPS = 1e-5
NEG = -1.0e30
XPAD_ROWS = NE * CAP + 2048

C_ID, C_CM, C_POW, C_EB = 0, 128, 256, 272
NCONST = 304
B_ID, B_TRI, B_ONE, B_BAND = 0, 128, 256, 384
B_ONE0 = 384 + 12 * 128
NB16 = B_ONE0 + 128

ENGS = ("pe", "act", "dve", "pool", "sp")


class Sched:
    def __init__(self, nc, stack, epoch=4000):
        self.nc = nc
        self.stack = stack
        self.epoch = epoch
        self.ops = {e: [] for e in ENGS}
        self.cnt = {e: 0 for e in ENGS}
        self.last_w = {}
        self.readers = {}
        self.dma_cnt = {}
        self.seen = {e: {} for e in ENGS}
        self.sems = {}
        self.nblock = 0
        self.alias = {}
        self.cond = ()
        self.cond_seen = []

    def _sem(self, sk):
        if sk not in self.sems:
            self.sems[sk] = self.stack.enter_context(self.nc.semaphore("sm%d" % len(self.sems)))
        return self.sems[sk]

    def _tok2sem(self, tok):
        if tok[0] == "eng":
            _, e, idx = tok
            ep = idx // self.epoch
            return ("eng", e, ep), idx - ep * self.epoch + 1
        _, key, n = tok
        return ("dma", key), 16 * n

    def add(self, eng, fn, reads=(), writes=(), dma_key=None, pe_chain=False, ndma=1):
        reads = tuple(x for r in reads for x in self.alias.get(r, (r,)))
        writes = tuple(x for w in writes for x in self.alias.get(w, (w,)))
        if eng == "pe":
            pe_chain = True
        deps = []
        for r in reads:
            t = self.last_w.get(r)
            if t is not None:
                deps.append(t)
        for w in writes:
            t = self.last_w.get(w)
            if t is not None:
                deps.append(t)
            deps.extend(self.readers.get(w, ()))
        waits = {}
        seen = self.seen[eng] if not self.cond else self.cond_seen[-1][eng]
        for tok in deps:
            if tok[0] == "eng" and tok[1] == eng and pe_chain:
                continue
            sk, v = self._tok2sem(tok)
            if seen.get(sk, 0) >= v:
                continue
            waits[sk] = max(waits.get(sk, 0), v)
        for sk, v in waits.items():
            seen[sk] = v
        if dma_key is not None:
            n = self.dma_cnt.get(dma_key, 0) + ndma
            self.dma_cnt[dma_key] = n
            tok = ("dma", dma_key, n)
            inc = (("dma", dma_key), 16)
        else:
            idx = self.cnt[eng]
            self.cnt[eng] = idx + 1
            tok = ("eng", eng, idx)
            inc = (("eng", eng, idx // self.epoch), 1)
        self.ops[eng].append(dict(fn=fn, waits=list(waits.items()), inc=inc, cond=self.cond,
                                  nd=(ndma if dma_key is not None else 1)))
        for r in reads:
            self.readers.setdefault(r, []).append(tok)
        for w in writes:
            self.last_w[w] = tok
            self.readers[w] = []
        return tok

    def begin_cond(self, cnt_ap, thresh):
        base = self.cond_seen[-1] if self.cond else self.seen
        self.cond = self.cond + ((cnt_ap, thresh),)
        self.cond_seen.append({e: dict(base[e]) for e in ENGS})

    def end_cond(self):
        self.cond = self.cond[:-1]
        self.cond_seen.pop()

    def drain_dmas(self, eng="sp"):
        waits = []
        for key, n in self.dma_cnt.items():
            sk = ("dma", key)
            if self.seen[eng].get(sk, 0) >= 16 * n:
                continue
            self.seen[eng][sk] = 16 * n
            waits.append((sk, 16 * n))
        self.ops[eng].append(dict(fn=None, waits=waits, inc=None, cond=(), nd=1))

    def emit_phase(self):
        nc = self.nc
        for e in ENGS:
            for op in self.ops[e]:
                for sk, _ in op["waits"]:
                    self._sem(sk)
                if op["inc"] is not None:
                    self._sem(op["inc"][0])
        sems = self.sems
        all_ops = self.ops
        self.ops = {e: [] for e in ENGS}
        self.nblock += 1
        with nc.Block() as block:
            engmap = {"pe": block.tensor, "act": block.scalar, "dve": block.vector,
                      "pool": block.gpsimd, "sp": block.sync}

            def run_op(e, op):
                for sk, v in op["waits"]:
                    e.wait_ge(sems[sk], v)
                if op["fn"] is not None:
                    inst = op["fn"](e)
                    sk, n = op["inc"]
                    if isinstance(inst, (list, tuple)):
                        for ii in inst:
                            ii.then_inc(sems[sk], n)
                    else:
                        inst.then_inc(sems[sk], n)

            def emit_ops(e, ops, depth):
                i = 0
                while i < len(ops):
                    op = ops[i]
                    if len(op["cond"]) <= depth:
                        run_op(e, op)
                        i += 1
                        continue
                    tag = op["cond"][depth]
                    j = i
                    while j < len(ops) and len(ops[j]["cond"]) > depth and ops[j]["cond"][depth] is tag:
                        j += 1
                    grp = ops[i:j]
                    cnt_ap, thresh = tag
                    self._nreg = getattr(self, "_nreg", 0) + 1
                    creg = e.alloc_register("condreg%d" % self._nreg)
                    e.reg_load(creg, cnt_ap)
                    with e.If_cmp(creg, thresh, comp_op="IS_GT"):
                        emit_ops(e, grp, depth + 1)
                    with e.Else():
                        incs = {}
                        for o2 in grp:
                            if o2["inc"] is None:
                                continue
                            sk, n = o2["inc"]
                            incs[sk] = incs.get(sk, 0) + n * o2["nd"]
                        for sk, tot in incs.items():
                            if sk[0] == "dma":
                                e.sem_inc(sems[sk], tot)
                            else:
                                e.drain().then_inc(sems[sk], tot)
                    e.free_register(creg)
                    i = j

            def mk(ops):
                def body(e):
                    emit_ops(e, ops, 0)
                return body

            for ename in ENGS:
                if all_ops[ename]:
                    engmap[ename](mk(all_ops[ename]))


def make_consts():
    c = np.zeros((128, NCONST), np.float32)
    cbs = np.zeros((128, NB16), np.float32)
    ar = np.arange(128)
    c[:, C_ID:C_ID + 128] = np.eye(128)
    cbs[:, B_ID:B_ID + 128] = np.eye(128)
    cbs[:, B_TRI:B_TRI + 128] = (ar[:, None] < ar[None, :])
    cbs[:, B_ONE:B_ONE + 128] = 1.0
    cbs[0, B_ONE0:B_ONE0 + 128] = 1.0
    c[:, C_CM:C_CM + 128] = np.where(ar[None, :] <= ar[:, None], 0.0, NEG)
    for wg, w in enumerate((2, 4, 8, 16)):
        tp = ar[:, None]
        t = ar[None, :]
        dd = t - tp
        main = np.where((dd >= 0) & (dd < w), 1.0 / w, 0.0) - (dd == 0)
        prev = np.where((t + 128 - tp) < w, 1.0 / w, 0.0)
        cntf = np.minimum(w, t + 1).astype(np.float64)
        first = np.where((dd >= 0) & (dd < w), 1.0 / cntf, 0.0) - (dd == 0)
        for v, m in enumerate((main, prev, first)):
            o = B_BAND + 128 * (3 * wg + v)
            cbs[:, o:o + 128] = m
    c[:, C_POW:C_POW + 16] = 2.0 ** (-np.arange(16))[None, :]
    c[:, C_EB:C_EB + 32] = (np.arange(32) * CAP)[None, :]
    slopes = 2.0 ** (-8.0 * np.arange(1, 9) / 8)
    al = np.zeros((3, 16, 128), np.float32)
    al[0] = ar[None, :]
    al[1] = (-128.0 * np.arange(16))[:, None]
    al[2] = 1.0
    arr = np.zeros((3, 8, 128), np.float32)
    arr[0] = slopes[:, None]
    arr[1] = slopes[:, None]
    arr[2] = -slopes[:, None] * ar[None, :]
    return c, cbs, al.reshape(3, 2048), arr.reshape(3, 1024)


def build_nc(dbg=(), stop_after=None):
    nc = bass.Bass("TRN2", target_bir_lowering=False)
    dt = lambda name, shape, dty=F32: nc.dram_tensor(name, list(shape), dty, kind="ExternalInput").ap()
    x_d = dt("x", [S_LEN, D])
    ccol_d = dt("c_col", [128, 8])
    wada_d = dt("w_ada", [D, 6 * D])
    bada_d = dt("b_ada", [1, 6 * D])
    win_d = dt("w_in", [D, 3784])
    kvg_d = dt("kv_norm_g", [1, 128])
    wuk_d = dt("w_uk", [128, 512])
    wuv_d = dt("w_uv", [128, 512])
    wpool_d = dt("w_pool", [4, 128, 128])
    pscale_d = dt("pool_scale_col", [128, 4])
    wba_d = dt("w_ba", [512, D])
    wbp_d = dt("w_bp", [512, D])
    wout_d = dt("w_out", [D, D])
    ln1g_d = dt("ln1_g", [1, D])
    ln1b_d = dt("ln1_b", [1, D])
    wr_d = dt("w_router", [D, NE])
    br_d = dt("b_router", [1, NE])
    wgu_d = dt("w_gate_up", [NE, D, 2 * D])
    bgu_d = dt("b_gu_col", [128, NE * 16])
    wd_d = dt("w_down", [NE, D, D])
    bd_d = dt("b_down", [NE, D])
    ln2g_d = dt("ln2_g", [1, D])
    ln2b_d = dt("ln2_b", [1, D])
    consts_d = dt("consts", [128, NCONST])
    constsb_d = dt("constsb", [128, NB16])
    alL_d = dt("alibiL", [3, 2048])
    alR_d = dt("alibiR", [3, 1024])
    out_d = nc.dram_tensor("out", [S_LEN, D], F32, kind="ExternalOutput").ap()
    x1_d = nc.dram_tensor("x1_scr", [S_LEN, D], F32, kind="Internal").ap()
    xpad_d = nc.dram_tensor("xpad_scr", [XPAD_ROWS, D], BF16, kind="Internal").ap()
    ypad_d = nc.dram_tensor("ypad_scr", [XPAD_ROWS, D], BF16, kind="Internal").ap()
    dbg_out = {}

    with ExitStack() as top:
        S = Sched(nc, top)
        S.alias = {"score0": ("score0a", "score0b"), "score1": ("score1a", "score1b")}
        add = S.add
        PS = [top.enter_context(nc.psum_tensor("psb%d" % i, [128, 512], F32)) for i in range(8)]
        psn = lambda b: "ps%d" % b
        rot_state = [0]

        def rot():
            b = rot_state[0] % 8
            rot_state[0] += 1
            return b

        def T(stack, name, shape, dty):
            return stack.enter_context(nc.sbuf_tensor(name, list(shape), dty))

        def dump(name, ap, shape, dty, reads):
            if name not in dbg:
                return
            o = nc.dram_tensor("dbg_" + name, list(shape), dty, kind="ExternalOutput").ap()
            dbg_out[name] = o
            add("sp", lambda e: e.dma_start(out=o, in_=ap), reads=reads, writes=["dbg_" + name], dma_key="dbg_" + name)

        cst = T(top, "cst", [128, NCONST], F32)
        cb = T(top, "cb", [128, NB16], BF16)
        modcol = T(top, "modcol", [128, 4, 8], F32)
        gate_bc = T(top, "gate_bc", [128, 2, D], F32)
        gates = T(top, "gates", [128, NT, 4], F32)
        desti = T(top, "desti", [128, NT, 4], U32)
        cnt_f = T(top, "cnt_f", [1, NE], F32)
        cnt_i = T(top, "cnt_i", [1, NE], mybir.dt.int32)
        identF = cst[:, C_ID:C_ID + 128]
        identB = cb[:, B_ID:B_ID + 128]
        triB = cb[:, B_TRI:B_TRI + 128]
        onesB = cb[:, B_ONE:B_ONE + 128]
        ones0B = cb[:, B_ONE0:B_ONE0 + 128]
        cmaskF = cst[:, C_CM:C_CM + 128]
        band = lambda wg, v: cb[:, B_BAND + 128 * (3 * wg + v):B_BAND + 128 * (3 * wg + v) + 128]

        add("sp", lambda e: e.dma_start(out=cst[:], in_=consts_d), writes=["cst"], dma_key="cst")
        add("pool", lambda e: e.dma_start(out=cb[:], in_=constsb_d), writes=["cb"], dma_key="cb")

        with ExitStack() as ph:
            NSLOT = 4
            ring = [T(ph, "wr%d" % i, [128, 4096], BF16) for i in range(NSLOT)]
            loads = []
            wada_v = wada_d.rearrange("(c p) n -> p c n", p=128)
            ada_load = lambda j: (wada_v[:, :, 512 * j:512 * (j + 1)], 8, 512)
            for j in range(4):
                loads.append(ada_load(j))
            win_v = win_d.rearrange("(c p) n -> p c n", p=128)
            wba_v = wba_d.rearrange("(c p) n -> p c n", p=128)
            wbp_v = wbp_d.rearrange("(c p) n -> p c n", p=128)
            wout_v = wout_d.rearrange("(c p) n -> p c n", p=128)
            GCH = [(0, 512), (512, 1024), (1024, 1224), (1224, 1736),
                   (1736, 2248), (2248, 2760), (2760, 3272), (3272, 3784)]
            for g in range(4):
                for (a, b) in GCH:
                    loads.append((win_v[:, :, a:b], 8, b - a))
                if g == 0:
                    for j in range(4, 12):
                        loads.append(ada_load(j))
                loads.append((wba_v, 4, 1024))
                loads.append((wbp_v, 4, 1024))
                loads.append((wout_v[:, :, 0:512], 8, 512))
                loads.append((wout_v[:, :, 512:1024], 8, 512))
            issued = [0]
            LOOK = 2

            def wget(n):
                while issued[0] <= min(n + LOOK, len(loads) - 1):
                    k = issued[0]
                    src, c_, n_ = loads[k]
                    slot = ring[k % NSLOT]
                    dst = slot[:, 0:c_ * n_].rearrange("p (c n) -> p c n", c=c_)
                    add("pool", lambda e, dst=dst, src=src: e.dma_start(out=dst, in_=src),
                        writes=["wr%d" % (k % NSLOT)], dma_key="wr%d" % (k % NSLOT))
                    issued[0] += 1
                src, c_, n_ = loads[n]
                return "wr%d" % (n % NSLOT), ring[n % NSLOT][:, 0:c_ * n_].rearrange("p (c n) -> p c n", c=c_)

            x1b = T(ph, "x1b", [128, D], BF16)
            wuk_n = x1b
            wukT2 = T(ph, "wukT2", [128, 4, 128], BF16)
            wuv = T(ph, "wuv", [128, 512], BF16)
            wpool = T(ph, "wpool", [128, 4, 128], BF16)
            pscale = T(ph, "pscale", [128, 4], F32)
            kvg_bc = T(ph, "kvg_bc", [128, 128], F32)
            ln1g_bc = T(ph, "ln1g_bc", [128, D], F32)
            ln1b_bc = T(ph, "ln1b_bc", [128, D], F32)
            wrt = T(ph, "wrt", [128, 8, NE], F32)
            br_bc = T(ph, "br_bc", [128, NE], F32)
            alL = T(ph, "alL", [128, 2048], BF16)
            alR = T(ph, "alR", [128, 1024], BF16)
            Mall = T(ph, "Mall", [128, NT, NE], BF16)
            NM = T(ph, "NM", [128, S_LEN], BF16)
            mixT = T(ph, "mixT", [128, 8, 512], BF16)
            add("pool", lambda e: e.memset(mixT[:, 0:2, :], 0.0), writes=["mixT"])
            xp = T(ph, "xp", [128, D], F32)
            xr = T(ph, "xr", [128, D], F32)
            diag = [T(ph, "diag%d" % i, [128, 8, 128], BF16) for i in range(2)]

            add("pool", lambda e: e.dma_start(out=wuk_n[:, 0:512], in_=wuk_d), writes=["x1b"], dma_key="wuk_n")
            add("pool", lambda e: e.dma_start(out=wuv[:], in_=wuv_d), writes=["wuv"], dma_key="wuv")
            add("pool", lambda e: e.dma_start(out=wpool[:], in_=wpool_d.rearrange("g c d -> c g d")), writes=["wpool"], dma_key="wpool")
            add("sp", lambda e: e.dma_start(out=pscale[:], in_=pscale_d), writes=["pscale"], dma_key="pscale")
            add("sp", lambda e: e.dma_start(out=kvg_bc[:], in_=kvg_d.broadcast_to([128, 128])), writes=["kvg_bc"], dma_key="kvg_bc")
            add("sp", lambda e: e.dma_start(out=ln1g_bc[:], in_=ln1g_d.broadcast_to([128, D])), writes=["ln1g_bc"], dma_key="ln1g_bc")
            add("sp", lambda e: e.dma_start(out=ln1b_bc[:], in_=ln1b_d.broadcast_to([128, D])), writes=["ln1b_bc"], dma_key="ln1b_bc")
            add("sp", lambda e: e.dma_start(out=wrt[:], in_=wr_d.rearrange("(c p) n -> p c n", p=128)), writes=["wrt"], dma_key="wrt")
            add("sp", lambda e: e.dma_start(out=br_bc[:], in_=br_d.broadcast_to([128, NE])), writes=["br_bc"], dma_key="br_bc")
            add("pool", lambda e: e.memset(alL[:], 0.0), writes=["alL"])
            add("pool", lambda e: e.memset(alR[:], 0.0), writes=["alR"])
            add("pool", lambda e: e.dma_start(out=alL[0:3, :], in_=alL_d), writes=["alL"], dma_key="alL")
            add("pool", lambda e: e.dma_start(out=alR[0:3, :], in_=alR_d), writes=["alR"], dma_key="alR")
            zf_list = list(range(0, XPAD_ROWS, 2048))

            def zero_fill(nmax):
                for _ in range(nmax):
                    if not zf_list:
                        return
                    r0 = zf_list.pop(0)
                    nr = min(2048, XPAD_ROWS - r0)
                    add("sp", lambda e, r0=r0, nr=nr: e.dma_start(
                        out=xpad_d[r0:r0 + nr, :].rearrange("(p a) n -> p a n", p=128),
                        in_=mixT[:, 0:2, :].rearrange("p a b -> p (a b)").unsqueeze(1).broadcast_to([128, nr // 128, 1024])),
                        reads=["mixT"], writes=["xpadZ%d" % r0], dma_key="xpadZ")
            xpadZ = ["xpadZ%d" % r0 for r0 in range(0, XPAD_ROWS, 2048)]

            b = rot()
            psb = PS[b][:].bitcast(BF16)

            def f_wukT(e, psb=psb):
                last = None
                for pr in range(4):
                    last = e.transpose(out=psb[:, pr * 128:(pr + 1) * 128], in_=wuk_n[:, pr * 128:(pr + 1) * 128], identity=identB)
                return last
            add("pe", f_wukT, reads=["x1b", "cb"], writes=[psn(b)])
            add("dve", lambda e, psb=psb: e.tensor_copy(out=wukT2[:].rearrange("p a b -> p (a b)"), in_=psb[:, 0:512]), reads=[psn(b)], writes=["wukT2"])

            c_sb = T(ph, "c_sb", [128, 8], F32)
            c_si = T(ph, "c_si", [128, 8], BF16)
            olatn = T(ph, "olatn", [128, 8, 128], BF16)
            condB = olatn
            bb = [xp[:, 0:512], xp[:, 512:1024]]
            modtmp = [xr[:, 0:512], xr[:, 512:1024]]
            add("sp", lambda e: e.dma_start(out=c_sb[:], in_=ccol_d), writes=["c_sb"], dma_key="c_sb")
            add("act", lambda e: e.activation(out=c_si[:], in_=c_sb[:], func=AF.Silu), reads=["c_sb"], writes=["c_si"])
            add("dve", lambda e: e.tensor_copy(out=condB[:], in_=c_si[:].unsqueeze(2).broadcast_to([128, 8, 128])), reads=["c_si"], writes=["olatn"])
            VEC = {0: ("col", 1, 0.0), 1: ("col", 0, 1.0), 2: ("gate", 0, 0.0), 3: ("col", 3, 0.0), 4: ("col", 2, 1.0), 5: ("gate", 1, 0.0)}
            def ada_chunk(j):
                v, half = j // 2, j % 2
                wres, wv = wget(j if j < 4 else j + 8)
                add("act", lambda e, j=j: e.dma_start(out=bb[j % 2], in_=bada_d[:, 512 * j:512 * (j + 1)].broadcast_to([128, 512])),
                    writes=["xp%d" % (j % 2)], dma_key="xp%d" % (j % 2))
                b = rot()

                def f_mod(e, b=b, wv=wv):
                    last = None
                    for k in range(8):
                        last = e.matmul(PS[b][:], lhsT=condB[:, k, :], rhs=wv[:, k, :], start=(k == 0), stop=(k == 7))
                    return last
                add("pe", f_mod, reads=["olatn", wres], writes=[psn(b)])
                kind, idx, addc = VEC[v]
                if kind == "gate":
                    add("dve", lambda e, b=b, j=j, idx=idx, half=half: e.tensor_tensor(
                        out=gate_bc[:, idx, half * 512:(half + 1) * 512], in0=PS[b][:], in1=bb[j % 2], op=ALU.add),
                        reads=[psn(b), "xp%d" % (j % 2)], writes=["gate_bc%d_%d" % (idx, half)])
                else:
                    mt = modtmp[j % 2]
                    add("dve", lambda e, b=b, j=j, mt=mt: e.tensor_tensor(out=mt, in0=PS[b][:], in1=bb[j % 2], op=ALU.add),
                        reads=[psn(b), "xp%d" % (j % 2)], writes=["xr"])
                    b2 = rot()

                    def f_tr(e, b2=b2, mt=mt):
                        last = None
                        for q4 in range(4):
                            last = e.transpose(out=PS[b2][:, q4 * 128:(q4 + 1) * 128], in_=mt[:, q4 * 128:(q4 + 1) * 128], identity=identF)
                        return last
                    add("pe", f_tr, reads=["xr", "cst"], writes=[psn(b2)])
                    add("dve", lambda e, b2=b2, idx=idx, half=half, addc=addc: e.tensor_scalar(
                        out=modcol[:, idx, 4 * half:4 * half + 4], in0=PS[b2][:].rearrange("p (a b) -> p a b", a=4)[:, :, 0],
                        scalar1=addc, scalar2=None, op0=ALU.add),
                        reads=[psn(b2)], writes=["modcol%d_%d" % (idx, half)])
            for j in range(4):
                ada_chunk(j)
            MODC = ["modcol%d_%d" % (i, h) for i in range(4) for h in range(2)]
            MODC1 = ["modcol%d_%d" % (i, h) for i in range(2) for h in range(2)]
            GATE1 = ["gate_bc0_0", "gate_bc0_1"]
            GATE2 = ["gate_bc1_0", "gate_bc1_1"]
            dump("modcol", modcol[:], [128, 4, 8], F32, MODC)
            dump("gate_bc", gate_bc[:], [128, 2, D], F32, GATE1 + GATE2)

            if stop_after == "A":
                S.drain_dmas("sp")
                S.emit_phase()
                return nc, dbg_out

            xs = [xr, xp]
            hT = T(ph, "hT", [128, 8, 512], BF16)
            qT = T(ph, "qT", [128, 4, 512], BF16)
            qlatT = T(ph, "qlatT", [128, 8, 512], BF16)
            qiT = T(ph, "qiT", [128, 4, 512], BF16)
            kiT2 = T(ph, "kiT2", [128, 2, S_LEN], BF16)
            ckv_tok = T(ph, "ckv_tok", [128, NT, 128], BF16)
            ckvT = T(ph, "ckvT", [128, S_LEN], BF16)
            wi_tok = T(ph, "wi_tok", [128, 4, 8], F32)
            u_buf = T(ph, "u_buf", [128, 5, 512], BF16)
            pooledT = T(ph, "pooledT", [128, 4, 512], BF16)
            ypoolT = T(ph, "ypoolT", [128, 4, 512], BF16)
            yattnT = T(ph, "yattnT", [128, 4, 512], BF16)
            sgT = T(ph, "sgT", [128, 16, 512], BF16)
            ssq = T(ph, "ssq", [128, 4], F32)
            rstd4 = T(ph, "rstd4", [128, 4], F32)
            sqj = T(ph, "sqj", [128, 128], BF16)
            score = [T(ph, "score%d" % i, [128, S_LEN], F32) for i in range(2)]
            NMT = T(ph, "NMT", [128, NT, 128], BF16)
            Rb = [T(ph, "Rb%d" % i, [128, 512], BF16) for i in range(3)]
            PT = [T(ph, "PT%d" % i, [128, 512], BF16) for i in range(2)]
            rden = T(ph, "rden", [128, 512], F32)
            bis = T(ph, "bis", [128, 8], F32)
            wtab = T(ph, "wtab", [128, 16], F32)
            amax = [T(ph, "amax%d" % i, [128, 4], F32) for i in range(2)]
            t1 = T(ph, "t1", [128, 512], F32)
            t2 = T(ph, "t2", [128, 512], F32)
            x1t = T(ph, "x1t", [128, D], F32)
            h2T = T(ph, "h2T", [128, 8, 128], F32)
            lnst = T(ph, "lnst", [128, 2, 6], F32)
            lnmv = T(ph, "lnmv", [128, 4], F32)
            lg = T(ph, "lg", [128, NE], F32)
            top8 = T(ph, "top8", [128, 8], F32)
            rsm = T(ph, "rsm", [128, 8], F32)
            e4 = T(ph, "e4", [128, 4], F32)
            destf = T(ph, "destf", [128, NE], F32)
            destk = T(ph, "destk", [128, 4], F32)
            junk32 = T(ph, "junk32", [128, NE], F32)

            evac_flip = [0]

            def evac(out_ap, in_ap, reads, writes, eng=None):
                if eng is None:
                    eng = "act" if evac_flip[0] % 2 == 0 else "dve"
                    evac_flip[0] += 1
                if eng == "act":
                    add("act", lambda e: e.activation(out=out_ap, in_=in_ap, func=AF.Identity), reads=reads, writes=writes)
                else:
                    add("dve", lambda e: e.tensor_copy(out=out_ap, in_=in_ap), reads=reads, writes=writes)

            HT = ["hT0", "hT1", "hT2", "hT3"]

            def mm8(b, out_ap, lhs_fn, rhs_fn, reads, nk=8):
                def f(e):
                    last = None
                    for k in range(nk):
                        last = e.matmul(out_ap, lhsT=lhs_fn(k), rhs=rhs_fn(k), start=(k == 0), stop=(k == nk - 1))
                    return last
                add("pe", f, reads=reads, writes=[psn(b)])

            CH0 = 12
            for g in range(4):
                cbase = 4 if g == 0 else 24 + 12 * (g - 1)
                cb8 = cbase + (16 if g == 0 else 8)
                for t in range(4):
                    i = 4 * g + t
                    xb_ = xs[i % 2]
                    xsn = ["xr"] if i % 2 == 0 else ["xp0", "xp1"]
                    add("sp", lambda e, i=i, xb_=xb_: e.dma_start(out=xb_[:], in_=x_d[i * 128:(i + 1) * 128, :]),
                        writes=xsn, dma_key="xs%d" % (i % 2))
                    for hf in range(2):
                        b = rot()

                        def f_xt(e, b=b, xb_=xb_, hf=hf):
                            last = None
                            for c4 in range(4):
                                c = 4 * hf + c4
                                last = e.transpose(out=PS[b][:, c4 * 128:(c4 + 1) * 128], in_=xb_[:, c * 128:(c + 1) * 128], identity=identF)
                            return last
                        add("pe", f_xt, reads=xsn + ["cst"], writes=[psn(b)])
                        for c4 in range(4):
                            c = 4 * hf + c4
                            if c4 % 2 == 0:
                                add("act", lambda e, b=b, c=c, c4=c4, t=t: e.activation(
                                    out=hT[:, c, t * 128:(t + 1) * 128], in_=PS[b][:, c4 * 128:(c4 + 1) * 128], func=AF.Identity,
                                    scale=modcol[:, 0, c:c + 1], bias=modcol[:, 1, c:c + 1]),
                                    reads=[psn(b)] + MODC1, writes=["hT%d" % t])
                            else:
                                add("dve", lambda e, b=b, c=c, c4=c4, t=t: e.tensor_scalar(
                                    out=hT[:, c, t * 128:(t + 1) * 128], in0=PS[b][:, c4 * 128:(c4 + 1) * 128],
                                    scalar1=modcol[:, 0, c:c + 1], scalar2=modcol[:, 1, c:c + 1], op0=ALU.mult, op1=ALU.add),
                                    reads=[psn(b)] + MODC1, writes=["hT%d" % t])
                if g == 0:
                    zero_fill(100)
                    dump("hT", hT[:], [128, 8, 512], BF16, HT)

                wres, wv = wget(cbase + 0)
                for pr in range(4):
                    b = rot()
                    mm8(b, PS[b][:], lambda k, wv=wv, pr=pr: wv[:, k, pr * 128:(pr + 1) * 128], lambda k: hT[:, k, :], [wres] + HT)
                    evac(qT[:, pr, :], PS[b][:], [psn(b)], ["qT"])
                for h in range(8):
                    pr, hb = h // 2, (h % 2) * 64
                    b = rot()
                    add("pe", lambda e, b=b, pr=pr, hb=hb: e.matmul(PS[b][:], lhsT=wukT2[hb:hb + 64, pr, :], rhs=qT[hb:hb + 64, pr, :], start=True, stop=True),
                        reads=["wukT2", "qT"], writes=[psn(b)])
                    if h % 2 == 0:
                        add("act", lambda e, b=b, h=h: e.activation(out=qlatT[:, h, :], in_=PS[b][:], func=AF.Identity, scale=0.125),
                            reads=[psn(b)], writes=["qlatT"])
                    else:
                        add("dve", lambda e, b=b, h=h: e.tensor_scalar(out=qlatT[:, h, :], in0=PS[b][:], scalar1=0.125, scalar2=None, op0=ALU.mult),
                            reads=[psn(b)], writes=["qlatT"])
                if g == 0:
                    dump("qlatT", qlatT[:], [128, 8, 512], BF16, ["qlatT"])

                wres, wv = wget(cbase + 1)
                b = rot()
                for t in range(4):
                    mm8(b, PS[b][:, t * 128:(t + 1) * 128], lambda k, t=t: hT[:, k, t * 128:(t + 1) * 128], lambda k, wv=wv: wv[:, k, 0:128], [wres] + HT)
                for t in range(4):
                    add("act", lambda e, b=b, t=t: e.activation(out=sqj[:], in_=PS[b][:, t * 128:(t + 1) * 128], func=AF.Square, accum_out=ssq[:, t:t + 1]),
                        reads=[psn(b)], writes=["sqj", "ssq"])
                add("dve", lambda e: e.tensor_scalar(out=rstd4[:], in0=ssq[:], scalar1=1.0 / 128, scalar2=EPS, op0=ALU.mult, op1=ALU.add), reads=["ssq"], writes=["rstd4"])
                add("act", lambda e: e.activation(out=rstd4[:], in_=rstd4[:], func=AF.Sqrt), reads=["rstd4"], writes=["rstd4"])
                add("dve", lambda e: e.reciprocal(out=rstd4[:], in_=rstd4[:]), reads=["rstd4"], writes=["rstd4"])
                for t in range(4):
                    i = 4 * g + t
                    add("dve", lambda e, b=b, t=t, i=i: e.scalar_tensor_tensor(
                        out=ckv_tok[:, i, :], in0=PS[b][:, t * 128:(t + 1) * 128], scalar=rstd4[:, t:t + 1], in1=kvg_bc[:], op0=ALU.mult, op1=ALU.mult),
                        reads=[psn(b), "rstd4", "kvg_bc"], writes=["ckv_tok%d" % i])
                b2 = rot()
                psb2 = PS[b2][:].bitcast(BF16)

                def f_ckT(e, psb2=psb2, g=g):
                    last = None
                    for t in range(4):
                        last = e.transpose(out=psb2[:, t * 128:(t + 1) * 128], in_=ckv_tok[:, 4 * g + t, :], identity=identB)
                    return last
                add("pe", f_ckT, reads=["ckv_tok%d" % (4 * g + t) for t in range(4)] + ["cb"], writes=[psn(b2)])
                evac(ckvT[:, g * 512:(g + 1) * 512], psb2[:, 0:512], [psn(b2)], ["ckvT%d" % g])
                for pr in range(3):
                    b = rot()
                    mm8(b, PS[b][:], lambda k, wv=wv, pr=pr: wv[:, k, 128 + pr * 128:256 + pr * 128], lambda k: hT[:, k, :], [wres] + HT)
                    evac(qiT[:, pr, :], PS[b][:], [psn(b)], ["qiT"])
                wres, wv = wget(cbase + 2)
                b = rot()
                mm8(b, PS[b][:], lambda k, wv=wv: wv[:, k, 0:128], lambda k: hT[:, k, :], [wres] + HT)
                evac(qiT[:, 3, :], PS[b][:], [psn(b)], ["qiT"])
                b = rot()
                mm8(b, PS[b][0:64, :], lambda k, wv=wv: wv[:, k, 128:192], lambda k: hT[:, k, :], [wres] + HT)
                mm8(b, PS[b][64:128, :], lambda k, wv=wv: wv[:, k, 128:192], lambda k: hT[:, k, :], [wres] + HT)
                if g == 0:
                    add("pool", lambda e: e.memset(kiT2[:], 0.0), writes=["kiT2_z"])
                evac(kiT2[0:64, 0, g * 512:(g + 1) * 512], PS[b][0:64, :], [psn(b), "kiT2_z"], ["kiT2_%d" % g])
                evac(kiT2[64:128, 1, g * 512:(g + 1) * 512], PS[b][64:128, :], [psn(b), "kiT2_z"], ["kiT2_%d" % g])
                b = rot()
                for t in range(4):
                    mm8(b, PS[b][:, t * 8:(t + 1) * 8], lambda k, t=t: hT[:, k, t * 128:(t + 1) * 128], lambda k, wv=wv: wv[:, k, 192:200], [wres] + HT)
                evac(wi_tok[:].rearrange("p a b -> p (a b)"), PS[b][:, 0:32], [psn(b)], ["wi_tok"], eng="dve")
                if g == 0:
                    dump("ckv_tok", ckv_tok[:, 0:4, :], [128, 4, 128], BF16, ["ckv_tok%d" % t for t in range(4)])
                    dump("ckvT", ckvT[:, 0:512], [128, 512], BF16, ["ckvT0"])
                    dump("qiT", qiT[:], [128, 4, 512], BF16, ["qiT"])
                    dump("kiT2", kiT2[:, 0, 0:512], [128, 512], BF16, ["kiT2_0"])
                    dump("wi_tok", wi_tok[:], [128, 4, 8], F32, ["wi_tok"])

                def stage_B4sg(g=g, cbase=cbase):
                    wres, wv = wget(cbase + 3)
                    if g > 0:
                        add("pool", lambda e: e.tensor_copy(out=u_buf[:, 0, :], in_=u_buf[:, 4, :]), reads=["u4"], writes=["u0"])
                    for t in range(4):
                        b = rot()
                        mm8(b, PS[b][:], lambda k, t=t: hT[:, k, t * 128:(t + 1) * 128], lambda k, wv=wv: wv[:, k, :], [wres] + HT)
                        evac(u_buf[:, t + 1, :], PS[b][:], [psn(b)], ["u%d" % (t + 1)])
                    for t in range(4):
                        i = 4 * g + t
                        b = rot()

                        def f_pool(e, b=b, t=t, i=i):
                            last = None
                            for wg in range(4):
                                o = PS[b][:, wg * 128:(wg + 1) * 128]
                                if i == 0:
                                    last = e.matmul(o, lhsT=u_buf[:, t + 1, wg * 128:(wg + 1) * 128], rhs=band(wg, 2), start=True, stop=True)
                                else:
                                    e.matmul(o, lhsT=u_buf[:, t + 1, wg * 128:(wg + 1) * 128], rhs=band(wg, 0), start=True, stop=False)
                                    last = e.matmul(o, lhsT=u_buf[:, t, wg * 128:(wg + 1) * 128], rhs=band(wg, 1), start=False, stop=True)
                            return last
                        add("pe", f_pool, reads=["u%d" % t, "u%d" % (t + 1), "cb"], writes=[psn(b)])
                        evac(pooledT[:, :, t * 128:(t + 1) * 128], PS[b][:].rearrange("p (a b) -> p a b", a=4), [psn(b)], ["pooledT"])
                    for wg in range(4):
                        b = rot()
                        add("pe", lambda e, b=b, wg=wg: e.matmul(PS[b][:], lhsT=wpool[:, wg, :], rhs=pooledT[:, wg, :], start=True, stop=True),
                            reads=["wpool", "pooledT"], writes=[psn(b)])
                        add("act", lambda e, b=b, wg=wg: e.activation(out=ypoolT[:, wg, :], in_=PS[b][:], func=AF.Identity, scale=pscale[:, wg:wg + 1]),
                            reads=[psn(b), "pscale"], writes=["ypoolT"])
                    if g == 0:
                        dump("pooledT", pooledT[:], [128, 4, 512], BF16, ["pooledT"])
                        dump("ypoolT", ypoolT[:], [128, 4, 512], BF16, ["ypoolT"])

                    for m in range(4):
                        wres, wv = wget(cbase + 4 + m)
                        for q4 in range(4):
                            n = 4 * m + q4
                            b = rot()
                            mm8(b, PS[b][:], lambda k, wv=wv, q4=q4: wv[:, k, q4 * 128:(q4 + 1) * 128], lambda k: hT[:, k, :], [wres] + HT)
                            add("act", lambda e, b=b, n=n: e.activation(out=sgT[:, n, :], in_=PS[b][:], func=AF.Sigmoid), reads=[psn(b)], writes=["sgT"])
                    if g == 0:
                        dump("sgT", sgT[:], [128, 16, 512], BF16, ["sgT"])


                if stop_after == "B4":
                    S.drain_dmas("sp")
                    S.emit_phase()
                    return nc, dbg_out

                def build_diag(t, g=g):
                    i = 4 * g + t
                    dg = diag[i % 2]
                    dgn = "diag%d" % (i % 2)
                    for h in range(8):
                        add("dve", lambda e, h=h, t=t, dg=dg: e.tensor_scalar(out=dg[:, h, :], in0=identB, scalar1=wi_tok[:, t, h:h + 1], scalar2=None, op0=ALU.mult),
                            reads=["cb", "wi_tok"], writes=[dgn])

                def indexer(t, g=g):
                    i = 4 * g + t
                    sc = score[i % 2]
                    scn = "score%d" % (i % 2)
                    dg = diag[i % 2]
                    dgn = "diag%d" % (i % 2)
                    nch = i // 4 + 1
                    for c in range(nch):
                        N = 512 if c < nch - 1 else 128 * (i % 4 + 1)
                        bS = 1

                        def dots(h, c=c, N=N, t=t):
                            pr, hb = h // 2, (h % 2) * 64
                            bD = 6 + (h % 2)
                            R = Rb[h % 3]
                            add("pe", lambda e, bD=bD, pr=pr, hb=hb: e.matmul(
                                PS[bD][:, 0:N], lhsT=qiT[:, pr, t * 128:(t + 1) * 128], rhs=kiT2[:, hb // 64, c * 512:c * 512 + N], start=True, stop=True),
                                reads=["qiT", "kiT2_%d" % c], writes=[psn(bD)])
                            add("act", lambda e, bD=bD, R=R: e.activation(out=R[:, 0:N], in_=PS[bD][:, 0:N], func=AF.Relu),
                                reads=[psn(bD)], writes=["Rb%d" % (h % 3)])

                        def wsum(h, N=N, bS=bS):
                            R = Rb[h % 3]
                            add("pe", lambda e, h=h, R=R: e.matmul(PS[bS][:, 0:N], lhsT=dg[:, h, :], rhs=R[:, 0:N], start=(h == 0), stop=(h == 7)),
                                reads=[dgn, "Rb%d" % (h % 3)], writes=[psn(bS)], pe_chain=True)
                        dots(0)
                        dots(1)
                        for h in range(8):
                            wsum(h)
                            if h + 2 < 8:
                                dots(h + 2)
                        add("act", lambda e, bS=bS, c=c, sc=sc, N=N: e.activation(out=sc[:, c * 512:c * 512 + N], in_=PS[bS][:, 0:N], func=AF.Identity),
                            reads=[psn(bS)], writes=[scn])
                        add("dve", lambda e, N=N, c=c, i=i, sc=sc: e.tensor_reduce(out=amax[i % 2][:, c:c + 1], in_=sc[:, c * 512:c * 512 + N], axis=AX.X, op=ALU.max, apply_absolute_value=True),
                            reads=[scn], writes=["amax%d" % (i % 2)])
                        if c == nch - 1:
                            add("dve", lambda e, sc=sc, i=i: e.tensor_tensor(out=sc[:, i * 128:(i + 1) * 128], in0=sc[:, i * 128:(i + 1) * 128], in1=cmaskF, op=ALU.add),
                                reads=[scn, "cst"], writes=[scn])
                    return nch

                def threshold(t, nch, g=g):
                    i = 4 * g + t
                    sc = score[i % 2]
                    scn = "score%d" % (i % 2)
                    L = 128 * (i + 1)
                    A, mid, cnt, tmp, thr = (bis[:, k:k + 1] for k in range(5))
                    if i < 2:
                        add("dve", lambda e: e.memset(thr, -1.0e29), writes=["bis"])
                    else:
                        add("dve", lambda e: e.tensor_reduce(out=A, in_=amax[i % 2][:, 0:nch], axis=AX.X, op=ALU.max), reads=["amax%d" % (i % 2)], writes=["bis"])
                        add("dve", lambda e: e.tensor_scalar(out=wtab[:, 0:KBIS + 1], in0=cst[:, C_POW:C_POW + KBIS + 1], scalar1=A, scalar2=None, op0=ALU.mult),
                            reads=["bis", "cst"], writes=["wtab"])
                        add("dve", lambda e: e.memset(mid, 0.0), writes=["bis"])
                        for k in range(KBIS):
                            add("dve", lambda e: e.tensor_scalar(out=NM[:, 0:L], in0=sc[:, 0:L], scalar1=mid, scalar2=None, op0=ALU.is_ge, op1=ALU.add, accum_out=cnt),
                                reads=[scn, "bis"], writes=["NM", "bis"])
                            add("dve", lambda e: e.tensor_scalar(out=tmp, in0=cnt, scalar1=256.0, scalar2=-0.5, op0=ALU.is_ge, op1=ALU.add), reads=["bis"], writes=["bis"])
                            add("dve", lambda e, k=k: e.scalar_tensor_tensor(out=mid, in0=tmp, scalar=wtab[:, k:k + 1], in1=mid, op0=ALU.mult, op1=ALU.add),
                                reads=["bis", "wtab"], writes=["bis"])
                        add("dve", lambda e: e.tensor_tensor(out=thr, in0=mid, in1=wtab[:, KBIS:KBIS + 1], op=ALU.subtract), reads=["bis", "wtab"], writes=["bis"])
                    add("dve", lambda e: e.tensor_scalar(out=NM[:, 0:L], in0=sc[:, 0:L], scalar1=thr, scalar2=-30000.0, op0=ALU.is_lt, op1=ALU.mult),
                        reads=[scn, "bis"], writes=["NM"])
                    if g == 0 and t == 3:
                        dump("score3", sc[:, 0:512], [128, 512], F32, [scn])
                        dump("thr3", bis[:], [128, 8], F32, ["bis"])
                        dump("NM3", NM[:, 0:512], [128, 512], BF16, ["NM"])

                def attentionA(t, g=g):
                    i = 4 * g + t
                    for j0 in range(0, i + 1, 8):
                        nb = min(8, i + 1 - j0)
                        b = 6 + (j0 // 8) % 2
                        psb = PS[b][:].bitcast(BF16)

                        def f_nmt(e, psb=psb, j0=j0, nb=nb):
                            last = None
                            for jj in range(nb):
                                last = e.transpose(out=psb[:, jj * 128:(jj + 1) * 128], in_=NM[:, (j0 + jj) * 128:(j0 + jj + 1) * 128], identity=identB)
                            return last
                        add("pe", f_nmt, reads=["NM", "cb"], writes=[psn(b)])
                        evac(NMT[:, j0:j0 + nb, :].rearrange("p a b -> p (a b)"), psb[:, 0:nb * 128], [psn(b)], ["NMT"], eng="dve")

                def attentionB(t, g=g):
                    i = 4 * g + t
                    units = [(j, hh) for j in range(i + 1) for hh in range(2)]

                    LB = (0, 7)

                    def qk(n):
                        j, hh = units[n]
                        d = i - j
                        bl = LB[n % 2]
                        o3 = PS[bl][:].rearrange("p (a b) -> p a b", a=4)

                        def f(e):
                            e.matmul(o3, lhsT=ckvT[:, j * 128:(j + 1) * 128], rhs=qlatT[:, 4 * hh:4 * hh + 4, t * 128:(t + 1) * 128], start=True, stop=False)
                            e.matmul(o3, lhsT=alL[:, d * 128:(d + 1) * 128], rhs=alR[:].rearrange("p (a b) -> p a b", a=8)[:, 4 * hh:4 * hh + 4, :], start=False, stop=False)
                            return e.matmul(o3, lhsT=identB, rhs=NMT[:, j, :].unsqueeze(1).broadcast_to([128, 4, 128]), start=False, stop=True)
                        add("pe", f, reads=["ckvT%d" % (j // 4), "qlatT", "alL", "alR", "cb", "NMT"], writes=[psn(bl)])
                        add("act", lambda e, n=n, bl=bl: e.activation(out=PT[n % 2][:], in_=PS[bl][:], func=AF.Exp), reads=[psn(bl)], writes=["PT%d" % (n % 2)])

                    def pv(n):
                        j, hh = units[n]

                        def f(e):
                            e.matmul(PS[2 + hh][:], lhsT=ckv_tok[:, j, :], rhs=PT[n % 2][:], start=(j == 0), stop=(j == i))
                            return e.matmul(PS[4 + hh][:], lhsT=onesB, rhs=PT[n % 2][:], start=(j == 0), stop=(j == i))
                        add("pe", f, reads=["ckv_tok%d" % j, "PT%d" % (n % 2), "cb"], writes=[psn(2 + hh), psn(4 + hh)], pe_chain=True)
                    for n in range(len(units)):
                        qk(n)
                        if n > 0:
                            pv(n - 1)
                    pv(len(units) - 1)

                def attentionC(t, g=g):
                    i = 4 * g + t
                    for hh in range(2):
                        add("dve", lambda e, hh=hh: e.tensor_scalar(out=rden[:], in0=PS[4 + hh][:], scalar1=1.0e-30, scalar2=None, op0=ALU.max), reads=[psn(4 + hh)], writes=["rden"])
                        add("dve", lambda e: e.reciprocal(out=rden[:], in_=rden[:]), reads=["rden"], writes=["rden"])
                        add("dve", lambda e, hh=hh: e.tensor_tensor(out=olatn[:, 4 * hh:4 * hh + 4, :].rearrange("p a b -> p (a b)"), in0=PS[2 + hh][:], in1=rden[:], op=ALU.mult),
                            reads=[psn(2 + hh), "rden"], writes=["olatn"])

                def attentionD(t, g=g):
                    i = 4 * g + t
                    b = 7

                    def f_y(e):
                        last = None
                        for h in range(8):
                            pr, hb = h // 2, (h % 2) * 64
                            last = e.matmul(PS[b][hb:hb + 64, pr * 128:(pr + 1) * 128], lhsT=wuv[:, h * 64:(h + 1) * 64], rhs=olatn[:, h, :], start=True, stop=True)
                        return last
                    add("pe", f_y, reads=["wuv", "olatn"], writes=[psn(b)])
                    evac(yattnT[:, :, t * 128:(t + 1) * 128], PS[b][:].rearrange("p (a b) -> p a b", a=4), [psn(b)], ["yattnT"], eng="act")
                    if g == 0 and t == 3:
                        dump("olatn3", olatn[:], [128, 8, 128], BF16, ["olatn"])

                nchs = {}
                import os as _os
                if _os.environ.get("IDX1"):
                    indexer(int(_os.environ["IDX1"]))
                    S.drain_dmas("sp"); S.emit_phase(); return nc, dbg_out
                build_diag(0)
                build_diag(1)
                nchs[0] = indexer(0)
                threshold(0, nchs[0])
                stage_B4sg()
                if g == 0:
                    for j in range(4, 12):
                        ada_chunk(j)
                nchs[1] = indexer(1)
                for t in range(4):
                    attentionA(t)
                    if t + 2 < 4:
                        build_diag(t + 2)
                    if t + 1 < 4:
                        threshold(t + 1, nchs[t + 1])
                    attentionB(t)
                    attentionC(t)
                    if t + 2 < 4:
                        nchs[t + 2] = indexer(t + 2)
                    attentionD(t)
                if g == 0:
                    dump("yattnT", yattnT[:], [128, 4, 512], BF16, ["yattnT"])

                if stop_after == "B5":
                    S.drain_dmas("sp")
                    S.emit_phase()
                    return nc, dbg_out

                wresA, wvA = wget(cb8 + 0)
                wresP, wvP = wget(cb8 + 1)
                for n in range(8):
                    bA = rot()
                    mm8(bA, PS[bA][:], lambda k, n=n, wvA=wvA: wvA[:, k, n * 128:(n + 1) * 128], lambda k: yattnT[:, k, :], [wresA, "yattnT"], nk=4)
                    bP = rot()
                    mm8(bP, PS[bP][:], lambda k, n=n, wvP=wvP: wvP[:, k, n * 128:(n + 1) * 128], lambda k: ypoolT[:, k, :], [wresP, "ypoolT"], nk=4)
                    add("dve", lambda e, bA=bA, n=n: e.tensor_tensor(out=t1[:], in0=PS[bA][:], in1=sgT[:, n, :], op=ALU.mult), reads=[psn(bA), "sgT"], writes=["t1"])
                    add("dve", lambda e, bP=bP, n=n: e.tensor_tensor(out=t2[:], in0=PS[bP][:], in1=sgT[:, 8 + n, :], op=ALU.mult), reads=[psn(bP), "sgT"], writes=["t2"])
                    add("pool", lambda e, n=n: e.tensor_tensor(out=mixT[:, n, :], in0=t1[:], in1=t2[:], op=ALU.add), reads=["t1", "t2"], writes=["mixT"])
                if g == 0:
                    dump("mixT", mixT[:], [128, 8, 512], BF16, ["mixT"])
                wres0, wv0 = wget(cb8 + 2)
                wres1, wv1 = wget(cb8 + 3)
                def o_bufs(t, g=g):
                    i = 4 * g + t
                    if t % 2 == 0:
                        xp_, xpn = xp[:, :], ["xp0", "xp1"]
                        xr_, xrn = xr[:, :], "xr"
                        x1t_, x1tn = x1t[:, :], "x1t"
                        x1b_, x1bn = x1b[:, :], "x1b"
                        h2T_, h2Tn = h2T[:, :, :], "h2T"
                    else:
                        xp_, xpn = score[0][:, 0:1024], ["score0a", "score0a"]
                        x1t_, x1tn = score[0][:, 1024:2048], "score0b"
                        xr_, xrn = score[1][:, 0:1024], "score1a"
                        h2T_, h2Tn = score[1][:, 1024:2048].rearrange("p (a b) -> p a b", a=8), "score1b"
                        x1b_, x1bn = NM[:, 0:1024], "NM"
                    return i, xp_, xpn, xr_, xrn, x1t_, x1tn, x1b_, x1bn, h2T_, h2Tn

                def O_A(t):
                    i, xp_, xpn, xr_, xrn, x1t_, x1tn, x1b_, x1bn, h2T_, h2Tn = o_bufs(t)
                    add("sp", lambda e, i=i, xr_=xr_: e.dma_start(out=xr_, in_=x_d[i * 128:(i + 1) * 128, :]), writes=[xrn], dma_key="xrld%d" % (t % 2))
                    for hf, (wres_, wv_) in enumerate(((wres0, wv0), (wres1, wv1))):
                        b = rot()
                        mm8(b, PS[b][:], lambda k, t=t: mixT[:, k, t * 128:(t + 1) * 128], lambda k, wv_=wv_: wv_[:, k, :], [wres_, "mixT"])
                        add("dve", lambda e, b=b, hf=hf, xp_=xp_: e.tensor_tensor(out=xp_[:, hf * 512:(hf + 1) * 512], in0=PS[b][:], in1=gate_bc[:, 0, hf * 512:(hf + 1) * 512], op=ALU.mult),
                            reads=[psn(b)] + GATE1, writes=[xpn[hf]])
                    add("dve", lambda e, xp_=xp_, xr_=xr_: e.scalar_tensor_tensor(out=xp_, in0=xr_, scalar=ALPHA, in1=xp_, op0=ALU.mult, op1=ALU.add),
                        reads=[xrn] + xpn, writes=xpn)
                    for hf in range(2):
                        add("dve", lambda e, hf=hf, xp_=xp_: e.bn_stats(out=lnst[:, hf, :], in_=xp_[:, hf * 512:(hf + 1) * 512]), reads=xpn, writes=["lnst"])
                    add("dve", lambda e: e.bn_aggr(out=lnmv[:, 0:2], in_=lnst[:].rearrange("p a b -> p (a b)")), reads=["lnst"], writes=["lnmv"])
                    add("dve", lambda e: e.tensor_scalar(out=lnmv[:, 2:3], in0=lnmv[:, 1:2], scalar1=EPS, scalar2=None, op0=ALU.add), reads=["lnmv"], writes=["lnmv"])
                    add("act", lambda e: e.activation(out=lnmv[:, 2:3], in_=lnmv[:, 2:3], func=AF.Sqrt), reads=["lnmv"], writes=["lnmv"])
                    add("dve", lambda e: e.reciprocal(out=lnmv[:, 2:3], in_=lnmv[:, 2:3]), reads=["lnmv"], writes=["lnmv"])
                    add("dve", lambda e: e.scalar_tensor_tensor(out=lnmv[:, 3:4], in0=lnmv[:, 0:1], scalar=-1.0, in1=lnmv[:, 2:3], op0=ALU.mult, op1=ALU.mult), reads=["lnmv"], writes=["lnmv"])
                    add("act", lambda e, xp_=xp_, x1t_=x1t_: e.activation(out=x1t_, in_=xp_, func=AF.Identity, scale=lnmv[:, 2:3], bias=lnmv[:, 3:4]),
                        reads=xpn + ["lnmv"], writes=[x1tn])
                    add("pool", lambda e, x1t_=x1t_: e.tensor_tensor(out=x1t_, in0=x1t_, in1=ln1g_bc[:], op=ALU.mult), reads=[x1tn, "ln1g_bc"], writes=[x1tn])
                    add("pool", lambda e, x1t_=x1t_: e.tensor_tensor(out=x1t_, in0=x1t_, in1=ln1b_bc[:], op=ALU.add), reads=[x1tn, "ln1b_bc"], writes=[x1tn])

                def O_R(t):
                    i, xp_, xpn, xr_, xrn, x1t_, x1tn, x1b_, x1bn, h2T_, h2Tn = o_bufs(t)
                    add("sp", lambda e, i=i, x1t_=x1t_: e.dma_start(out=x1_d[i * 128:(i + 1) * 128, :], in_=x1t_), reads=[x1tn], writes=["x1d%d" % i], dma_key="x1d")
                    add("act", lambda e, x1t_=x1t_, x1b_=x1b_: e.activation(out=x1b_, in_=x1t_, func=AF.Identity), reads=[x1tn], writes=[x1bn])
                    for hf in range(2):
                        b = rot()

                        def f_x1t(e, b=b, hf=hf, x1t_=x1t_):
                            last = None
                            for c4 in range(4):
                                c = 4 * hf + c4
                                last = e.transpose(out=PS[b][:, c4 * 128:(c4 + 1) * 128], in_=x1t_[:, c * 128:(c + 1) * 128], identity=identF)
                            return last
                        add("pe", f_x1t, reads=[x1tn, "cst"], writes=[psn(b)])
                        for c4 in range(4):
                            c = 4 * hf + c4
                            add("act", lambda e, b=b, c=c, c4=c4, h2T_=h2T_: e.activation(out=h2T_[:, c, :], in_=PS[b][:, c4 * 128:(c4 + 1) * 128], func=AF.Identity,
                                                                                scale=modcol[:, 2, c:c + 1], bias=modcol[:, 3, c:c + 1]),
                                reads=[psn(b)] + MODC, writes=[h2Tn])
                    b = rot()
                    mm8(b, PS[b][:, 0:NE], lambda k, h2T_=h2T_: h2T_[:, k, :], lambda k: wrt[:, k, :], [h2Tn, "wrt"])
                    add("dve", lambda e, b=b: e.tensor_tensor(out=lg[:], in0=PS[b][:, 0:NE], in1=br_bc[:], op=ALU.add), reads=[psn(b), "br_bc"], writes=["lg"])
                    add("dve", lambda e: e.max(out=top8[:], in_=lg[:]), reads=["lg"], writes=["top8"])
                    add("dve", lambda e, i=i: e.tensor_scalar(out=Mall[:, i, :], in0=lg[:], scalar1=top8[:, 3:4], scalar2=None, op0=ALU.is_ge), reads=["lg", "top8"], writes=["Mall%d" % i])
                    add("dve", lambda e: e.tensor_scalar(out=rsm[:, 0:1], in0=top8[:, 0:1], scalar1=-1.0, scalar2=None, op0=ALU.mult), reads=["top8"], writes=["rsm"])
                    add("act", lambda e: e.activation(out=e4[:], in_=top8[:, 0:4], func=AF.Exp, bias=rsm[:, 0:1], scale=1.0, accum_out=rsm[:, 1:2]), reads=["top8", "rsm"], writes=["e4", "rsm"])
                    add("dve", lambda e: e.reciprocal(out=rsm[:, 2:3], in_=rsm[:, 1:2]), reads=["rsm"], writes=["rsm"])
                    add("dve", lambda e, i=i: e.tensor_scalar(out=gates[:, i, :], in0=e4[:], scalar1=rsm[:, 2:3], scalar2=None, op0=ALU.mult), reads=["e4", "rsm"], writes=["gates%d" % i])
                    b = rot()

                    def f_rank(e, b=b, i=i):
                        last = None
                        for j in range(i):
                            last = e.matmul(PS[b][:, 0:NE], lhsT=onesB, rhs=Mall[:, j, :], start=(j == 0), stop=False)
                        return e.matmul(PS[b][:, 0:NE], lhsT=triB, rhs=Mall[:, i, :], start=(i == 0), stop=True)
                    add("pe", f_rank, reads=["Mall%d" % j for j in range(i + 1)] + ["cb"], writes=[psn(b)])
                    add("dve", lambda e, b=b: e.tensor_scalar(out=destf[:], in0=PS[b][:, 0:NE], scalar1=float(CAP - 1), scalar2=None, op0=ALU.min), reads=[psn(b)], writes=["destf"])
                    add("dve", lambda e: e.tensor_tensor(out=destf[:], in0=destf[:], in1=cst[:, C_EB:C_EB + NE], op=ALU.add), reads=["destf", "cst"], writes=["destf"])
                    for k in range(4):
                        add("dve", lambda e, k=k: e.scalar_tensor_tensor(out=junk32[:], in0=lg[:], scalar=top8[:, k:k + 1], in1=destf[:], op0=ALU.is_equal, op1=ALU.mult, accum_out=destk[:, k:k + 1]),
                            reads=["lg", "top8", "destf"], writes=["junk32", "destk"])
                    add("dve", lambda e, i=i: e.tensor_copy(out=desti[:, i, :], in_=destk[:]), reads=["destk"], writes=["desti%d" % i])
                    for k in range(4):
                        add("pool", lambda e, i=i, k=k, x1b_=x1b_: e.indirect_dma_start(out=xpad_d, out_offset=bass.IndirectOffsetOnAxis(ap=desti[:, i, k:k + 1], axis=0), in_=x1b_, in_offset=None),
                            reads=[x1bn, "desti%d" % i] + xpadZ, writes=["xpadS%d_%d" % (i, k)], dma_key="xpadS")
                    if i == 0:
                        dump("x1t0", x1t[:], [128, D], F32, ["x1t"])
                        dump("lg0", lg[:], [128, NE], F32, ["lg"])
                        dump("destk0", destk[:], [128, 4], F32, ["destk"])


                O_A(0)
                for t in range(4):
                    if t + 1 < 4:
                        O_A(t + 1)
                    O_R(t)

            b = rot()

            def f_cnt(e, b=b):
                last = None
                for j in range(NT):
                    last = e.matmul(PS[b][:, 0:NE], lhsT=onesB, rhs=Mall[:, j, :], start=(j == 0), stop=(j == NT - 1))
                return last
            add("pe", f_cnt, reads=["Mall%d" % j for j in range(NT)] + ["cb"], writes=[psn(b)])
            add("dve", lambda e, b=b: e.tensor_copy(out=cnt_f[:], in_=PS[b][0:1, 0:NE]), reads=[psn(b)], writes=["cnt_f"])
            add("dve", lambda e: e.tensor_copy(out=cnt_i[:], in_=cnt_f[:]), reads=["cnt_f"], writes=["cnt_i"])
            dump("cnt_f", cnt_f[:], [1, NE], F32, ["cnt_f"])
            dump("gates", gates[:], [128, NT, 4], F32, ["gates%d" % i for i in range(NT)])
            dump("desti", desti[:], [128, NT, 4], U32, ["desti%d" % i for i in range(NT)])
            S.drain_dmas("sp")
            S.emit_phase()
        XPADS = ["xpadS%d_%d" % (i, k) for i in range(NT) for k in range(4)]
        if stop_after == "B":
            return nc, dbg_out

        with ExitStack() as ph:
            NSL = 8
            CPE = 3
            ring = [T(ph, "er%d" % i, [128, 8, 1024], BF16) for i in range(NSL)]
            wgu_v = wgu_d.rearrange("e (c p) n -> e p c n", p=128)
            wd_v = wd_d.rearrange("e (c p) n -> e p c n", p=128)
            loads = []
            for ex in range(NE):
                for m in range(2):
                    loads.append((wgu_v[ex][:, 4 * m:4 * m + 4, :], 4, 2048))
                loads.append((wd_v[ex][:, :, :], 8, 1024))
            issued = [0]
            LOOKC = 8

            def eprefetch(upto):
                while issued[0] <= min(upto, len(loads) - 1):
                    k = issued[0]
                    add("pool", lambda e, k=k: e.dma_start(out=ring[k % NSL][:].rearrange("p a b -> p (a b)").rearrange("p (c n) -> p c n", c=loads[k][1]), in_=loads[k][0]),
                        writes=["er%d" % (k % NSL)], dma_key="er%d" % (k % NSL))
                    issued[0] += 1

            def eget(n):
                assert n < issued[0]
                return "er%d" % (n % NSL), ring[n % NSL][:].rearrange("p a b -> p (a b)").rearrange("p (c n) -> p c n", c=loads[n][1])

            NTG = SG // 128
            xe_tok = [T(ph, "xe_tok%d" % i, [128, NTG, D], BF16) for i in range(3)]
            XeT = [T(ph, "XeT%d" % i, [128, 8, SG], BF16) for i in range(3)]
            actT = [T(ph, "actT%d" % i, [128, 8, SG], BF16) for i in range(3)]
            ye_tok = [T(ph, "ye_tok%d" % i, [128, NTG, D], BF16) for i in range(2)]
            bgu = T(ph, "bgu", [128, NE * 16], F32)
            bd16 = [T(ph, "bd16_%d" % i, [1, D], BF16) for i in range(2)]
            g1 = [T(ph, "g1_%d" % i, [128, SG], F32) for i in range(2)]
            l2 = [T(ph, "l2_%d" % i, [128, SG], F32) for i in range(2)]
            pp = [T(ph, "pp_%d" % i, [128, SG], F32) for i in range(2)]
            add("sp", lambda e: e.dma_start(out=bgu[:], in_=bgu_d), writes=["bgu"], dma_key="bgu")
            bgu3 = bgu[:].rearrange("p (e f) -> p e f", e=NE)
            add("dve", lambda e: e.tensor_scalar(out=bgu3[:, :, 8:16], in0=bgu3[:, :, 8:16], scalar1=1.0, scalar2=None, op0=ALU.add), reads=["bgu"], writes=["bgu"])
            bdsc = [T(ph, "bdsc_%d" % i, [128, D], BF16) for i in range(2)]
            for i2 in range(2):
                add("pool", lambda e, i2=i2: e.memset(bdsc[i2][:], 0.0), writes=["bdsc_%d" % i2])
            INV = 1.0 / 1.702
            g2s = T(ph, "g2s", [128, D], F32)
            add("dve", lambda e: e.tensor_scalar(out=g2s[:], in0=gate_bc[:, 1, :], scalar1=INV, scalar2=None, op0=ALU.mult), reads=GATE2, writes=["g2s"])
            groups = [(ex, sgi) for ex in range(NE) for sgi in range(NG)]

            class _Cond:
                def __init__(self, ex, sgi):
                    self.on = sgi > 0
                    self.ex, self.sgi = ex, sgi

                def __enter__(self):
                    if self.on:
                        S.begin_cond(cnt_i[0:1, self.ex:self.ex + 1], self.sgi * SG)

                def __exit__(self, *a):
                    if self.on:
                        S.end_cond()

            def bufi(gi):
                ex, sgi = groups[gi]
                return ex % 2 if sgi == 0 else 2

            def stage_load(gi):
                ex, sgi = groups[gi]
                par = bufi(gi)
                r0 = ex * CAP + sgi * SG
                xt = xe_tok[par]
                add("sp", lambda e, r0=r0, xt=xt: e.dma_start(out=xt[:], in_=xpad_d[r0:r0 + SG, :].rearrange("(a p) n -> p a n", p=128)),
                    reads=XPADS + xpadZ, writes=["xe_tok%d" % par], dma_key="xe_tok%d" % par)

            def stage_T(gi):
                ex, sgi = groups[gi]
                par = bufi(gi)
                xt, xtn, XT, XTn = xe_tok[par], "xe_tok%d" % par, XeT[par], "XeT%d" % par
                if True:
                    for k2 in range(4):
                        b = rot()
                        psb = PS[b][:].bitcast(BF16)

                        def f_xet(e, psb=psb, k2=k2, xt=xt):
                            last = None
                            for kk in range(2):
                                k = 2 * k2 + kk
                                for st in range(NTG):
                                    last = e.transpose(out=psb[:, kk * SG + st * 128:kk * SG + (st + 1) * 128], in_=xt[:, st, k * 128:(k + 1) * 128], identity=identB)
                            return last
                        add("pe", f_xet, reads=[xtn, "cb"], writes=[psn(b)])
                        for kk in range(2):
                            k = 2 * k2 + kk
                            add("act", lambda e, psb=psb, kk=kk, k=k, XT=XT: e.activation(out=XT[:, k, :], in_=psb[:, kk * SG:(kk + 1) * SG], func=AF.Identity,
                                                                                       scale=modcol[:, 2, k:k + 1], bias=modcol[:, 3, k:k + 1]),
                                reads=[psn(b)] + MODC, writes=[XTn])

            def stage_G(gi):
                ex, sgi = groups[gi]
                par = bufi(gi)
                XT, XTn, AT, ATn = XeT[par], "XeT%d" % par, actT[par], "actT%d" % par
                if sgi == 0:
                    if ex == 0:
                        add("pool", lambda e: e.dma_start(out=bd16[0][:], in_=bd_d[0:1, :]), writes=["bd16_0"], dma_key="bd16_0")
                    if ex + 1 < NE:
                        add("pool", lambda e, ex=ex: e.dma_start(out=bd16[(ex + 1) % 2][:], in_=bd_d[ex + 1:ex + 2, :]), writes=["bd16_%d" % ((ex + 1) % 2)], dma_key="bd16_%d" % ((ex + 1) % 2))
                    eprefetch(CPE * (ex - 1) + (CPE - 1) + NSL)
                    add("dve", lambda e, ex=ex: e.tensor_scalar(out=bdsc[ex % 2][0:1, :], in0=bd16[ex % 2][:], scalar1=1.702, scalar2=None, op0=ALU.mult),
                        reads=["bd16_%d" % (ex % 2)], writes=["bdsc_%d" % (ex % 2)])
                if True:
                    for fc in range(8):
                        res0, w0 = eget(CPE * ex + 0)
                        res1, w1 = eget(CPE * ex + 1)
                        wk = lambda k, col, w0=w0, w1=w1: (w0 if k < 4 else w1)[:, k % 4, col:col + 128]
                        bG = rot()
                        mm8(bG, PS[bG][:, 0:SG], lambda k, wk=wk, fc=fc: wk(k, fc * 128), lambda k, XT=XT: XT[:, k, :], [res0, res1, XTn])
                        bL = rot()
                        mm8(bL, PS[bL][:, 0:SG], lambda k, wk=wk, fc=fc: wk(k, 1024 + fc * 128), lambda k, XT=XT: XT[:, k, :], [res0, res1, XTn])
                        p2 = fc % 2
                        add("dve", lambda e, bG=bG, ex=ex, fc=fc, p2=p2: e.tensor_scalar(out=g1[p2][:], in0=PS[bG][:, 0:SG], scalar1=bgu[:, ex * 16 + fc:ex * 16 + fc + 1], scalar2=7.0, op0=ALU.add, op1=ALU.min),
                            reads=[psn(bG), "bgu"], writes=["g1_%d" % p2])
                        add("act", lambda e, p2=p2: e.activation(out=pp[p2][:], in_=g1[p2][:], func=AF.Silu, scale=1.702), reads=["g1_%d" % p2], writes=["pp_%d" % p2])
                        add("dve", lambda e, bL=bL, ex=ex, fc=fc, p2=p2: e.tensor_scalar(out=l2[p2][:], in0=PS[bL][:, 0:SG], scalar1=bgu[:, ex * 16 + 8 + fc:ex * 16 + 8 + fc + 1], scalar2=-6.0, op0=ALU.add, op1=ALU.max),
                            reads=[psn(bL), "bgu"], writes=["l2_%d" % p2])
                        add("dve", lambda e, p2=p2, fc=fc, AT=AT: e.scalar_tensor_tensor(out=AT[:, fc, :], in0=l2[p2][:], scalar=8.0, in1=pp[p2][:], op0=ALU.min, op1=ALU.mult),
                            reads=["l2_%d" % p2, "pp_%d" % p2], writes=[ATn])

            def stage_D(gi):
                ex, sgi = groups[gi]
                par = bufi(gi)
                AT, ATn = actT[par], "actT%d" % par
                ypar = 0 if sgi == 0 else 1
                yt, ytn = ye_tok[ypar], "ye_tok%d" % ypar
                r0 = ex * CAP + sgi * SG
                if True:
                    for hf in range(2):
                        resD, wD = eget(CPE * ex + 2)
                        for st in range(NTG):
                            b = rot()

                            def f_dn(e, b=b, wD=wD, st=st, ex=ex, hf=hf, AT=AT):
                                for k in range(8):
                                    e.matmul(PS[b][:], lhsT=AT[:, k, st * 128:(st + 1) * 128], rhs=wD[:, k, hf * 512:(hf + 1) * 512], start=(k == 0), stop=False)
                                return e.matmul(PS[b][:], lhsT=ones0B, rhs=bdsc[ex % 2][:, hf * 512:(hf + 1) * 512], start=False, stop=True)
                            add("pe", f_dn, reads=[resD, ATn, "cb", "bdsc_%d" % (ex % 2)], writes=[psn(b)])
                            add("dve", lambda e, b=b, st=st, hf=hf, yt=yt: e.tensor_tensor(out=yt[:, st, hf * 512:(hf + 1) * 512], in0=PS[b][:], in1=g2s[:, hf * 512:(hf + 1) * 512], op=ALU.mult),
                                reads=[psn(b), "g2s"], writes=[ytn])
                    add("sp", lambda e, r0=r0, yt=yt: e.dma_start(out=ypad_d[r0:r0 + SG, :].rearrange("(a p) n -> p a n", p=128), in_=yt[:]),
                        reads=[ytn], writes=["ypadS%d" % gi], dma_key="ypadS")

            g0 = lambda ex: ex * NG
            stage_load(g0(0))
            stage_load(g0(1))
            stage_T(g0(0))
            for ex in range(NE):
                stage_G(g0(ex))
                if ex + 1 < NE:
                    stage_T(g0(ex + 1))
                if ex + 2 < NE:
                    stage_load(g0(ex + 2))
                stage_load(g0(ex) + 1)
                stage_T(g0(ex) + 1)
                stage_D(g0(ex))
                for sgi in range(1, NG):
                    gi = g0(ex) + sgi
                    S.begin_cond(cnt_i[0:1, ex:ex + 1], sgi * SG)
                    if sgi > 1:
                        stage_load(gi)
                        stage_T(gi)
                    stage_G(gi)
                    stage_D(gi)
                for sgi in range(1, NG):
                    S.end_cond()
            S.drain_dmas("sp")
            S.emit_phase()
        YPADS = ["ypadS%d" % gi for gi in range(NE * NG)]

        with ExitStack() as ph:
            Yk = [[T(ph, "Yk%d_%d" % (s2, k), [128, D], BF16) for k in range(4)] for s2 in range(3)]
            x1r = [T(ph, "x1r%d" % i, [128, D], F32) for i in range(3)]
            acc = [T(ph, "acc%d" % i, [128, D], F32) for i in range(2)]
            ot = [T(ph, "ot%d" % i, [128, D], F32) for i in range(2)]
            ln2g_bc = T(ph, "ln2g_bc", [128, D], F32)
            ln2b_bc = T(ph, "ln2b_bc", [128, D], F32)
            add("sp", lambda e: e.dma_start(out=ln2g_bc[:], in_=ln2g_d.broadcast_to([128, D])), writes=["ln2g_bc"], dma_key="ln2g_bc")
            add("sp", lambda e: e.dma_start(out=ln2b_bc[:], in_=ln2b_d.broadcast_to([128, D])), writes=["ln2b_bc"], dma_key="ln2b_bc")
            dgk = [T(ph, "dgk%d" % i, [128, 4, 128], BF16) for i in range(2)]
            def d_fetch(i):
                s2 = i % 3
                for k in range(4):
                    add("pool", lambda e, i=i, k=k, s2=s2: e.indirect_dma_start(out=Yk[s2][k][:], out_offset=None, in_=ypad_d, in_offset=bass.IndirectOffsetOnAxis(ap=desti[:, i, k:k + 1], axis=0)),
                        reads=YPADS + ["desti%d" % i], writes=["Yk%d_%d" % (s2, k)], dma_key="Yk%d_%d" % (s2, k))
                add("sp", lambda e, i=i, s2=s2: e.dma_start(out=x1r[s2][:], in_=x1_d[i * 128:(i + 1) * 128, :]), reads=["x1d%d" % i], writes=["x1r%d" % s2], dma_key="x1r%d" % s2)

            lnst2 = [T(ph, "lnst2_%d" % i, [128, 2, 6], F32) for i in range(2)]
            lnmv2 = [T(ph, "lnmv2_%d" % i, [128, 4], F32) for i in range(2)]

            def D_A(i):
                s2, p2 = i % 3, i % 2
                a_, an = acc[p2], "acc%d" % p2
                st_, stn, mv_, mvn = lnst2[p2], "lnst2_%d" % p2, lnmv2[p2], "lnmv2_%d" % p2
                for k in range(4):
                    add("dve", lambda e, k=k: e.tensor_scalar(out=dgk[p2][:, k, :], in0=identB, scalar1=gates[:, i, k:k + 1], scalar2=None, op0=ALU.mult),
                        reads=["cb", "gates%d" % i], writes=["dgk%d" % p2])
                for hf in range(2):
                    b = rot()

                    def f_cmb(e, b=b, hf=hf):
                        last = None
                        for k in range(4):
                            last = e.matmul(PS[b][:], lhsT=dgk[p2][:, k, :], rhs=Yk[s2][k][:, hf * 512:(hf + 1) * 512], start=(k == 0), stop=(k == 3))
                        return last
                    add("pe", f_cmb, reads=["dgk%d" % p2] + ["Yk%d_%d" % (s2, k) for k in range(4)], writes=[psn(b)])
                    add("dve", lambda e, b=b, hf=hf: e.scalar_tensor_tensor(out=a_[:, hf * 512:(hf + 1) * 512], in0=x1r[s2][:, hf * 512:(hf + 1) * 512], scalar=ALPHA,
                                                                         in1=PS[b][:], op0=ALU.mult, op1=ALU.add),
                        reads=["x1r%d" % s2, psn(b)], writes=[an])
                for hf in range(2):
                    add("dve", lambda e, hf=hf: e.bn_stats(out=st_[:, hf, :], in_=a_[:, hf * 512:(hf + 1) * 512]), reads=[an], writes=[stn])
                add("dve", lambda e: e.bn_aggr(out=mv_[:, 0:2], in_=st_[:].rearrange("p a b -> p (a b)")), reads=[stn], writes=[mvn])

            def D_B(i):
                p2 = i % 2
                a_, an = acc[p2], "acc%d" % p2
                mv_, mvn = lnmv2[p2], "lnmv2_%d" % p2
                o_, on = ot[p2], "ot%d" % p2
                add("dve", lambda e: e.tensor_scalar(out=mv_[:, 2:3], in0=mv_[:, 1:2], scalar1=EPS, scalar2=None, op0=ALU.add), reads=[mvn], writes=[mvn])
                add("act", lambda e: e.activation(out=mv_[:, 2:3], in_=mv_[:, 2:3], func=AF.Sqrt), reads=[mvn], writes=[mvn])
                add("dve", lambda e: e.reciprocal(out=mv_[:, 2:3], in_=mv_[:, 2:3]), reads=[mvn], writes=[mvn])
                add("dve", lambda e: e.scalar_tensor_tensor(out=mv_[:, 3:4], in0=mv_[:, 0:1], scalar=-1.0, in1=mv_[:, 2:3], op0=ALU.mult, op1=ALU.mult), reads=[mvn], writes=[mvn])
                add("act", lambda e: e.activation(out=o_[:], in_=a_[:], func=AF.Identity, scale=mv_[:, 2:3], bias=mv_[:, 3:4]), reads=[an, mvn], writes=[on])
                add("dve", lambda e: e.tensor_tensor(out=o_[:], in0=o_[:], in1=ln2g_bc[:], op=ALU.mult), reads=[on, "ln2g_bc"], writes=[on])
                add("dve", lambda e: e.tensor_tensor(out=o_[:], in0=o_[:], in1=ln2b_bc[:], op=ALU.add), reads=[on, "ln2b_bc"], writes=[on])
                add("sp", lambda e: e.dma_start(out=out_d[i * 128:(i + 1) * 128, :], in_=o_[:]), reads=[on], writes=["outd%d" % i], dma_key="outd")

            d_fetch(0)
            d_fetch(1)
            D_A(0)
            for i in range(NT):
                if i + 2 < NT:
                    d_fetch(i + 2)
                if i + 1 < NT:
                    D_A(i + 1)
                D_B(i)
            S.drain_dmas("sp")
            S.emit_phase()
    return nc, dbg_out


def make_in_maps(inputs, cores=range(8)):
    f = lambda a: np.ascontiguousarray(np.asarray(a, dtype=np.float32))
    consts, constsb, alL, alR = make_consts()
    shared = {
        "w_ada": f(inputs["w_ada"][0]),
        "b_ada": f(inputs["b_ada"][0]).reshape(1, -1),
        "w_in": f(inputs["w_in"][0]),
        "kv_norm_g": f(inputs["kv_norm_g"][0]).reshape(1, -1),
        "w_uk": f(inputs["w_uk"][0]).reshape(128, 512),
        "w_uv": f(inputs["w_uv"][0]).reshape(128, 512),
        "w_pool": f(inputs["w_pool_group"][0]),
        "pool_scale_col": f(np.asarray(inputs["pool_scale"][0]).reshape(4, 128).T),
        "w_ba": f(inputs["w_branch_attn"][0]),
        "w_bp": f(inputs["w_branch_pool"][0]),
        "w_out": f(inputs["w_out"][0]),
        "ln1_g": f(inputs["ln1_g"][0]).reshape(1, -1),
        "ln1_b": f(inputs["ln1_b"][0]).reshape(1, -1),
        "w_router": f(inputs["w_router"][0]),
        "b_router": f(inputs["b_router"][0]).reshape(1, -1),
        "w_gate_up": f(inputs["w_gate_up"][0]),
        "b_gu_col": f(np.asarray(inputs["b_gate_up"][0]).reshape(NE, 16, 128).transpose(2, 0, 1).reshape(128, NE * 16)),
        "w_down": f(inputs["w_down"][0]),
        "b_down": f(inputs["b_down"][0]),
        "ln2_g": f(inputs["ln2_g"][0]).reshape(1, -1),
        "ln2_b": f(inputs["ln2_b"][0]).reshape(1, -1),
        "consts": consts, "constsb": constsb, "alibiL": alL, "alibiR": alR,
    }
    maps = []
    for b in cores:
        m = dict(shared)
        m["x"] = f(inputs["x"][b])
        m["c_col"] = f(np.asarray(inputs["c"][b]).reshape(8, 128).T)
        maps.append(m)
    return maps


def kernel(**inputs):
    nc, _ = build_nc()
    maps = make_in_maps(inputs)
    res = run_bass_kernel_spmd(nc, maps, core_ids=list(range(8)))
    out = np.stack([np.asarray(r["out"], dtype=np.float32) for r in res.results], axis=0)
    return out
```

```python
import numpy as np
from contextlib import ExitStack
import concourse.bass as bass
import concourse.mybir as mybir
from concourse.bass_utils import run_bass_kernel_spmd

F32 = mybir.dt.float32
BF16 = mybir.dt.bfloat16
U32 = mybir.dt.uint32
AF = mybir.ActivationFunctionType
ALU = mybir.AluOpType
AX = mybir.AxisListType

S_LEN = 2048
D = 1024
NT = 16
CAP = 1024
SG = 256
NG = CAP // SG
NE = 32
KBIS = 12
ALPHA = 2.0 ** 0.25
EPS = 1e-5
NEG = -1.0e30
XPAD_ROWS = NE * CAP + 2048

C_ID, C_CM, C_POW, C_EB = 0, 128, 256, 272
NCONST = 304
B_ID, B_TRI, B_ONE, B_BAND = 0, 128, 256, 384
B_ONE0 = 384 + 12 * 128
NB16 = B_ONE0 + 128

ENGS = ("pe", "act", "dve", "pool", "sp")


class Sched:
    def __init__(self, nc, stack, epoch=4000):
        self.nc = nc
        self.stack = stack
        self.epoch = epoch
        self.ops = {e: [] for e in ENGS}
        self.cnt = {e: 0 for e in ENGS}
        self.last_w = {}
        self.readers = {}
        self.dma_cnt = {}
        self.seen = {e: {} for e in ENGS}
        self.sems = {}
        self.nblock = 0
        self.alias = {}
        self.cond = ()
        self.cond_seen = []

    def _sem(self, sk):
        if sk not in self.sems:
            self.sems[sk] = self.stack.enter_context(self.nc.semaphore("sm%d" % len(self.sems)))
        return self.sems[sk]

    def _tok2sem(self, tok):
        if tok[0] == "eng":
            _, e, idx = tok
            ep = idx // self.epoch
            return ("eng", e, ep), idx - ep * self.epoch + 1
        _, key, n = tok
        return ("dma", key), 16 * n

    def add(self, eng, fn, reads=(), writes=(), dma_key=None, pe_chain=False, ndma=1):
        reads = tuple(x for r in reads for x in self.alias.get(r, (r,)))
        writes = tuple(x for w in writes for x in self.alias.get(w, (w,)))
        if eng == "pe":
            pe_chain = True
        deps = []
        for r in reads:
            t = self.last_w.get(r)
            if t is not None:
                deps.append(t)
        for w in writes:
            t = self.last_w.get(w)
            if t is not None:
                deps.append(t)
            deps.extend(self.readers.get(w, ()))
        waits = {}
        seen = self.seen[eng] if not self.cond else self.cond_seen[-1][eng]
        for tok in deps:
            if tok[0] == "eng" and tok[1] == eng and pe_chain:
                continue
            sk, v = self._tok2sem(tok)
            if seen.get(sk, 0) >= v:
                continue
            waits[sk] = max(waits.get(sk, 0), v)
        for sk, v in waits.items():
            seen[sk] = v
        if dma_key is not None:
            n = self.dma_cnt.get(dma_key, 0) + ndma
            self.dma_cnt[dma_key] = n
            tok = ("dma", dma_key, n)
            inc = (("dma", dma_key), 16)
        else:
            idx = self.cnt[eng]
            self.cnt[eng] = idx + 1
            tok = ("eng", eng, idx)
            inc = (("eng", eng, idx // self.epoch), 1)
        self.ops[eng].append(dict(fn=fn, waits=list(waits.items()), inc=inc, cond=self.cond,
                                  nd=(ndma if dma_key is not None else 1)))
        for r in reads:
            self.readers.setdefault(r, []).append(tok)
        for w in writes:
            self.last_w[w] = tok
            self.readers[w] = []
        return tok

    def begin_cond(self, cnt_ap, thresh):
        base = self.cond_seen[-1] if self.cond else self.seen
        self.cond = self.cond + ((cnt_ap, thresh),)
        self.cond_seen.append({e: dict(base[e]) for e in ENGS})

    def end_cond(self):
        self.cond = self.cond[:-1]
        self.cond_seen.pop()

    def drain_dmas(self, eng="sp"):
        waits = []
        for key, n in self.dma_cnt.items():
            sk = ("dma", key)
            if self.seen[eng].get(sk, 0) >= 16 * n:
                continue
            self.seen[eng][sk] = 16 * n
            waits.append((sk, 16 * n))
        self.ops[eng].append(dict(fn=None, waits=waits, inc=None, cond=(), nd=1))

    def emit_phase(self):
        nc = self.nc
        for e in ENGS:
            for op in self.ops[e]:
                for sk, _ in op["waits"]:
                    self._sem(sk)
                if op["inc"] is not None:
                    self._sem(op["inc"][0])
        sems = self.sems
        all_ops = self.ops
        self.ops = {e: [] for e in ENGS}
        self.nblock += 1
        with nc.Block() as block:
            engmap = {"pe": block.tensor, "act": block.scalar, "dve": block.vector,
                      "pool": block.gpsimd, "sp": block.sync}

            def run_op(e, op):
                for sk, v in op["waits"]:
                    e.wait_ge(sems[sk], v)
                if op["fn"] is not None:
                    inst = op["fn"](e)
                    sk, n = op["inc"]
                    if isinstance(inst, (list, tuple)):
                        for ii in inst:
                            ii.then_inc(sems[sk], n)
                    else:
                        inst.then_inc(sems[sk], n)

            def emit_ops(e, ops, depth):
                i = 0
                while i < len(ops):
                    op = ops[i]
                    if len(op["cond"]) <= depth:
                        run_op(e, op)
                        i += 1
                        continue
                    tag = op["cond"][depth]
                    j = i
                    while j < len(ops) and len(ops[j]["cond"]) > depth and ops[j]["cond"][depth] is tag:
                        j += 1
                    grp = ops[i:j]
                    cnt_ap, thresh = tag
                    self._nreg = getattr(self, "_nreg", 0) + 1
                    creg = e.alloc_register("condreg%d" % self._nreg)
                    e.reg_load(creg, cnt_ap)
                    with e.If_cmp(creg, thresh, comp_op="IS_GT"):
                        emit_ops(e, grp, depth + 1)
                    with e.Else():
                        incs = {}
                        for o2 in grp:
                            if o2["inc"] is None:
                                continue
                            sk, n = o2["inc"]
                            incs[sk] = incs.get(sk, 0) + n * o2["nd"]
                        for sk, tot in incs.items():
                            if sk[0] == "dma":
                                e.sem_inc(sems[sk], tot)
                            else:
                                e.drain().then_inc(sems[sk], tot)
                    e.free_register(creg)
                    i = j

            def mk(ops):
                def body(e):
                    emit_ops(e, ops, 0)
                return body

            for ename in ENGS:
                if all_ops[ename]:
                    engmap[ename](mk(all_ops[ename]))


def make_consts():
    c = np.zeros((128, NCONST), np.float32)
    cbs = np.zeros((128, NB16), np.float32)
    ar = np.arange(128)
    c[:, C_ID:C_ID + 128] = np.eye(128)
    cbs[:, B_ID:B_ID + 128] = np.eye(128)
    cbs[:, B_TRI:B_TRI + 128] = (ar[:, None] < ar[None, :])
    cbs[:, B_ONE:B_ONE + 128] = 1.0
    cbs[0, B_ONE0:B_ONE0 + 128] = 1.0
    c[:, C_CM:C_CM + 128] = np.where(ar[None, :] <= ar[:, None], 0.0, NEG)
    for wg, w in enumerate((2, 4, 8, 16)):
        tp = ar[:, None]
        t = ar[None, :]
        dd = t - tp
        main = np.where((dd >= 0) & (dd < w), 1.0 / w, 0.0) - (dd == 0)
        prev = np.where((t + 128 - tp) < w, 1.0 / w, 0.0)
        cntf = np.minimum(w, t + 1).astype(np.float64)
        first = np.where((dd >= 0) & (dd < w), 1.0 / cntf, 0.0) - (dd == 0)
        for v, m in enumerate((main, prev, first)):
            o = B_BAND + 128 * (3 * wg + v)
            cbs[:, o:o + 128] = m
    c[:, C_POW:C_POW + 16] = 2.0 ** (-np.arange(16))[None, :]
    c[:, C_EB:C_EB + 32] = (np.arange(32) * CAP)[None, :]
    slopes = 2.0 ** (-8.0 * np.arange(1, 9) / 8)
    al = np.zeros((3, 16, 128), np.float32)
    al[0] = ar[None, :]
    al[1] = (-128.0 * np.arange(16))[:, None]
    al[2] = 1.0
    arr = np.zeros((3, 8, 128), np.float32)
    arr[0] = slopes[:, None]
    arr[1] = slopes[:, None]
    arr[2] = -slopes[:, None] * ar[None, :]
    return c, cbs, al.reshape(3, 2048), arr.reshape(3, 1024)


def build_nc(dbg=(), stop_after=None):
    nc = bass.Bass("TRN2", target_bir_lowering=False)
    dt = lambda name, shape, dty=F32: nc.dram_tensor(name, list(shape), dty, kind="ExternalInput").ap()
    x_d = dt("x", [S_LEN, D])
    ccol_d = dt("c_col", [128, 8])
    wada_d = dt("w_ada", [D, 6 * D])
    bada_d = dt("b_ada", [1, 6 * D])
    win_d = dt("w_in", [D, 3784])
    kvg_d = dt("kv_norm_g", [1, 128])
    wuk_d = dt("w_uk", [128, 512])
    wuv_d = dt("w_uv", [128, 512])
    wpool_d = dt("w_pool", [4, 128, 128])
    pscale_d = dt("pool_scale_col", [128, 4])
    wba_d = dt("w_ba", [512, D])
    wbp_d = dt("w_bp", [512, D])
    wout_d = dt("w_out", [D, D])
    ln1g_d = dt("ln1_g", [1, D])
    ln1b_d = dt("ln1_b", [1, D])
    wr_d = dt("w_router", [D, NE])
    br_d = dt("b_router", [1, NE])
    wgu_d = dt("w_gate_up", [NE, D, 2 * D])
    bgu_d = dt("b_gu_col", [128, NE * 16])
    wd_d = dt("w_down", [NE, D, D])
    bd_d = dt("b_down", [NE, D])
    ln2g_d = dt("ln2_g", [1, D])
    ln2b_d = dt("ln2_b", [1, D])
    consts_d = dt("consts", [128, NCONST])
    constsb_d = dt("constsb", [128, NB16])
    alL_d = dt("alibiL", [3, 2048])
    alR_d = dt("alibiR", [3, 1024])
    out_d = nc.dram_tensor("out", [S_LEN, D], F32, kind="ExternalOutput").ap()
    x1_d = nc.dram_tensor("x1_scr", [S_LEN, D], F32, kind="Internal").ap()
    xpad_d = nc.dram_tensor("xpad_scr", [XPAD_ROWS, D], BF16, kind="Internal").ap()
    ypad_d = nc.dram_tensor("ypad_scr", [XPAD_ROWS, D], BF16, kind="Internal").ap()
    dbg_out = {}

    with ExitStack() as top:
        S = Sched(nc, top)
        S.alias = {"score0": ("score0a", "score0b"), "score1": ("score1a", "score1b")}
        add = S.add
        PS = [top.enter_context(nc.psum_tensor("psb%d" % i, [128, 512], F32)) for i in range(8)]
        psn = lambda b: "ps%d" % b
        rot_state = [0]

        def rot():
            b = rot_state[0] % 8
            rot_state[0] += 1
            return b

        def T(stack, name, shape, dty):
            return stack.enter_context(nc.sbuf_tensor(name, list(shape), dty))

        def dump(name, ap, shape, dty, reads):
            if name not in dbg:
                return
            o = nc.dram_tensor("dbg_" + name, list(shape), dty, kind="ExternalOutput").ap()
            dbg_out[name] = o
            add("sp", lambda e: e.dma_start(out=o, in_=ap), reads=reads, writes=["dbg_" + name], dma_key="dbg_" + name)

        cst = T(top, "cst", [128, NCONST], F32)
        cb = T(top, "cb", [128, NB16], BF16)
        modcol = T(top, "modcol", [128, 4, 8], F32)
        gate_bc = T(top, "gate_bc", [128, 2, D], F32)
        gates = T(top, "gates", [128, NT, 4], F32)
        desti = T(top, "desti", [128, NT, 4], U32)
        cnt_f = T(top, "cnt_f", [1, NE], F32)
        cnt_i = T(top, "cnt_i", [1, NE], mybir.dt.int32)
        identF = cst[:, C_ID:C_ID + 128]
        identB = cb[:, B_ID:B_ID + 128]
        triB = cb[:, B_TRI:B_TRI + 128]
        onesB = cb[:, B_ONE:B_ONE + 128]
        ones0B = cb[:, B_ONE0:B_ONE0 + 128]
        cmaskF = cst[:, C_CM:C_CM + 128]
        band = lambda wg, v: cb[:, B_BAND + 128 * (3 * wg + v):B_BAND + 128 * (3 * wg + v) + 128]

        add("sp", lambda e: e.dma_start(out=cst[:], in_=consts_d), writes=["cst"], dma_key="cst")
        add("pool", lambda e: e.dma_start(out=cb[:], in_=constsb_d), writes=["cb"], dma_key="cb")

        with ExitStack() as ph:
            NSLOT = 4
            ring = [T(ph, "wr%d" % i, [128, 4096], BF16) for i in range(NSLOT)]
            loads = []
            wada_v = wada_d.rearrange("(c p) n -> p c n", p=128)
            ada_load = lambda j: (wada_v[:, :, 512 * j:512 * (j + 1)], 8, 512)
            for j in range(4):
                loads.append(ada_load(j))
            win_v = win_d.rearrange("(c p) n -> p c n", p=128)
            wba_v = wba_d.rearrange("(c p) n -> p c n", p=128)
            wbp_v = wbp_d.rearrange("(c p) n -> p c n", p=128)
            wout_v = wout_d.rearrange("(c p) n -> p c n", p=128)
            GCH = [(0, 512), (512, 1024), (1024, 1224), (1224, 1736),
                   (1736, 2248), (2248, 2760), (2760, 3272), (3272, 3784)]
            for g in range(4):
                for (a, b) in GCH:
                    loads.append((win_v[:, :, a:b], 8, b - a))
                if g == 0:
                    for j in range(4, 12):
                        loads.append(ada_load(j))
                loads.append((wba_v, 4, 1024))
                loads.append((wbp_v, 4, 1024))
                loads.append((wout_v[:, :, 0:512], 8, 512))
                loads.append((wout_v[:, :, 512:1024], 8, 512))
            issued = [0]
            LOOK = 2

            def wget(n):
                while issued[0] <= min(n + LOOK, len(loads) - 1):
                    k = issued[0]
                    src, c_, n_ = loads[k]
                    slot = ring[k % NSLOT]
                    dst = slot[:, 0:c_ * n_].rearrange("p (c n) -> p c n", c=c_)
                    add("pool", lambda e, dst=dst, src=src: e.dma_start(out=dst, in_=src),
                        writes=["wr%d" % (k % NSLOT)], dma_key="wr%d" % (k % NSLOT))
                    issued[0] += 1
                src, c_, n_ = loads[n]
                return "wr%d" % (n % NSLOT), ring[n % NSLOT][:, 0:c_ * n_].rearrange("p (c n) -> p c n", c=c_)

            x1b = T(ph, "x1b", [128, D], BF16)
            wuk_n = x1b
            wukT2 = T(ph, "wukT2", [128, 4, 128], BF16)
            wuv = T(ph, "wuv", [128, 512], BF16)
            wpool = T(ph, "wpool", [128, 4, 128], BF16)
            pscale = T(ph, "pscale", [128, 4], F32)
            kvg_bc = T(ph, "kvg_bc", [128, 128], F32)
            ln1g_bc = T(ph, "ln1g_bc", [128, D], F32)
            ln1b_bc = T(ph, "ln1b_bc", [128, D], F32)
            wrt = T(ph, "wrt", [128, 8, NE], F32)
            br_bc = T(ph, "br_bc", [128, NE], F32)
            alL = T(ph, "alL", [128, 2048], BF16)
            alR = T(ph, "alR", [128, 1024], BF16)
            Mall = T(ph, "Mall", [128, NT, NE], BF16)
            NM = T(ph, "NM", [128, S_LEN], BF16)
            mixT = T(ph, "mixT", [128, 8, 512], BF16)
            add("pool", lambda e: e.memset(mixT[:, 0:2, :], 0.0), writes=["mixT"])
            xp = T(ph, "xp", [128, D], F32)
            xr = T(ph, "xr", [128, D], F32)
            diag = [T(ph, "diag%d" % i, [128, 8, 128], BF16) for i in range(2)]

            add("pool", lambda e: e.dma_start(out=wuk_n[:, 0:512], in_=wuk_d), writes=["x1b"], dma_key="wuk_n")
            add("pool", lambda e: e.dma_start(out=wuv[:], in_=wuv_d), writes=["wuv"], dma_key="wuv")
            add("pool", lambda e: e.dma_start(out=wpool[:], in_=wpool_d.rearrange("g c d -> c g d")), writes=["wpool"], dma_key="wpool")
            add("sp", lambda e: e.dma_start(out=pscale[:], in_=pscale_d), writes=["pscale"], dma_key="pscale")
            add("sp", lambda e: e.dma_start(out=kvg_bc[:], in_=kvg_d.broadcast_to([128, 128])), writes=["kvg_bc"], dma_key="kvg_bc")
            add("sp", lambda e: e.dma_start(out=ln1g_bc[:], in_=ln1g_d.broadcast_to([128, D])), writes=["ln1g_bc"], dma_key="ln1g_bc")
            add("sp", lambda e: e.dma_start(out=ln1b_bc[:], in_=ln1b_d.broadcast_to([128, D])), writes=["ln1b_bc"], dma_key="ln1b_bc")
            add("sp", lambda e: e.dma_start(out=wrt[:], in_=wr_d.rearrange("(c p) n -> p c n", p=128)), writes=["wrt"], dma_key="wrt")
            add("sp", lambda e: e.dma_start(out=br_bc[:], in_=br_d.broadcast_to([128, NE])), writes=["br_bc"], dma_key="br_bc")
            add("pool", lambda e: e.memset(alL[:], 0.0), writes=["alL"])
            add("pool", lambda e: e.memset(alR[:], 0.0), writes=["alR"])
            add("pool", lambda e: e.dma_start(out=alL[0:3, :], in_=alL_d), writes=["alL"], dma_key="alL")
            add("pool", lambda e: e.dma_start(out=alR[0:3, :], in_=alR_d), writes=["alR"], dma_key="alR")
            zf_list = list(range(0, XPAD_ROWS, 2048))

            def zero_fill(nmax):
                for _ in range(nmax):
                    if not zf_list:
                        return
                    r0 = zf_list.pop(0)
                    nr = min(2048, XPAD_ROWS - r0)
                    add("sp", lambda e, r0=r0, nr=nr: e.dma_start(
                        out=xpad_d[r0:r0 + nr, :].rearrange("(p a) n -> p a n", p=128),
                        in_=mixT[:, 0:2, :].rearrange("p a b -> p (a b)").unsqueeze(1).broadcast_to([128, nr // 128, 1024])),
                        reads=["mixT"], writes=["xpadZ%d" % r0], dma_key="xpadZ")
            xpadZ = ["xpadZ%d" % r0 for r0 in range(0, XPAD_ROWS, 2048)]

            b = rot()
            psb = PS[b][:].bitcast(BF16)

            def f_wukT(e, psb=psb):
                last = None
                for pr in range(4):
                    last = e.transpose(out=psb[:, pr * 128:(pr + 1) * 128], in_=wuk_n[:, pr * 128:(pr + 1) * 128], identity=identB)
                return last
            add("pe", f_wukT, reads=["x1b", "cb"], writes=[psn(b)])
            add("dve", lambda e, psb=psb: e.tensor_copy(out=wukT2[:].rearrange("p a b -> p (a b)"), in_=psb[:, 0:512]), reads=[psn(b)], writes=["wukT2"])

            c_sb = T(ph, "c_sb", [128, 8], F32)
            c_si = T(ph, "c_si", [128, 8], BF16)
            olatn = T(ph, "olatn", [128, 8, 128], BF16)
            condB = olatn
            bb = [xp[:, 0:512], xp[:, 512:1024]]
            modtmp = [xr[:, 0:512], xr[:, 512:1024]]
            add("sp", lambda e: e.dma_start(out=c_sb[:], in_=ccol_d), writes=["c_sb"], dma_key="c_sb")
            add("act", lambda e: e.activation(out=c_si[:], in_=c_sb[:], func=AF.Silu), reads=["c_sb"], writes=["c_si"])
            add("dve", lambda e: e.tensor_copy(out=condB[:], in_=c_si[:].unsqueeze(2).broadcast_to([128, 8, 128])), reads=["c_si"], writes=["olatn"])
            VEC = {0: ("col", 1, 0.0), 1: ("col", 0, 1.0), 2: ("gate", 0, 0.0), 3: ("col", 3, 0.0), 4: ("col", 2, 1.0), 5: ("gate", 1, 0.0)}
            def ada_chunk(j):
                v, half = j // 2, j % 2
                wres, wv = wget(j if j < 4 else j + 8)
                add("act", lambda e, j=j: e.dma_start(out=bb[j % 2], in_=bada_d[:, 512 * j:512 * (j + 1)].broadcast_to([128, 512])),
                    writes=["xp%d" % (j % 2)], dma_key="xp%d" % (j % 2))
                b = rot()

                def f_mod(e, b=b, wv=wv):
                    last = None
                    for k in range(8):
                        last = e.matmul(PS[b][:], lhsT=condB[:, k, :], rhs=wv[:, k, :], start=(k == 0), stop=(k == 7))
                    return last
                add("pe", f_mod, reads=["olatn", wres], writes=[psn(b)])
                kind, idx, addc = VEC[v]
                if kind == "gate":
                    add("dve", lambda e, b=b, j=j, idx=idx, half=half: e.tensor_tensor(
                        out=gate_bc[:, idx, half * 512:(half + 1) * 512], in0=PS[b][:], in1=bb[j % 2], op=ALU.add),
                        reads=[psn(b), "xp%d" % (j % 2)], writes=["gate_bc%d_%d" % (idx, half)])
                else:
                    mt = modtmp[j % 2]
                    add("dve", lambda e, b=b, j=j, mt=mt: e.tensor_tensor(out=mt, in0=PS[b][:], in1=bb[j % 2], op=ALU.add),
                        reads=[psn(b), "xp%d" % (j % 2)], writes=["xr"])
                    b2 = rot()

                    def f_tr(e, b2=b2, mt=mt):
                        last = None
                        for q4 in range(4):
                            last = e.transpose(out=PS[b2][:, q4 * 128:(q4 + 1) * 128], in_=mt[:, q4 * 128:(q4 + 1) * 128], identity=identF)
                        return last
                    add("pe", f_tr, reads=["xr", "cst"], writes=[psn(b2)])
                    add("dve", lambda e, b2=b2, idx=idx, half=half, addc=addc: e.tensor_scalar(
                        out=modcol[:, idx, 4 * half:4 * half + 4], in0=PS[b2][:].rearrange("p (a b) -> p a b", a=4)[:, :, 0],
                        scalar1=addc, scalar2=None, op0=ALU.add),
                        reads=[psn(b2)], writes=["modcol%d_%d" % (idx, half)])
            for j in range(4):
                ada_chunk(j)
            MODC = ["modcol%d_%d" % (i, h) for i in range(4) for h in range(2)]
            MODC1 = ["modcol%d_%d" % (i, h) for i in range(2) for h in range(2)]
            GATE1 = ["gate_bc0_0", "gate_bc0_1"]
            GATE2 = ["gate_bc1_0", "gate_bc1_1"]
            dump("modcol", modcol[:], [128, 4, 8], F32, MODC)
            dump("gate_bc", gate_bc[:], [128, 2, D], F32, GATE1 + GATE2)

            if stop_after == "A":
                S.drain_dmas("sp")
                S.emit_phase()
                return nc, dbg_out

            xs = [xr, xp]
            hT = T(ph, "hT", [128, 8, 512], BF16)
            qT = T(ph, "qT", [128, 4, 512], BF16)
            qlatT = T(ph, "qlatT", [128, 8, 512], BF16)
            qiT = T(ph, "qiT", [128, 4, 512], BF16)
            kiT2 = T(ph, "kiT2", [128, 2, S_LEN], BF16)
            ckv_tok = T(ph, "ckv_tok", [128, NT, 128], BF16)
            ckvT = T(ph, "ckvT", [128, S_LEN], BF16)
            wi_tok = T(ph, "wi_tok", [128, 4, 8], F32)
            u_buf = T(ph, "u_buf", [128, 5, 512], BF16)
            pooledT = T(ph, "pooledT", [128, 4, 512], BF16)
            ypoolT = T(ph, "ypoolT", [128, 4, 512], BF16)
            yattnT = T(ph, "yattnT", [128, 4, 512], BF16)
            sgT = T(ph, "sgT", [128, 16, 512], BF16)
            ssq = T(ph, "ssq", [128, 4], F32)
            rstd4 = T(ph, "rstd4", [128, 4], F32)
            sqj = T(ph, "sqj", [128, 128], BF16)
            score = [T(ph, "score%d" % i, [128, S_LEN], F32) for i in range(2)]
            NMT = T(ph, "NMT", [128, NT, 128], BF16)
            Rb = [T(ph, "Rb%d" % i, [128, 512], BF16) for i in range(3)]
            PT = [T(ph, "PT%d" % i, [128, 512], BF16) for i in range(2)]
            rden = T(ph, "rden", [128, 512], F32)
            bis = T(ph, "bis", [128, 8], F32)
            wtab = T(ph, "wtab", [128, 16], F32)
            amax = [T(ph, "amax%d" % i, [128, 4], F32) for i in range(2)]
            t1 = T(ph, "t1", [128, 512], F32)
            t2 = T(ph, "t2", [128, 512], F32)
            x1t = T(ph, "x1t", [128, D], F32)
            h2T = T(ph, "h2T", [128, 8, 128], F32)
            lnst = T(ph, "lnst", [128, 2, 6], F32)
            lnmv = T(ph, "lnmv", [128, 4], F32)
            lg = T(ph, "lg", [128, NE], F32)
            top8 = T(ph, "top8", [128, 8], F32)
            rsm = T(ph, "rsm", [128, 8], F32)
            e4 = T(ph, "e4", [128, 4], F32)
            destf = T(ph, "destf", [128, NE], F32)
            destk = T(ph, "destk", [128, 4], F32)
            junk32 = T(ph, "junk32", [128, NE], F32)

            evac_flip = [0]

            def evac(out_ap, in_ap, reads, writes, eng=None):
                if eng is None:
                    eng = "act" if evac_flip[0] % 2 == 0 else "dve"
                    evac_flip[0] += 1
                if eng == "act":
                    add("act", lambda e: e.activation(out=out_ap, in_=in_ap, func=AF.Identity), reads=reads, writes=writes)
                else:
                    add("dve", lambda e: e.tensor_copy(out=out_ap, in_=in_ap), reads=reads, writes=writes)

            HT = ["hT0", "hT1", "hT2", "hT3"]

            def mm8(b, out_ap, lhs_fn, rhs_fn, reads, nk=8):
                def f(e):
                    last = None
                    for k in range(nk):
                        last = e.matmul(out_ap, lhsT=lhs_fn(k), rhs=rhs_fn(k), start=(k == 0), stop=(k == nk - 1))
                    return last
                add("pe", f, reads=reads, writes=[psn(b)])

            CH0 = 12
            for g in range(4):
                cbase = 4 if g == 0 else 24 + 12 * (g - 1)
                cb8 = cbase + (16 if g == 0 else 8)
                for t in range(4):
                    i = 4 * g + t
                    xb_ = xs[i % 2]
                    xsn = ["xr"] if i % 2 == 0 else ["xp0", "xp1"]
                    add("sp", lambda e, i=i, xb_=xb_: e.dma_start(out=xb_[:], in_=x_d[i * 128:(i + 1) * 128, :]),
                        writes=xsn, dma_key="xs%d" % (i % 2))
                    for hf in range(2):
                        b = rot()

                        def f_xt(e, b=b, xb_=xb_, hf=hf):
                            last = None
                            for c4 in range(4):
                                c = 4 * hf + c4
                                last = e.transpose(out=PS[b][:, c4 * 128:(c4 + 1) * 128], in_=xb_[:, c * 128:(c + 1) * 128], identity=identF)
                            return last
                        add("pe", f_xt, reads=xsn + ["cst"], writes=[psn(b)])
                        for c4 in range(4):
                            c = 4 * hf + c4
                            if c4 % 2 == 0:
                                add("act", lambda e, b=b, c=c, c4=c4, t=t: e.activation(
                                    out=hT[:, c, t * 128:(t + 1) * 128], in_=PS[b][:, c4 * 128:(c4 + 1) * 128], func=AF.Identity,
                                    scale=modcol[:, 0, c:c + 1], bias=modcol[:, 1, c:c + 1]),
                                    reads=[psn(b)] + MODC1, writes=["hT%d" % t])
                            else:
                                add("dve", lambda e, b=b, c=c, c4=c4, t=t: e.tensor_scalar(
                                    out=hT[:, c, t * 128:(t + 1) * 128], in0=PS[b][:, c4 * 128:(c4 + 1) * 128],
                                    scalar1=modcol[:, 0, c:c + 1], scalar2=modcol[:, 1, c:c + 1], op0=ALU.mult, op1=ALU.add),
                                    reads=[psn(b)] + MODC1, writes=["hT%d" % t])
                if g == 0:
                    zero_fill(100)
                    dump("hT", hT[:], [128, 8, 512], BF16, HT)

                wres, wv = wget(cbase + 0)
                for pr in range(4):
                    b = rot()
                    mm8(b, PS[b][:], lambda k, wv=wv, pr=pr: wv[:, k, pr * 128:(pr + 1) * 128], lambda k: hT[:, k, :], [wres] + HT)
                    evac(qT[:, pr, :], PS[b][:], [psn(b)], ["qT"])
                for h in range(8):
                    pr, hb = h // 2, (h % 2) * 64
                    b = rot()
                    add("pe", lambda e, b=b, pr=pr, hb=hb: e.matmul(PS[b][:], lhsT=wukT2[hb:hb + 64, pr, :], rhs=qT[hb:hb + 64, pr, :], start=True, stop=True),
                        reads=["wukT2", "qT"], writes=[psn(b)])
                    if h % 2 == 0:
                        add("act", lambda e, b=b, h=h: e.activation(out=qlatT[:, h, :], in_=PS[b][:], func=AF.Identity, scale=0.125),
                            reads=[psn(b)], writes=["qlatT"])
                    else:
                        add("dve", lambda e, b=b, h=h: e.tensor_scalar(out=qlatT[:, h, :], in0=PS[b][:], scalar1=0.125, scalar2=None, op0=ALU.mult),
                            reads=[psn(b)], writes=["qlatT"])
                if g == 0:
                    dump("qlatT", qlatT[:], [128, 8, 512], BF16, ["qlatT"])

                wres, wv = wget(cbase + 1)
                b = rot()
                for t in range(4):
                    mm8(b, PS[b][:, t * 128:(t + 1) * 128], lambda k, t=t: hT[:, k, t * 128:(t + 1) * 128], lambda k, wv=wv: wv[:, k, 0:128], [wres] + HT)
                for t in range(4):
                    add("act", lambda e, b=b, t=t: e.activation(out=sqj[:], in_=PS[b][:, t * 128:(t + 1) * 128], func=AF.Square, accum_out=ssq[:, t:t + 1]),
                        reads=[psn(b)], writes=["sqj", "ssq"])
                add("dve", lambda e: e.tensor_scalar(out=rstd4[:], in0=ssq[:], scalar1=1.0 / 128, scalar2=EPS, op0=ALU.mult, op1=ALU.add), reads=["ssq"], writes=["rstd4"])
                add("act", lambda e: e.activation(out=rstd4[:], in_=rstd4[:], func=AF.Sqrt), reads=["rstd4"], writes=["rstd4"])
                add("dve", lambda e: e.reciprocal(out=rstd4[:], in_=rstd4[:]), reads=["rstd4"], writes=["rstd4"])
                for t in range(4):
                    i = 4 * g + t
                    add("dve", lambda e, b=b, t=t, i=i: e.scalar_tensor_tensor(
                        out=ckv_tok[:, i, :], in0=PS[b][:, t * 128:(t + 1) * 128], scalar=rstd4[:, t:t + 1], in1=kvg_bc[:], op0=ALU.mult, op1=ALU.mult),
                        reads=[psn(b), "rstd4", "kvg_bc"], writes=["ckv_tok%d" % i])
                b2 = rot()
                psb2 = PS[b2][:].bitcast(BF16)

                def f_ckT(e, psb2=psb2, g=g):
                    last = None
                    for t in range(4):
                        last = e.transpose(out=psb2[:, t * 128:(t + 1) * 128], in_=ckv_tok[:, 4 * g + t, :], identity=identB)
                    return last
                add("pe", f_ckT, reads=["ckv_tok%d" % (4 * g + t) for t in range(4)] + ["cb"], writes=[psn(b2)])
                evac(ckvT[:, g * 512:(g + 1) * 512], psb2[:, 0:512], [psn(b2)], ["ckvT%d" % g])
                for pr in range(3):
                    b = rot()
                    mm8(b, PS[b][:], lambda k, wv=wv, pr=pr: wv[:, k, 128 + pr * 128:256 + pr * 128], lambda k: hT[:, k, :], [wres] + HT)
                    evac(qiT[:, pr, :], PS[b][:], [psn(b)], ["qiT"])
                wres, wv = wget(cbase + 2)
                b = rot()
                mm8(b, PS[b][:], lambda k, wv=wv: wv[:, k, 0:128], lambda k: hT[:, k, :], [wres] + HT)
                evac(qiT[:, 3, :], PS[b][:], [psn(b)], ["qiT"])
                b = rot()
                mm8(b, PS[b][0:64, :], lambda k, wv=wv: wv[:, k, 128:192], lambda k: hT[:, k, :], [wres] + HT)
                mm8(b, PS[b][64:128, :], lambda k, wv=wv: wv[:, k, 128:192], lambda k: hT[:, k, :], [wres] + HT)
                if g == 0:
                    add("pool", lambda e: e.memset(kiT2[:], 0.0), writes=["kiT2_z"])
                evac(kiT2[0:64, 0, g * 512:(g + 1) * 512], PS[b][0:64, :], [psn(b), "kiT2_z"], ["kiT2_%d" % g])
                evac(kiT2[64:128, 1, g * 512:(g + 1) * 512], PS[b][64:128, :], [psn(b), "kiT2_z"], ["kiT2_%d" % g])
                b = rot()
                for t in range(4):
                    mm8(b, PS[b][:, t * 8:(t + 1) * 8], lambda k, t=t: hT[:, k, t * 128:(t + 1) * 128], lambda k, wv=wv: wv[:, k, 192:200], [wres] + HT)
                evac(wi_tok[:].rearrange("p a b -> p (a b)"), PS[b][:, 0:32], [psn(b)], ["wi_tok"], eng="dve")
                if g == 0:
                    dump("ckv_tok", ckv_tok[:, 0:4, :], [128, 4, 128], BF16, ["ckv_tok%d" % t for t in range(4)])
                    dump("ckvT", ckvT[:, 0:512], [128, 512], BF16, ["ckvT0"])
                    dump("qiT", qiT[:], [128, 4, 512], BF16, ["qiT"])
                    dump("kiT2", kiT2[:, 0, 0:512], [128, 512], BF16, ["kiT2_0"])
                    dump("wi_tok", wi_tok[:], [128, 4, 8], F32, ["wi_tok"])

                def stage_B4sg(g=g, cbase=cbase):
                    wres, wv = wget(cbase + 3)
                    if g > 0:
                        add("pool", lambda e: e.tensor_copy(out=u_buf[:, 0, :], in_=u_buf[:, 4, :]), reads=["u4"], writes=["u0"])
                    for t in range(4):
                        b = rot()
                        mm8(b, PS[b][:], lambda k, t=t: hT[:, k, t * 128:(t + 1) * 128], lambda k, wv=wv: wv[:, k, :], [wres] + HT)
                        evac(u_buf[:, t + 1, :], PS[b][:], [psn(b)], ["u%d" % (t + 1)])
                    for t in range(4):
                        i = 4 * g + t
                        b = rot()

                        def f_pool(e, b=b, t=t, i=i):
                            last = None
                            for wg in range(4):
                                o = PS[b][:, wg * 128:(wg + 1) * 128]
                                if i == 0:
                                    last = e.matmul(o, lhsT=u_buf[:, t + 1, wg * 128:(wg + 1) * 128], rhs=band(wg, 2), start=True, stop=True)
                                else:
                                    e.matmul(o, lhsT=u_buf[:, t + 1, wg * 128:(wg + 1) * 128], rhs=band(wg, 0), start=True, stop=False)
                                    last = e.matmul(o, lhsT=u_buf[:, t, wg * 128:(wg + 1) * 128], rhs=band(wg, 1), start=False, stop=True)
                            return last
                        add("pe", f_pool, reads=["u%d" % t, "u%d" % (t + 1), "cb"], writes=[psn(b)])
                        evac(pooledT[:, :, t * 128:(t + 1) * 128], PS[b][:].rearrange("p (a b) -> p a b", a=4), [psn(b)], ["pooledT"])
                    for wg in range(4):
                        b = rot()
                        add("pe", lambda e, b=b, wg=wg: e.matmul(PS[b][:], lhsT=wpool[:, wg, :], rhs=pooledT[:, wg, :], start=True, stop=True),
                            reads=["wpool", "pooledT"], writes=[psn(b)])
                        add("act", lambda e, b=b, wg=wg: e.activation(out=ypoolT[:, wg, :], in_=PS[b][:], func=AF.Identity, scale=pscale[:, wg:wg + 1]),
                            reads=[psn(b), "pscale"], writes=["ypoolT"])
                    if g == 0:
                        dump("pooledT", pooledT[:], [128, 4, 512], BF16, ["pooledT"])
                        dump("ypoolT", ypoolT[:], [128, 4, 512], BF16, ["ypoolT"])

                    for m in range(4):
                        wres, wv = wget(cbase + 4 + m)
                        for q4 in range(4):
                            n = 4 * m + q4
                            b = rot()
                            mm8(b, PS[b][:], lambda k, wv=wv, q4=q4: wv[:, k, q4 * 128:(q4 + 1) * 128], lambda k: hT[:, k, :], [wres] + HT)
                            add("act", lambda e, b=b, n=n: e.activation(out=sgT[:, n, :], in_=PS[b][:], func=AF.Sigmoid), reads=[psn(b)], writes=["sgT"])
                    if g == 0:
                        dump("sgT", sgT[:], [128, 16, 512], BF16, ["sgT"])


                if stop_after == "B4":
                    S.drain_dmas("sp")
                    S.emit_phase()
                    return nc, dbg_out

                def build_diag(t, g=g):
                    i = 4 * g + t
                    dg = diag[i % 2]
                    dgn = "diag%d" % (i % 2)
                    for h in range(8):
                        add("dve", lambda e, h=h, t=t, dg=dg: e.tensor_scalar(out=dg[:, h, :], in0=identB, scalar1=wi_tok[:, t, h:h + 1], scalar2=None, op0=ALU.mult),
                            reads=["cb", "wi_tok"], writes=[dgn])

                def indexer(t, g=g):
                    i = 4 * g + t
                    sc = score[i % 2]
                    scn = "score%d" % (i % 2)
                    dg = diag[i % 2]
                    dgn = "diag%d" % (i % 2)
                    nch = i // 4 + 1
                    for c in range(nch):
                        N = 512 if c < nch - 1 else 128 * (i % 4 + 1)
                        bS = 1

                        def dots(h, c=c, N=N, t=t):
                            pr, hb = h // 2, (h % 2) * 64
                            bD = 6 + (h % 2)
                            R = Rb[h % 3]
                            add("pe", lambda e, bD=bD, pr=pr, hb=hb: e.matmul(
                                PS[bD][:, 0:N], lhsT=qiT[:, pr, t * 128:(t + 1) * 128], rhs=kiT2[:, hb // 64, c * 512:c * 512 + N], start=True, stop=True),
                                reads=["qiT", "kiT2_%d" % c], writes=[psn(bD)])
                            add("act", lambda e, bD=bD, R=R: e.activation(out=R[:, 0:N], in_=PS[bD][:, 0:N], func=AF.Relu),
                                reads=[psn(bD)], writes=["Rb%d" % (h % 3)])

                        def wsum(h, N=N, bS=bS):
                            R = Rb[h % 3]
                            add("pe", lambda e, h=h, R=R: e.matmul(PS[bS][:, 0:N], lhsT=dg[:, h, :], rhs=R[:, 0:N], start=(h == 0), stop=(h == 7)),
                                reads=[dgn, "Rb%d" % (h % 3)], writes=[psn(bS)], pe_chain=True)
                        dots(0)
                        dots(1)
                        for h in range(8):
                            wsum(h)
                            if h + 2 < 8:
                                dots(h + 2)
                        add("act", lambda e, bS=bS, c=c, sc=sc, N=N: e.activation(out=sc[:, c * 512:c * 512 + N], in_=PS[bS][:, 0:N], func=AF.Identity),
                            reads=[psn(bS)], writes=[scn])
                        add("dve", lambda e, N=N, c=c, i=i, sc=sc: e.tensor_reduce(out=amax[i % 2][:, c:c + 1], in_=sc[:, c * 512:c * 512 + N], axis=AX.X, op=ALU.max, apply_absolute_value=True),
                            reads=[scn], writes=["amax%d" % (i % 2)])
                        if c == nch - 1:
                            add("dve", lambda e, sc=sc, i=i: e.tensor_tensor(out=sc[:, i * 128:(i + 1) * 128], in0=sc[:, i * 128:(i + 1) * 128], in1=cmaskF, op=ALU.add),
                                reads=[scn, "cst"], writes=[scn])
                    return nch

                def threshold(t, nch, g=g):
                    i = 4 * g + t
                    sc = score[i % 2]
                    scn = "score%d" % (i % 2)
                    L = 128 * (i + 1)
                    A, mid, cnt, tmp, thr = (bis[:, k:k + 1] for k in range(5))
                    if i < 2:
                        add("dve", lambda e: e.memset(thr, -1.0e29), writes=["bis"])
                    else:
                        add("dve", lambda e: e.tensor_reduce(out=A, in_=amax[i % 2][:, 0:nch], axis=AX.X, op=ALU.max), reads=["amax%d" % (i % 2)], writes=["bis"])
                        add("dve", lambda e: e.tensor_scalar(out=wtab[:, 0:KBIS + 1], in0=cst[:, C_POW:C_POW + KBIS + 1], scalar1=A, scalar2=None, op0=ALU.mult),
                            reads=["bis", "cst"], writes=["wtab"])
                        add("dve", lambda e: e.memset(mid, 0.0), writes=["bis"])
                        for k in range(KBIS):
                            add("dve", lambda e: e.tensor_scalar(out=NM[:, 0:L], in0=sc[:, 0:L], scalar1=mid, scalar2=None, op0=ALU.is_ge, op1=ALU.add, accum_out=cnt),
                                reads=[scn, "bis"], writes=["NM", "bis"])
                            add("dve", lambda e: e.tensor_scalar(out=tmp, in0=cnt, scalar1=256.0, scalar2=-0.5, op0=ALU.is_ge, op1=ALU.add), reads=["bis"], writes=["bis"])
                            add("dve", lambda e, k=k: e.scalar_tensor_tensor(out=mid, in0=tmp, scalar=wtab[:, k:k + 1], in1=mid, op0=ALU.mult, op1=ALU.add),
                                reads=["bis", "wtab"], writes=["bis"])
                        add("dve", lambda e: e.tensor_tensor(out=thr, in0=mid, in1=wtab[:, KBIS:KBIS + 1], op=ALU.subtract), reads=["bis", "wtab"], writes=["bis"])
                    add("dve", lambda e: e.tensor_scalar(out=NM[:, 0:L], in0=sc[:, 0:L], scalar1=thr, scalar2=-30000.0, op0=ALU.is_lt, op1=ALU.mult),
                        reads=[scn, "bis"], writes=["NM"])
                    if g == 0 and t == 3:
                        dump("score3", sc[:, 0:512], [128, 512], F32, [scn])
                        dump("thr3", bis[:], [128, 8], F32, ["bis"])
                        dump("NM3", NM[:, 0:512], [128, 512], BF16, ["NM"])

                def attentionA(t, g=g):
                    i = 4 * g + t
                    for j0 in range(0, i + 1, 8):
                        nb = min(8, i + 1 - j0)
                        b = 6 + (j0 // 8) % 2
                        psb = PS[b][:].bitcast(BF16)

                        def f_nmt(e, psb=psb, j0=j0, nb=nb):
                            last = None
                            for jj in range(nb):
                                last = e.transpose(out=psb[:, jj * 128:(jj + 1) * 128], in_=NM[:, (j0 + jj) * 128:(j0 + jj + 1) * 128], identity=identB)
                            return last
                        add("pe", f_nmt, reads=["NM", "cb"], writes=[psn(b)])
                        evac(NMT[:, j0:j0 + nb, :].rearrange("p a b -> p (a b)"), psb[:, 0:nb * 128], [psn(b)], ["NMT"], eng="dve")

                def attentionB(t, g=g):
                    i = 4 * g + t
                    units = [(j, hh) for j in range(i + 1) for hh in range(2)]

                    LB = (0, 7)

                    def qk(n):
                        j, hh = units[n]
                        d = i - j
                        bl = LB[n % 2]
                        o3 = PS[bl][:].rearrange("p (a b) -> p a b", a=4)

                        def f(e):
                            e.matmul(o3, lhsT=ckvT[:, j * 128:(j + 1) * 128], rhs=qlatT[:, 4 * hh:4 * hh + 4, t * 128:(t + 1) * 128], start=True, stop=False)
                            e.matmul(o3, lhsT=alL[:, d * 128:(d + 1) * 128], rhs=alR[:].rearrange("p (a b) -> p a b", a=8)[:, 4 * hh:4 * hh + 4, :], start=False, stop=False)
                            return e.matmul(o3, lhsT=identB, rhs=NMT[:, j, :].unsqueeze(1).broadcast_to([128, 4, 128]), start=False, stop=True)
                        add("pe", f, reads=["ckvT%d" % (j // 4), "qlatT", "alL", "alR", "cb", "NMT"], writes=[psn(bl)])
                        add("act", lambda e, n=n, bl=bl: e.activation(out=PT[n % 2][:], in_=PS[bl][:], func=AF.Exp), reads=[psn(bl)], writes=["PT%d" % (n % 2)])

                    def pv(n):
                        j, hh = units[n]

                        def f(e):
                            e.matmul(PS[2 + hh][:], lhsT=ckv_tok[:, j, :], rhs=PT[n % 2][:], start=(j == 0), stop=(j == i))
                            return e.matmul(PS[4 + hh][:], lhsT=onesB, rhs=PT[n % 2][:], start=(j == 0), stop=(j == i))
                        add("pe", f, reads=["ckv_tok%d" % j, "PT%d" % (n % 2), "cb"], writes=[psn(2 + hh), psn(4 + hh)], pe_chain=True)
                    for n in range(len(units)):
                        qk(n)
                        if n > 0:
                            pv(n - 1)
                    pv(len(units) - 1)

                def attentionC(t, g=g):
                    i = 4 * g + t
                    for hh in range(2):
                        add("dve", lambda e, hh=hh: e.tensor_scalar(out=rden[:], in0=PS[4 + hh][:], scalar1=1.0e-30, scalar2=None, op0=ALU.max), reads=[psn(4 + hh)], writes=["rden"])
                        add("dve", lambda e: e.reciprocal(out=rden[:], in_=rden[:]), reads=["rden"], writes=["rden"])
                        add("dve", lambda e, hh=hh: e.tensor_tensor(out=olatn[:, 4 * hh:4 * hh + 4, :].rearrange("p a b -> p (a b)"), in0=PS[2 + hh][:], in1=rden[:], op=ALU.mult),
                            reads=[psn(2 + hh), "rden"], writes=["olatn"])

                def attentionD(t, g=g):
                    i = 4 * g + t
                    b = 7

                    def f_y(e):
                        last = None
                        for h in range(8):
                            pr, hb = h // 2, (h % 2) * 64
                            last = e.matmul(PS[b][hb:hb + 64, pr * 128:(pr + 1) * 128], lhsT=wuv[:, h * 64:(h + 1) * 64], rhs=olatn[:, h, :], start=True, stop=True)
                        return last
                    add("pe", f_y, reads=["wuv", "olatn"], writes=[psn(b)])
                    evac(yattnT[:, :, t * 128:(t + 1) * 128], PS[b][:].rearrange("p (a b) -> p a b", a=4), [psn(b)], ["yattnT"], eng="act")
                    if g == 0 and t == 3:
                        dump("olatn3", olatn[:], [128, 8, 128], BF16, ["olatn"])

                nchs = {}
                import os as _os
                if _os.environ.get("IDX1"):
                    indexer(int(_os.environ["IDX1"]))
                    S.drain_dmas("sp"); S.emit_phase(); return nc, dbg_out
                build_diag(0)
                build_diag(1)
                nchs[0] = indexer(0)
                threshold(0, nchs[0])
                stage_B4sg()
                if g == 0:
                    for j in range(4, 12):
                        ada_chunk(j)
                nchs[1] = indexer(1)
                for t in range(4):
                    attentionA(t)
                    if t + 2 < 4:
                        build_diag(t + 2)
                    if t + 1 < 4:
                        threshold(t + 1, nchs[t + 1])
                    attentionB(t)
                    attentionC(t)
                    if t + 2 < 4:
                        nchs[t + 2] = indexer(t + 2)
                    attentionD(t)
                if g == 0:
                    dump("yattnT", yattnT[:], [128, 4, 512], BF16, ["yattnT"])

                if stop_after == "B5":
                    S.drain_dmas("sp")
                    S.emit_phase()
                    return nc, dbg_out

                wresA, wvA = wget(cb8 + 0)
                wresP, wvP = wget(cb8 + 1)
                for n in range(8):
                    bA = rot()
                    mm8(bA, PS[bA][:], lambda k, n=n, wvA=wvA: wvA[:, k, n * 128:(n + 1) * 128], lambda k: yattnT[:, k, :], [wresA, "yattnT"], nk=4)
                    bP = rot()
                    mm8(bP, PS[bP][:], lambda k, n=n, wvP=wvP: wvP[:, k, n * 128:(n + 1) * 128], lambda k: ypoolT[:, k, :], [wresP, "ypoolT"], nk=4)
                    add("dve", lambda e, bA=bA, n=n: e.tensor_tensor(out=t1[:], in0=PS[bA][:], in1=sgT[:, n, :], op=ALU.mult), reads=[psn(bA), "sgT"], writes=["t1"])
                    add("dve", lambda e, bP=bP, n=n: e.tensor_tensor(out=t2[:], in0=PS[bP][:], in1=sgT[:, 8 + n, :], op=ALU.mult), reads=[psn(bP), "sgT"], writes=["t2"])
                    add("pool", lambda e, n=n: e.tensor_tensor(out=mixT[:, n, :], in0=t1[:], in1=t2[:], op=ALU.add), reads=["t1", "t2"], writes=["mixT"])
                if g == 0:
                    dump("mixT", mixT[:], [128, 8, 512], BF16, ["mixT"])
                wres0, wv0 = wget(cb8 + 2)
                wres1, wv1 = wget(cb8 + 3)
                def o_bufs(t, g=g):
                    i = 4 * g + t
                    if t % 2 == 0:
                        xp_, xpn = xp[:, :], ["xp0", "xp1"]
                        xr_, xrn = xr[:, :], "xr"
                        x1t_, x1tn = x1t[:, :], "x1t"
                        x1b_, x1bn = x1b[:, :], "x1b"
                        h2T_, h2Tn = h2T[:, :, :], "h2T"
                    else:
                        xp_, xpn = score[0][:, 0:1024], ["score0a", "score0a"]
                        x1t_, x1tn = score[0][:, 1024:2048], "score0b"
                        xr_, xrn = score[1][:, 0:1024], "score1a"
                        h2T_, h2Tn = score[1][:, 1024:2048].rearrange("p (a b) -> p a b", a=8), "score1b"
                        x1b_, x1bn = NM[:, 0:1024], "NM"
                    return i, xp_, xpn, xr_, xrn, x1t_, x1tn, x1b_, x1bn, h2T_, h2Tn

                def O_A(t):
                    i, xp_, xpn, xr_, xrn, x1t_, x1tn, x1b_, x1bn, h2T_, h2Tn = o_bufs(t)
                    add("sp", lambda e, i=i, xr_=xr_: e.dma_start(out=xr_, in_=x_d[i * 128:(i + 1) * 128, :]), writes=[xrn], dma_key="xrld%d" % (t % 2))
                    for hf, (wres_, wv_) in enumerate(((wres0, wv0), (wres1, wv1))):
                        b = rot()
                        mm8(b, PS[b][:], lambda k, t=t: mixT[:, k, t * 128:(t + 1) * 128], lambda k, wv_=wv_: wv_[:, k, :], [wres_, "mixT"])
                        add("dve", lambda e, b=b, hf=hf, xp_=xp_: e.tensor_tensor(out=xp_[:, hf * 512:(hf + 1) * 512], in0=PS[b][:], in1=gate_bc[:, 0, hf * 512:(hf + 1) * 512], op=ALU.mult),
                            reads=[psn(b)] + GATE1, writes=[xpn[hf]])
                    add("dve", lambda e, xp_=xp_, xr_=xr_: e.scalar_tensor_tensor(out=xp_, in0=xr_, scalar=ALPHA, in1=xp_, op0=ALU.mult, op1=ALU.add),
                        reads=[xrn] + xpn, writes=xpn)
                    for hf in range(2):
                        add("dve", lambda e, hf=hf, xp_=xp_: e.bn_stats(out=lnst[:, hf, :], in_=xp_[:, hf * 512:(hf + 1) * 512]), reads=xpn, writes=["lnst"])
                    add("dve", lambda e: e.bn_aggr(out=lnmv[:, 0:2], in_=lnst[:].rearrange("p a b -> p (a b)")), reads=["lnst"], writes=["lnmv"])
                    add("dve", lambda e: e.tensor_scalar(out=lnmv[:, 2:3], in0=lnmv[:, 1:2], scalar1=EPS, scalar2=None, op0=ALU.add), reads=["lnmv"], writes=["lnmv"])
                    add("act", lambda e: e.activation(out=lnmv[:, 2:3], in_=lnmv[:, 2:3], func=AF.Sqrt), reads=["lnmv"], writes=["lnmv"])
                    add("dve", lambda e: e.reciprocal(out=lnmv[:, 2:3], in_=lnmv[:, 2:3]), reads=["lnmv"], writes=["lnmv"])
                    add("dve", lambda e: e.scalar_tensor_tensor(out=lnmv[:, 3:4], in0=lnmv[:, 0:1], scalar=-1.0, in1=lnmv[:, 2:3], op0=ALU.mult, op1=ALU.mult), reads=["lnmv"], writes=["lnmv"])
                    add("act", lambda e, xp_=xp_, x1t_=x1t_: e.activation(out=x1t_, in_=xp_, func=AF.Identity, scale=lnmv[:, 2:3], bias=lnmv[:, 3:4]),
                        reads=xpn + ["lnmv"], writes=[x1tn])
                    add("pool", lambda e, x1t_=x1t_: e.tensor_tensor(out=x1t_, in0=x1t_, in1=ln1g_bc[:], op=ALU.mult), reads=[x1tn, "ln1g_bc"], writes=[x1tn])
                    add("pool", lambda e, x1t_=x1t_: e.tensor_tensor(out=x1t_, in0=x1t_, in1=ln1b_bc[:], op=ALU.add), reads=[x1tn, "ln1b_bc"], writes=[x1tn])

                def O_R(t):
                    i, xp_, xpn, xr_, xrn, x1t_, x1tn, x1b_, x1bn, h2T_, h2Tn = o_bufs(t)
                    add("sp", lambda e, i=i, x1t_=x1t_: e.dma_start(out=x1_d[i * 128:(i + 1) * 128, :], in_=x1t_), reads=[x1tn], writes=["x1d%d" % i], dma_key="x1d")
                    add("act", lambda e, x1t_=x1t_, x1b_=x1b_: e.activation(out=x1b_, in_=x1t_, func=AF.Identity), reads=[x1tn], writes=[x1bn])
                    for hf in range(2):
                        b = rot()

                        def f_x1t(e, b=b, hf=hf, x1t_=x1t_):
                            last = None
                            for c4 in range(4):
                                c = 4 * hf + c4
                                last = e.transpose(out=PS[b][:, c4 * 128:(c4 + 1) * 128], in_=x1t_[:, c * 128:(c + 1) * 128], identity=identF)
                            return last
                        add("pe", f_x1t, reads=[x1tn, "cst"], writes=[psn(b)])
                        for c4 in range(4):
                            c = 4 * hf + c4
                            add("act", lambda e, b=b, c=c, c4=c4, h2T_=h2T_: e.activation(out=h2T_[:, c, :], in_=PS[b][:, c4 * 128:(c4 + 1) * 128], func=AF.Identity,
                                                                                scale=modcol[:, 2, c:c + 1], bias=modcol[:, 3, c:c + 1]),
                                reads=[psn(b)] + MODC, writes=[h2Tn])
                    b = rot()
                    mm8(b, PS[b][:, 0:NE], lambda k, h2T_=h2T_: h2T_[:, k, :], lambda k: wrt[:, k, :], [h2Tn, "wrt"])
                    add("dve", lambda e, b=b: e.tensor_tensor(out=lg[:], in0=PS[b][:, 0:NE], in1=br_bc[:], op=ALU.add), reads=[psn(b), "br_bc"], writes=["lg"])
                    add("dve", lambda e: e.max(out=top8[:], in_=lg[:]), reads=["lg"], writes=["top8"])
                    add("dve", lambda e, i=i: e.tensor_scalar(out=Mall[:, i, :], in0=lg[:], scalar1=top8[:, 3:4], scalar2=None, op0=ALU.is_ge), reads=["lg", "top8"], writes=["Mall%d" % i])
                    add("dve", lambda e: e.tensor_scalar(out=rsm[:, 0:1], in0=top8[:, 0:1], scalar1=-1.0, scalar2=None, op0=ALU.mult), reads=["top8"], writes=["rsm"])
                    add("act", lambda e: e.activation(out=e4[:], in_=top8[:, 0:4], func=AF.Exp, bias=rsm[:, 0:1], scale=1.0, accum_out=rsm[:, 1:2]), reads=["top8", "rsm"], writes=["e4", "rsm"])
                    add("dve", lambda e: e.reciprocal(out=rsm[:, 2:3], in_=rsm[:, 1:2]), reads=["rsm"], writes=["rsm"])
                    add("dve", lambda e, i=i: e.tensor_scalar(out=gates[:, i, :], in0=e4[:], scalar1=rsm[:, 2:3], scalar2=None, op0=ALU.mult), reads=["e4", "rsm"], writes=["gates%d" % i])
                    b = rot()

                    def f_rank(e, b=b, i=i):
                        last = None
                        for j in range(i):
                            last = e.matmul(PS[b][:, 0:NE], lhsT=onesB, rhs=Mall[:, j, :], start=(j == 0), stop=False)
                        return e.matmul(PS[b][:, 0:NE], lhsT=triB, rhs=Mall[:, i, :], start=(i == 0), stop=True)
                    add("pe", f_rank, reads=["Mall%d" % j for j in range(i + 1)] + ["cb"], writes=[psn(b)])
                    add("dve", lambda e, b=b: e.tensor_scalar(out=destf[:], in0=PS[b][:, 0:NE], scalar1=float(CAP - 1), scalar2=None, op0=ALU.min), reads=[psn(b)], writes=["destf"])
                    add("dve", lambda e: e.tensor_tensor(out=destf[:], in0=destf[:], in1=cst[:, C_EB:C_EB + NE], op=ALU.add), reads=["destf", "cst"], writes=["destf"])
                    for k in range(4):
                        add("dve", lambda e, k=k: e.scalar_tensor_tensor(out=junk32[:], in0=lg[:], scalar=top8[:, k:k + 1], in1=destf[:], op0=ALU.is_equal, op1=ALU.mult, accum_out=destk[:, k:k + 1]),
                            reads=["lg", "top8", "destf"], writes=["junk32", "destk"])
                    add("dve", lambda e, i=i: e.tensor_copy(out=desti[:, i, :], in_=destk[:]), reads=["destk"], writes=["desti%d" % i])
                    for k in range(4):
                        add("pool", lambda e, i=i, k=k, x1b_=x1b_: e.indirect_dma_start(out=xpad_d, out_offset=bass.IndirectOffsetOnAxis(ap=desti[:, i, k:k + 1], axis=0), in_=x1b_, in_offset=None),
                            reads=[x1bn, "desti%d" % i] + xpadZ, writes=["xpadS%d_%d" % (i, k)], dma_key="xpadS")
                    if i == 0:
                        dump("x1t0", x1t[:], [128, D], F32, ["x1t"])
                        dump("lg0", lg[:], [128, NE], F32, ["lg"])
                        dump("destk0", destk[:], [128, 4], F32, ["destk"])


                O_A(0)
                for t in range(4):
                    if t + 1 < 4:
                        O_A(t + 1)
                    O_R(t)

            b = rot()

            def f_cnt(e, b=b):
                last = None
                for j in range(NT):
                    last = e.matmul(PS[b][:, 0:NE], lhsT=onesB, rhs=Mall[:, j, :], start=(j == 0), stop=(j == NT - 1))
                return last
            add("pe", f_cnt, reads=["Mall%d" % j for j in range(NT)] + ["cb"], writes=[psn(b)])
            add("dve", lambda e, b=b: e.tensor_copy(out=cnt_f[:], in_=PS[b][0:1, 0:NE]), reads=[psn(b)], writes=["cnt_f"])
            add("dve", lambda e: e.tensor_copy(out=cnt_i[:], in_=cnt_f[:]), reads=["cnt_f"], writes=["cnt_i"])
            dump("cnt_f", cnt_f[:], [1, NE], F32, ["cnt_f"])
            dump("gates", gates[:], [128, NT, 4], F32, ["gates%d" % i for i in range(NT)])
            dump("desti", desti[:], [128, NT, 4], U32, ["desti%d" % i for i in range(NT)])
            S.drain_dmas("sp")
            S.emit_phase()
        XPADS = ["xpadS%d_%d" % (i, k) for i in range(NT) for k in range(4)]
        if stop_after == "B":
            return nc, dbg_out

        with ExitStack() as ph:
            NSL = 8
            CPE = 3
            ring = [T(ph, "er%d" % i, [128, 8, 1024], BF16) for i in range(NSL)]
            wgu_v = wgu_d.rearrange("e (c p) n -> e p c n", p=128)
            wd_v = wd_d.rearrange("e (c p) n -> e p c n", p=128)
            loads = []
            for ex in range(NE):
                for m in range(2):
                    loads.append((wgu_v[ex][:, 4 * m:4 * m + 4, :], 4, 2048))
                loads.append((wd_v[ex][:, :, :], 8, 1024))
            issued = [0]
            LOOKC = 8

            def eprefetch(upto):
                while issued[0] <= min(upto, len(loads) - 1):
                    k = issued[0]
                    add("pool", lambda e, k=k: e.dma_start(out=ring[k % NSL][:].rearrange("p a b -> p (a b)").rearrange("p (c n) -> p c n", c=loads[k][1]), in_=loads[k][0]),
                        writes=["er%d" % (k % NSL)], dma_key="er%d" % (k % NSL))
                    issued[0] += 1

            def eget(n):
                assert n < issued[0]
                return "er%d" % (n % NSL), ring[n % NSL][:].rearrange("p a b -> p (a b)").rearrange("p (c n) -> p c n", c=loads[n][1])

            NTG = SG // 128
            xe_tok = [T(ph, "xe_tok%d" % i, [128, NTG, D], BF16) for i in range(3)]
            XeT = [T(ph, "XeT%d" % i, [128, 8, SG], BF16) for i in range(3)]
            actT = [T(ph, "actT%d" % i, [128, 8, SG], BF16) for i in range(3)]
            ye_tok = [T(ph, "ye_tok%d" % i, [128, NTG, D], BF16) for i in range(2)]
            bgu = T(ph, "bgu", [128, NE * 16], F32)
            bd16 = [T(ph, "bd16_%d" % i, [1, D], BF16) for i in range(2)]
            g1 = [T(ph, "g1_%d" % i, [128, SG], F32) for i in range(2)]
            l2 = [T(ph, "l2_%d" % i, [128, SG], F32) for i in range(2)]
            pp = [T(ph, "pp_%d" % i, [128, SG], F32) for i in range(2)]
            add("sp", lambda e: e.dma_start(out=bgu[:], in_=bgu_d), writes=["bgu"], dma_key="bgu")
            bgu3 = bgu[:].rearrange("p (e f) -> p e f", e=NE)
            add("dve", lambda e: e.tensor_scalar(out=bgu3[:, :, 8:16], in0=bgu3[:, :, 8:16], scalar1=1.0, scalar2=None, op0=ALU.add), reads=["bgu"], writes=["bgu"])
            bdsc = [T(ph, "bdsc_%d" % i, [128, D], BF16) for i in range(2)]
            for i2 in range(2):
                add("pool", lambda e, i2=i2: e.memset(bdsc[i2][:], 0.0), writes=["bdsc_%d" % i2])
            INV = 1.0 / 1.702
            g2s = T(ph, "g2s", [128, D], F32)
            add("dve", lambda e: e.tensor_scalar(out=g2s[:], in0=gate_bc[:, 1, :], scalar1=INV, scalar2=None, op0=ALU.mult), reads=GATE2, writes=["g2s"])
            groups = [(ex, sgi) for ex in range(NE) for sgi in range(NG)]

            class _Cond:
                def __init__(self, ex, sgi):
                    self.on = sgi > 0
                    self.ex, self.sgi = ex, sgi

                def __enter__(self):
                    if self.on:
                        S.begin_cond(cnt_i[0:1, self.ex:self.ex + 1], self.sgi * SG)

                def __exit__(self, *a):
                    if self.on:
                        S.end_cond()

            def bufi(gi):
                ex, sgi = groups[gi]
                return ex % 2 if sgi == 0 else 2

            def stage_load(gi):
                ex, sgi = groups[gi]
                par = bufi(gi)
                r0 = ex * CAP + sgi * SG
                xt = xe_tok[par]
                add("sp", lambda e, r0=r0, xt=xt: e.dma_start(out=xt[:], in_=xpad_d[r0:r0 + SG, :].rearrange("(a p) n -> p a n", p=128)),
                    reads=XPADS + xpadZ, writes=["xe_tok%d" % par], dma_key="xe_tok%d" % par)

            def stage_T(gi):
                ex, sgi = groups[gi]
                par = bufi(gi)
                xt, xtn, XT, XTn = xe_tok[par], "xe_tok%d" % par, XeT[par], "XeT%d" % par
                if True:
                    for k2 in range(4):
                        b = rot()
                        psb = PS[b][:].bitcast(BF16)

                        def f_xet(e, psb=psb, k2=k2, xt=xt):
                            last = None
                            for kk in range(2):
                                k = 2 * k2 + kk
                                for st in range(NTG):
                                    last = e.transpose(out=psb[:, kk * SG + st * 128:kk * SG + (st + 1) * 128], in_=xt[:, st, k * 128:(k + 1) * 128], identity=identB)
                            return last
                        add("pe", f_xet, reads=[xtn, "cb"], writes=[psn(b)])
                        for kk in range(2):
                            k = 2 * k2 + kk
                            add("act", lambda e, psb=psb, kk=kk, k=k, XT=XT: e.activation(out=XT[:, k, :], in_=psb[:, kk * SG:(kk + 1) * SG], func=AF.Identity,
                                                                                       scale=modcol[:, 2, k:k + 1], bias=modcol[:, 3, k:k + 1]),
                                reads=[psn(b)] + MODC, writes=[XTn])

            def stage_G(gi):
                ex, sgi = groups[gi]
                par = bufi(gi)
                XT, XTn, AT, ATn = XeT[par], "XeT%d" % par, actT[par], "actT%d" % par
                if sgi == 0:
                    if ex == 0:
                        add("pool", lambda e: e.dma_start(out=bd16[0][:], in_=bd_d[0:1, :]), writes=["bd16_0"], dma_key="bd16_0")
                    if ex + 1 < NE:
                        add("pool", lambda e, ex=ex: e.dma_start(out=bd16[(ex + 1) % 2][:], in_=bd_d[ex + 1:ex + 2, :]), writes=["bd16_%d" % ((ex + 1) % 2)], dma_key="bd16_%d" % ((ex + 1) % 2))
                    eprefetch(CPE * (ex - 1) + (CPE - 1) + NSL)
                    add("dve", lambda e, ex=ex: e.tensor_scalar(out=bdsc[ex % 2][0:1, :], in0=bd16[ex % 2][:], scalar1=1.702, scalar2=None, op0=ALU.mult),
                        reads=["bd16_%d" % (ex % 2)], writes=["bdsc_%d" % (ex % 2)])
                if True:
                    for fc in range(8):
                        res0, w0 = eget(CPE * ex + 0)
                        res1, w1 = eget(CPE * ex + 1)
                        wk = lambda k, col, w0=w0, w1=w1: (w0 if k < 4 else w1)[:, k % 4, col:col + 128]
                        bG = rot()
                        mm8(bG, PS[bG][:, 0:SG], lambda k, wk=wk, fc=fc: wk(k, fc * 128), lambda k, XT=XT: XT[:, k, :], [res0, res1, XTn])
                        bL = rot()
                        mm8(bL, PS[bL][:, 0:SG], lambda k, wk=wk, fc=fc: wk(k, 1024 + fc * 128), lambda k, XT=XT: XT[:, k, :], [res0, res1, XTn])
                        p2 = fc % 2
                        add("dve", lambda e, bG=bG, ex=ex, fc=fc, p2=p2: e.tensor_scalar(out=g1[p2][:], in0=PS[bG][:, 0:SG], scalar1=bgu[:, ex * 16 + fc:ex * 16 + fc + 1], scalar2=7.0, op0=ALU.add, op1=ALU.min),
                            reads=[psn(bG), "bgu"], writes=["g1_%d" % p2])
                        add("act", lambda e, p2=p2: e.activation(out=pp[p2][:], in_=g1[p2][:], func=AF.Silu, scale=1.702), reads=["g1_%d" % p2], writes=["pp_%d" % p2])
                        add("dve", lambda e, bL=bL, ex=ex, fc=fc, p2=p2: e.tensor_scalar(out=l2[p2][:], in0=PS[bL][:, 0:SG], scalar1=bgu[:, ex * 16 + 8 + fc:ex * 16 + 8 + fc + 1], scalar2=-6.0, op0=ALU.add, op1=ALU.max),
                            reads=[psn(bL), "bgu"], writes=["l2_%d" % p2])
                        add("dve", lambda e, p2=p2, fc=fc, AT=AT: e.scalar_tensor_tensor(out=AT[:, fc, :], in0=l2[p2][:], scalar=8.0, in1=pp[p2][:], op0=ALU.min, op1=ALU.mult),
                            reads=["l2_%d" % p2, "pp_%d" % p2], writes=[ATn])

            def stage_D(gi):
                ex, sgi = groups[gi]
                par = bufi(gi)
                AT, ATn = actT[par], "actT%d" % par
                ypar = 0 if sgi == 0 else 1
                yt, ytn = ye_tok[ypar], "ye_tok%d" % ypar
                r0 = ex * CAP + sgi * SG
                if True:
                    for hf in range(2):
                        resD, wD = eget(CPE * ex + 2)
                        for st in range(NTG):
                            b = rot()

                            def f_dn(e, b=b, wD=wD, st=st, ex=ex, hf=hf, AT=AT):
                                for k in range(8):
                                    e.matmul(PS[b][:], lhsT=AT[:, k, st * 128:(st + 1) * 128], rhs=wD[:, k, hf * 512:(hf + 1) * 512], start=(k == 0), stop=False)
                                return e.matmul(PS[b][:], lhsT=ones0B, rhs=bdsc[ex % 2][:, hf * 512:(hf + 1) * 512], start=False, stop=True)
                            add("pe", f_dn, reads=[resD, ATn, "cb", "bdsc_%d" % (ex % 2)], writes=[psn(b)])
                            add("dve", lambda e, b=b, st=st, hf=hf, yt=yt: e.tensor_tensor(out=yt[:, st, hf * 512:(hf + 1) * 512], in0=PS[b][:], in1=g2s[:, hf * 512:(hf + 1) * 512], op=ALU.mult),
                                reads=[psn(b), "g2s"], writes=[ytn])
                    add("sp", lambda e, r0=r0, yt=yt: e.dma_start(out=ypad_d[r0:r0 + SG, :].rearrange("(a p) n -> p a n", p=128), in_=yt[:]),
                        reads=[ytn], writes=["ypadS%d" % gi], dma_key="ypadS")

            g0 = lambda ex: ex * NG
            stage_load(g0(0))
            stage_load(g0(1))
            stage_T(g0(0))
            for ex in range(NE):
                stage_G(g0(ex))
                if ex + 1 < NE:
                    stage_T(g0(ex + 1))
                if ex + 2 < NE:
                    stage_load(g0(ex + 2))
                stage_load(g0(ex) + 1)
                stage_T(g0(ex) + 1)
                stage_D(g0(ex))
                for sgi in range(1, NG):
                    gi = g0(ex) + sgi
                    S.begin_cond(cnt_i[0:1, ex:ex + 1], sgi * SG)
                    if sgi > 1:
                        stage_load(gi)
                        stage_T(gi)
                    stage_G(gi)
                    stage_D(gi)
                for sgi in range(1, NG):
                    S.end_cond()
            S.drain_dmas("sp")
            S.emit_phase()
        YPADS = ["ypadS%d" % gi for gi in range(NE * NG)]

        with ExitStack() as ph:
            Yk = [[T(ph, "Yk%d_%d" % (s2, k), [128, D], BF16) for k in range(4)] for s2 in range(3)]
            x1r = [T(ph, "x1r%d" % i, [128, D], F32) for i in range(3)]
            acc = [T(ph, "acc%d" % i, [128, D], F32) for i in range(2)]
            ot = [T(ph, "ot%d" % i, [128, D], F32) for i in range(2)]
            ln2g_bc = T(ph, "ln2g_bc", [128, D], F32)
            ln2b_bc = T(ph, "ln2b_bc", [128, D], F32)
            add("sp", lambda e: e.dma_start(out=ln2g_bc[:], in_=ln2g_d.broadcast_to([128, D])), writes=["ln2g_bc"], dma_key="ln2g_bc")
            add("sp", lambda e: e.dma_start(out=ln2b_bc[:], in_=ln2b_d.broadcast_to([128, D])), writes=["ln2b_bc"], dma_key="ln2b_bc")
            dgk = [T(ph, "dgk%d" % i, [128, 4, 128], BF16) for i in range(2)]
            def d_fetch(i):
                s2 = i % 3
                for k in range(4):
                    add("pool", lambda e, i=i, k=k, s2=s2: e.indirect_dma_start(out=Yk[s2][k][:], out_offset=None, in_=ypad_d, in_offset=bass.IndirectOffsetOnAxis(ap=desti[:, i, k:k + 1], axis=0)),
                        reads=YPADS + ["desti%d" % i], writes=["Yk%d_%d" % (s2, k)], dma_key="Yk%d_%d" % (s2, k))
                add("sp", lambda e, i=i, s2=s2: e.dma_start(out=x1r[s2][:], in_=x1_d[i * 128:(i + 1) * 128, :]), reads=["x1d%d" % i], writes=["x1r%d" % s2], dma_key="x1r%d" % s2)

            lnst2 = [T(ph, "lnst2_%d" % i, [128, 2, 6], F32) for i in range(2)]
            lnmv2 = [T(ph, "lnmv2_%d" % i, [128, 4], F32) for i in range(2)]

            cmb_banks = {}

            def D_A1(i):
                s2, p2 = i % 3, i % 2
                for k in range(4):
                    add("dve", lambda e, k=k: e.tensor_scalar(out=dgk[p2][:, k, :], in0=identB, scalar1=gates[:, i, k:k + 1], scalar2=None, op0=ALU.mult),
                        reads=["cb", "gates%d" % i], writes=["dgk%d" % p2])
                cmb_banks[i] = []
                for hf in range(2):
                    b = rot()
                    cmb_banks[i].append(b)

                    def f_cmb(e, b=b, hf=hf):
                        last = None
                        for k in range(4):
                            last = e.matmul(PS[b][:], lhsT=dgk[p2][:, k, :], rhs=Yk[s2][k][:, hf * 512:(hf + 1) * 512], start=(k == 0), stop=(k == 3))
                        return last
                    add("pe", f_cmb, reads=["dgk%d" % p2] + ["Yk%d_%d" % (s2, k) for k in range(4)], writes=[psn(b)])

            def D_A2(i):
                s2, p2 = i % 3, i % 2
                a_, an = acc[p2], "acc%d" % p2
                st_, stn, mv_, mvn = lnst2[p2], "lnst2_%d" % p2, lnmv2[p2], "lnmv2_%d" % p2
                for hf in range(2):
                    b = cmb_banks[i][hf]
                    add("dve", lambda e, b=b, hf=hf: e.scalar_tensor_tensor(out=a_[:, hf * 512:(hf + 1) * 512], in0=x1r[s2][:, hf * 512:(hf + 1) * 512], scalar=ALPHA,
                                                                         in1=PS[b][:], op0=ALU.mult, op1=ALU.add),
                        reads=["x1r%d" % s2, psn(b)], writes=[an])
                for hf in range(2):
                    add("dve", lambda e, hf=hf: e.bn_stats(out=st_[:, hf, :], in_=a_[:, hf * 512:(hf + 1) * 512]), reads=[an], writes=[stn])
                add("dve", lambda e: e.bn_aggr(out=mv_[:, 0:2], in_=st_[:].rearrange("p a b -> p (a b)")), reads=[stn], writes=[mvn])

            def D_B1(i):
                p2 = i % 2
                a_, an = acc[p2], "acc%d" % p2
                mv_, mvn = lnmv2[p2], "lnmv2_%d" % p2
                o_, on = ot[p2], "ot%d" % p2
                add("dve", lambda e: e.tensor_scalar(out=mv_[:, 2:3], in0=mv_[:, 1:2], scalar1=EPS, scalar2=None, op0=ALU.add), reads=[mvn], writes=[mvn])
                add("act", lambda e: e.activation(out=mv_[:, 2:3], in_=mv_[:, 2:3], func=AF.Sqrt), reads=[mvn], writes=[mvn])
                add("dve", lambda e: e.reciprocal(out=mv_[:, 2:3], in_=mv_[:, 2:3]), reads=[mvn], writes=[mvn])
                add("dve", lambda e: e.scalar_tensor_tensor(out=mv_[:, 3:4], in0=mv_[:, 0:1], scalar=-1.0, in1=mv_[:, 2:3], op0=ALU.mult, op1=ALU.mult), reads=[mvn], writes=[mvn])
                add("act", lambda e: e.activation(out=o_[:], in_=a_[:], func=AF.Identity, scale=mv_[:, 2:3], bias=mv_[:, 3:4]), reads=[an, mvn], writes=[on])

            def D_B2(i):
                p2 = i % 2
                o_, on = ot[p2], "ot%d" % p2
                add("dve", lambda e: e.tensor_tensor(out=o_[:], in0=o_[:], in1=ln2g_bc[:], op=ALU.mult), reads=[on, "ln2g_bc"], writes=[on])
                add("dve", lambda e: e.tensor_tensor(out=o_[:], in0=o_[:], in1=ln2b_bc[:], op=ALU.add), reads=[on, "ln2b_bc"], writes=[on])
                add("sp", lambda e: e.dma_start(out=out_d[i * 128:(i + 1) * 128, :], in_=o_[:]), reads=[on], writes=["outd%d" % i], dma_key="outd")

            d_fetch(0)
            d_fetch(1)
            D_A1(0)
            D_A2(0)
            for i in range(NT):
                if i + 2 < NT:
                    d_fetch(i + 2)
                if i + 1 < NT:
                    D_A1(i + 1)
                D_B1(i)
                if i + 1 < NT:
                    D_A2(i + 1)
                D_B2(i)
            S.drain_dmas("sp")
            S.emit_phase()
    return nc, dbg_out


def make_in_maps(inputs, cores=range(8)):
    f = lambda a: np.ascontiguousarray(np.asarray(a, dtype=np.float32))
    consts, constsb, alL, alR = make_consts()
    shared = {
        "w_ada": f(inputs["w_ada"][0]),
        "b_ada": f(inputs["b_ada"][0]).reshape(1, -1),
        "w_in": f(inputs["w_in"][0]),
        "kv_norm_g": f(inputs["kv_norm_g"][0]).reshape(1, -1),
        "w_uk": f(inputs["w_uk"][0]).reshape(128, 512),
        "w_uv": f(inputs["w_uv"][0]).reshape(128, 512),
        "w_pool": f(inputs["w_pool_group"][0]),
        "pool_scale_col": f(np.asarray(inputs["pool_scale"][0]).reshape(4, 128).T),
        "w_ba": f(inputs["w_branch_attn"][0]),
        "w_bp": f(inputs["w_branch_pool"][0]),
        "w_out": f(inputs["w_out"][0]),
        "ln1_g": f(inputs["ln1_g"][0]).reshape(1, -1),
        "ln1_b": f(inputs["ln1_b"][0]).reshape(1, -1),
        "w_router": f(inputs["w_router"][0]),
        "b_router": f(inputs["b_router"][0]).reshape(1, -1),
        "w_gate_up": f(inputs["w_gate_up"][0]),
        "b_gu_col": f(np.asarray(inputs["b_gate_up"][0]).reshape(NE, 16, 128).transpose(2, 0, 1).reshape(128, NE * 16)),
        "w_down": f(inputs["w_down"][0]),
        "b_down": f(inputs["b_down"][0]),
        "ln2_g": f(inputs["ln2_g"][0]).reshape(1, -1),
        "ln2_b": f(inputs["ln2_b"][0]).reshape(1, -1),
        "consts": consts, "constsb": constsb, "alibiL": alL, "alibiR": alR,
    }
    maps = []
    for b in cores:
        m = dict(shared)
        m["x"] = f(inputs["x"][b])
        m["c_col"] = f(np.asarray(inputs["c"][b]).reshape(8, 128).T)
        maps.append(m)
    return maps


def kernel(**inputs):
    nc, _ = build_nc()
    maps = make_in_maps(inputs)
    res = run_bass_kernel_spmd(nc, maps, core_ids=list(range(8)))
    out = np.stack([np.asarray(r["out"], dtype=np.float32) for r in res.results], axis=0)
    return out
```

```python
import numpy as np
from contextlib import ExitStack
import concourse.bass as bass
import concourse.mybir as mybir
from concourse.bass_utils import run_bass_kernel_spmd

F32 = mybir.dt.float32
BF16 = mybir.dt.bfloat16
U32 = mybir.dt.uint32
AF = mybir.ActivationFunctionType
ALU = mybir.AluOpType
AX = mybir.AxisListType

S_LEN = 2048
D = 1024
NT = 16
CAP = 1024
SG = 256
NG = CAP // SG
NE = 32
KBIS = 12
ALPHA = 2.0 ** 0.25
EPS = 1e-5
NEG = -1.0e30
XPAD_ROWS = NE * CAP + 2048

C_ID, C_CM, C_POW, C_EB = 0, 128, 256, 272
NCONST = 304
B_ID, B_TRI, B_ONE, B_BAND = 0, 128, 256, 384
B_ONE0 = 384 + 12 * 128
NB16 = B_ONE0 + 128

ENGS = ("pe", "act", "dve", "pool", "sp")


class Sched:
    def __init__(self, nc, stack, epoch=4000):
        self.nc = nc
        self.stack = stack
        self.epoch = epoch
        self.ops = {e: [] for e in ENGS}
        self.cnt = {e: 0 for e in ENGS}
        self.last_w = {}
        self.readers = {}
        self.dma_cnt = {}
        self.seen = {e: {} for e in ENGS}
        self.sems = {}
        self.nblock = 0
        self.alias = {}
        self.cond = ()
        self.cond_seen = []

    def _sem(self, sk):
        if sk not in self.sems:
            self.sems[sk] = self.stack.enter_context(self.nc.semaphore("sm%d" % len(self.sems)))
        return self.sems[sk]

    def _tok2sem(self, tok):
        if tok[0] == "eng":
            _, e, idx = tok
            ep = idx // self.epoch
            return ("eng", e, ep), idx - ep * self.epoch + 1
        _, key, n = tok
        return ("dma", key), 16 * n

    def add(self, eng, fn, reads=(), writes=(), dma_key=None, pe_chain=False, ndma=1):
        reads = tuple(x for r in reads for x in self.alias.get(r, (r,)))
        writes = tuple(x for w in writes for x in self.alias.get(w, (w,)))
        if eng == "pe":
            pe_chain = True
        deps = []
        for r in reads:
            t = self.last_w.get(r)
            if t is not None:
                deps.append(t)
        for w in writes:
            t = self.last_w.get(w)
            if t is not None:
                deps.append(t)
            deps.extend(self.readers.get(w, ()))
        waits = {}
        seen = self.seen[eng] if not self.cond else self.cond_seen[-1][eng]
        for tok in deps:
            if tok[0] == "eng" and tok[1] == eng and pe_chain:
                continue
            sk, v = self._tok2sem(tok)
            if seen.get(sk, 0) >= v:
                continue
            waits[sk] = max(waits.get(sk, 0), v)
        for sk, v in waits.items():
            seen[sk] = v
        if dma_key is not None:
            n = self.dma_cnt.get(dma_key, 0) + ndma
            self.dma_cnt[dma_key] = n
            tok = ("dma", dma_key, n)
            inc = (("dma", dma_key), 16)
        else:
            idx = self.cnt[eng]
            self.cnt[eng] = idx + 1
            tok = ("eng", eng, idx)
            inc = (("eng", eng, idx // self.epoch), 1)
        self.ops[eng].append(dict(fn=fn, waits=list(waits.items()), inc=inc, cond=self.cond,
                                  nd=(ndma if dma_key is not None else 1)))
        for r in reads:
            self.readers.setdefault(r, []).append(tok)
        for w in writes:
            self.last_w[w] = tok
            self.readers[w] = []
        return tok

    def begin_cond(self, cnt_ap, thresh):
        base = self.cond_seen[-1] if self.cond else self.seen
        self.cond = self.cond + ((cnt_ap, thresh),)
        self.cond_seen.append({e: dict(base[e]) for e in ENGS})

    def end_cond(self):
        self.cond = self.cond[:-1]
        self.cond_seen.pop()

    def drain_dmas(self, eng="sp"):
        waits = []
        for key, n in self.dma_cnt.items():
            sk = ("dma", key)
            if self.seen[eng].get(sk, 0) >= 16 * n:
                continue
            self.seen[eng][sk] = 16 * n
            waits.append((sk, 16 * n))
        self.ops[eng].append(dict(fn=None, waits=waits, inc=None, cond=(), nd=1))

    def emit_phase(self):
        nc = self.nc
        for e in ENGS:
            for op in self.ops[e]:
                for sk, _ in op["waits"]:
                    self._sem(sk)
                if op["inc"] is not None:
                    self._sem(op["inc"][0])
        sems = self.sems
        all_ops = self.ops
        self.ops = {e: [] for e in ENGS}
        self.nblock += 1
        with nc.Block() as block:
            engmap = {"pe": block.tensor, "act": block.scalar, "dve": block.vector,
                      "pool": block.gpsimd, "sp": block.sync}

            def run_op(e, op):
                for sk, v in op["waits"]:
                    e.wait_ge(sems[sk], v)
                if op["fn"] is not None:
                    inst = op["fn"](e)
                    sk, n = op["inc"]
                    if isinstance(inst, (list, tuple)):
                        for ii in inst:
                            ii.then_inc(sems[sk], n)
                    else:
                        inst.then_inc(sems[sk], n)

            def emit_ops(e, ops, depth):
                i = 0
                while i < len(ops):
                    op = ops[i]
                    if len(op["cond"]) <= depth:
                        run_op(e, op)
                        i += 1
                        continue
                    tag = op["cond"][depth]
                    j = i
                    while j < len(ops) and len(ops[j]["cond"]) > depth and ops[j]["cond"][depth] is tag:
                        j += 1
                    grp = ops[i:j]
                    cnt_ap, thresh = tag
                    self._nreg = getattr(self, "_nreg", 0) + 1
                    creg = e.alloc_register("condreg%d" % self._nreg)
                    e.reg_load(creg, cnt_ap)
                    with e.If_cmp(creg, thresh, comp_op="IS_GT"):
                        emit_ops(e, grp, depth + 1)
                    with e.Else():
                        incs = {}
                        for o2 in grp:
                            if o2["inc"] is None:
                                continue
                            sk, n = o2["inc"]
                            incs[sk] = incs.get(sk, 0) + n * o2["nd"]
                        for sk, tot in incs.items():
                            if sk[0] == "dma":
                                e.sem_inc(sems[sk], tot)
                            else:
                                e.drain().then_inc(sems[sk], tot)
                    e.free_register(creg)
                    i = j

            def mk(ops):
                def body(e):
                    emit_ops(e, ops, 0)
                return body

            for ename in ENGS:
                if all_ops[ename]:
                    engmap[ename](mk(all_ops[ename]))


def make_consts():
    c = np.zeros((128, NCONST), np.float32)
    cbs = np.zeros((128, NB16), np.float32)
    ar = np.arange(128)
    c[:, C_ID:C_ID + 128] = np.eye(128)
    cbs[:, B_ID:B_ID + 128] = np.eye(128)
    cbs[:, B_TRI:B_TRI + 128] = (ar[:, None] < ar[None, :])
    cbs[:, B_ONE:B_ONE + 128] = 1.0
    cbs[0, B_ONE0:B_ONE0 + 128] = 1.0
    c[:, C_CM:C_CM + 128] = np.where(ar[None, :] <= ar[:, None], 0.0, NEG)
    for wg, w in enumerate((2, 4, 8, 16)):
        tp = ar[:, None]
        t = ar[None, :]
        dd = t - tp
        main = np.where((dd >= 0) & (dd < w), 1.0 / w, 0.0) - (dd == 0)
        prev = np.where((t + 128 - tp) < w, 1.0 / w, 0.0)
        cntf = np.minimum(w, t + 1).astype(np.float64)
        first = np.where((dd >= 0) & (dd < w), 1.0 / cntf, 0.0) - (dd == 0)
        for v, m in enumerate((main, prev, first)):
            o = B_BAND + 128 * (3 * wg + v)
            cbs[:, o:o + 128] = m
    c[:, C_POW:C_POW + 16] = 2.0 ** (-np.arange(16))[None, :]
    c[:, C_EB:C_EB + 32] = (np.arange(32) * CAP)[None, :]
    slopes = 2.0 ** (-8.0 * np.arange(1, 9) / 8)
    al = np.zeros((3, 16, 128), np.float32)
    al[0] = ar[None, :]
    al[1] = (-128.0 * np.arange(16))[:, None]
    al[2] = 1.0
    arr = np.zeros((3, 8, 128), np.float32)
    arr[0] = slopes[:, None]
    arr[1] = slopes[:, None]
    arr[2] = -slopes[:, None] * ar[None, :]
    return c, cbs, al.reshape(3, 2048), arr.reshape(3, 1024)


def build_nc(dbg=(), stop_after=None):
    nc = bass.Bass("TRN2", target_bir_lowering=False)
    dt = lambda name, shape, dty=F32: nc.dram_tensor(name, list(shape), dty, kind="ExternalInput").ap()
    x_d = dt("x", [S_LEN, D])
    ccol_d = dt("c_col", [128, 8])
    wada_d = dt("w_ada", [D, 6 * D])
    bada_d = dt("b_ada", [1, 6 * D])
    win_d = dt("w_in", [D, 3784])
    kvg_d = dt("kv_norm_g", [1, 128])
    wuk_d = dt("w_uk", [128, 512])
    wuv_d = dt("w_uv", [128, 512])
    wpool_d = dt("w_pool", [4, 128, 128])
    pscale_d = dt("pool_scale_col", [128, 4])
    wba_d = dt("w_ba", [512, D])
    wbp_d = dt("w_bp", [512, D])
    wout_d = dt("w_out", [D, D])
    ln1g_d = dt("ln1_g", [1, D])
    ln1b_d = dt("ln1_b", [1, D])
    wr_d = dt("w_router", [D, NE])
    br_d = dt("b_router", [1, NE])
    wgu_d = dt("w_gate_up", [NE, D, 2 * D])
    bgu_d = dt("b_gu_col", [128, NE * 16])
    wd_d = dt("w_down", [NE, D, D])
    bd_d = dt("b_down", [NE, D])
    ln2g_d = dt("ln2_g", [1, D])
    ln2b_d = dt("ln2_b", [1, D])
    consts_d = dt("consts", [128, NCONST])
    constsb_d = dt("constsb", [128, NB16])
    alL_d = dt("alibiL", [3, 2048])
    alR_d = dt("alibiR", [3, 1024])
    out_d = nc.dram_tensor("out", [S_LEN, D], F32, kind="ExternalOutput").ap()
    x1_d = nc.dram_tensor("x1_scr", [S_LEN, D], F32, kind="Internal").ap()
    xpad_d = nc.dram_tensor("xpad_scr", [XPAD_ROWS, D], BF16, kind="Internal").ap()
    ypad_d = nc.dram_tensor("ypad_scr", [XPAD_ROWS, D], BF16, kind="Internal").ap()
    dbg_out = {}

    with ExitStack() as top:
        S = Sched(nc, top)
        S.alias = {"score0": ("score0a", "score0b"), "score1": ("score1a", "score1b")}
        add = S.add
        PS = [top.enter_context(nc.psum_tensor("psb%d" % i, [128, 512], F32)) for i in range(8)]
        psn = lambda b: "ps%d" % b
        rot_state = [0]

        def rot():
            b = rot_state[0] % 8
            rot_state[0] += 1
            return b

        def T(stack, name, shape, dty):
            return stack.enter_context(nc.sbuf_tensor(name, list(shape), dty))

        def dump(name, ap, shape, dty, reads):
            if name not in dbg:
                return
            o = nc.dram_tensor("dbg_" + name, list(shape), dty, kind="ExternalOutput").ap()
            dbg_out[name] = o
            add("sp", lambda e: e.dma_start(out=o, in_=ap), reads=reads, writes=["dbg_" + name], dma_key="dbg_" + name)

        cst = T(top, "cst", [128, NCONST], F32)
        cb = T(top, "cb", [128, NB16], BF16)
        modcol = T(top, "modcol", [128, 4, 8], F32)
        gate_bc = T(top, "gate_bc", [128, 2, D], F32)
        gates = T(top, "gates", [128, NT, 4], F32)
        desti = T(top, "desti", [128, NT, 4], U32)
        cnt_f = T(top, "cnt_f", [1, NE], F32)
        cnt_i = T(top, "cnt_i", [1, NE], mybir.dt.int32)
        identF = cst[:, C_ID:C_ID + 128]
        identB = cb[:, B_ID:B_ID + 128]
        triB = cb[:, B_TRI:B_TRI + 128]
        onesB = cb[:, B_ONE:B_ONE + 128]
        ones0B = cb[:, B_ONE0:B_ONE0 + 128]
        cmaskF = cst[:, C_CM:C_CM + 128]
        band = lambda wg, v: cb[:, B_BAND + 128 * (3 * wg + v):B_BAND + 128 * (3 * wg + v) + 128]

        add("sp", lambda e: e.dma_start(out=cst[:], in_=consts_d), writes=["cst"], dma_key="cst")
        add("pool", lambda e: e.dma_start(out=cb[:], in_=constsb_d), writes=["cb"], dma_key="cb")

        with ExitStack() as ph:
            NSLOT = 4
            ring = [T(ph, "wr%d" % i, [128, 4096], BF16) for i in range(NSLOT)]
            loads = []
            wada_v = wada_d.rearrange("(c p) n -> p c n", p=128)
            ada_load = lambda j: (wada_v[:, :, 512 * j:512 * (j + 1)], 8, 512)
            for j in range(4):
                loads.append(ada_load(j))
            win_v = win_d.rearrange("(c p) n -> p c n", p=128)
            wba_v = wba_d.rearrange("(c p) n -> p c n", p=128)
            wbp_v = wbp_d.rearrange("(c p) n -> p c n", p=128)
            wout_v = wout_d.rearrange("(c p) n -> p c n", p=128)
            GCH = [(0, 512), (512, 1024), (1024, 1224), (1224, 1736),
                   (1736, 2248), (2248, 2760), (2760, 3272), (3272, 3784)]
            for g in range(4):
                for (a, b) in GCH:
                    loads.append((win_v[:, :, a:b], 8, b - a))
                if g == 0:
                    for j in range(4, 12):
                        loads.append(ada_load(j))
                loads.append((wba_v, 4, 1024))
                loads.append((wbp_v, 4, 1024))
                loads.append((wout_v[:, :, 0:512], 8, 512))
                loads.append((wout_v[:, :, 512:1024], 8, 512))
            issued = [0]
            LOOK = 2

            def wget(n):
                while issued[0] <= min(n + LOOK, len(loads) - 1):
                    k = issued[0]
                    src, c_, n_ = loads[k]
                    slot = ring[k % NSLOT]
                    dst = slot[:, 0:c_ * n_].rearrange("p (c n) -> p c n", c=c_)
                    add("pool", lambda e, dst=dst, src=src: e.dma_start(out=dst, in_=src),
                        writes=["wr%d" % (k % NSLOT)], dma_key="wr%d" % (k % NSLOT))
                    issued[0] += 1
                src, c_, n_ = loads[n]
                return "wr%d" % (n % NSLOT), ring[n % NSLOT][:, 0:c_ * n_].rearrange("p (c n) -> p c n", c=c_)

            x1b = T(ph, "x1b", [128, D], BF16)
            wuk_n = x1b
            wukT2 = T(ph, "wukT2", [128, 4, 128], BF16)
            wuv = T(ph, "wuv", [128, 512], BF16)
            wpool = T(ph, "wpool", [128, 4, 128], BF16)
            pscale = T(ph, "pscale", [128, 4], F32)
            kvg_bc = T(ph, "kvg_bc", [128, 128], F32)
            ln1g_bc = T(ph, "ln1g_bc", [128, D], F32)
            ln1b_bc = T(ph, "ln1b_bc", [128, D], F32)
            wrt = T(ph, "wrt", [128, 8, NE], F32)
            br_bc = T(ph, "br_bc", [128, NE], F32)
            alL = T(ph, "alL", [128, 2048], BF16)
            alR = T(ph, "alR", [128, 1024], BF16)
            Mall = T(ph, "Mall", [128, NT, NE], BF16)
            NM = T(ph, "NM", [128, S_LEN], BF16)
            mixT = T(ph, "mixT", [128, 8, 512], BF16)
            add("pool", lambda e: e.memset(mixT[:, 0:2, :], 0.0), writes=["mixT"])
            xp = T(ph, "xp", [128, D], F32)
            xr = T(ph, "xr", [128, D], F32)
            diag = [T(ph, "diag%d" % i, [128, 8, 128], BF16) for i in range(2)]

            add("pool", lambda e: e.dma_start(out=wuk_n[:, 0:512], in_=wuk_d), writes=["x1b"], dma_key="wuk_n")
            add("pool", lambda e: e.dma_start(out=wuv[:], in_=wuv_d), writes=["wuv"], dma_key="wuv")
            add("pool", lambda e: e.dma_start(out=wpool[:], in_=wpool_d.rearrange("g c d -> c g d")), writes=["wpool"], dma_key="wpool")
            add("sp", lambda e: e.dma_start(out=pscale[:], in_=pscale_d), writes=["pscale"], dma_key="pscale")
            add("sp", lambda e: e.dma_start(out=kvg_bc[:], in_=kvg_d.broadcast_to([128, 128])), writes=["kvg_bc"], dma_key="kvg_bc")
            add("sp", lambda e: e.dma_start(out=ln1g_bc[:], in_=ln1g_d.broadcast_to([128, D])), writes=["ln1g_bc"], dma_key="ln1g_bc")
            add("sp", lambda e: e.dma_start(out=ln1b_bc[:], in_=ln1b_d.broadcast_to([128, D])), writes=["ln1b_bc"], dma_key="ln1b_bc")
            add("sp", lambda e: e.dma_start(out=wrt[:], in_=wr_d.rearrange("(c p) n -> p c n", p=128)), writes=["wrt"], dma_key="wrt")
            add("sp", lambda e: e.dma_start(out=br_bc[:], in_=br_d.broadcast_to([128, NE])), writes=["br_bc"], dma_key="br_bc")
            add("pool", lambda e: e.memset(alL[:], 0.0), writes=["alL"])
            add("pool", lambda e: e.memset(alR[:], 0.0), writes=["alR"])
            add("pool", lambda e: e.dma_start(out=alL[0:3, :], in_=alL_d), writes=["alL"], dma_key="alL")
            add("pool", lambda e: e.dma_start(out=alR[0:3, :], in_=alR_d), writes=["alR"], dma_key="alR")
            zf_list = list(range(0, XPAD_ROWS, 2048))

            def zero_fill(nmax):
                for _ in range(nmax):
                    if not zf_list:
                        return
                    r0 = zf_list.pop(0)
                    nr = min(2048, XPAD_ROWS - r0)
                    add("sp", lambda e, r0=r0, nr=nr: e.dma_start(
                        out=xpad_d[r0:r0 + nr, :].rearrange("(p a) n -> p a n", p=128),
                        in_=mixT[:, 0:2, :].rearrange("p a b -> p (a b)").unsqueeze(1).broadcast_to([128, nr // 128, 1024])),
                        reads=["mixT"], writes=["xpadZ%d" % r0], dma_key="xpadZ")
            xpadZ = ["xpadZ%d" % r0 for r0 in range(0, XPAD_ROWS, 2048)]

            b = rot()
            psb = PS[b][:].bitcast(BF16)

            def f_wukT(e, psb=psb):
                last = None
                for pr in range(4):
                    last = e.transpose(out=psb[:, pr * 128:(pr + 1) * 128], in_=wuk_n[:, pr * 128:(pr + 1) * 128], identity=identB)
                return last
            add("pe", f_wukT, reads=["x1b", "cb"], writes=[psn(b)])
            add("dve", lambda e, psb=psb: e.tensor_copy(out=wukT2[:].rearrange("p a b -> p (a b)"), in_=psb[:, 0:512]), reads=[psn(b)], writes=["wukT2"])

            c_sb = T(ph, "c_sb", [128, 8], F32)
            c_si = T(ph, "c_si", [128, 8], BF16)
            olatn = T(ph, "olatn", [128, 8, 128], BF16)
            condB = olatn
            bb = [xp[:, 0:512], xp[:, 512:1024]]
            modtmp = [xr[:, 0:512], xr[:, 512:1024]]
            add("sp", lambda e: e.dma_start(out=c_sb[:], in_=ccol_d), writes=["c_sb"], dma_key="c_sb")
            add("act", lambda e: e.activation(out=c_si[:], in_=c_sb[:], func=AF.Silu), reads=["c_sb"], writes=["c_si"])
            add("dve", lambda e: e.tensor_copy(out=condB[:], in_=c_si[:].unsqueeze(2).broadcast_to([128, 8, 128])), reads=["c_si"], writes=["olatn"])
            VEC = {0: ("col", 1, 0.0), 1: ("col", 0, 1.0), 2: ("gate", 0, 0.0), 3: ("col", 3, 0.0), 4: ("col", 2, 1.0), 5: ("gate", 1, 0.0)}
            def ada_chunk(j):
                v, half = j // 2, j % 2
                wres, wv = wget(j if j < 4 else j + 8)
                add("act", lambda e, j=j: e.dma_start(out=bb[j % 2], in_=bada_d[:, 512 * j:512 * (j + 1)].broadcast_to([128, 512])),
                    writes=["xp%d" % (j % 2)], dma_key="xp%d" % (j % 2))
                b = rot()

                def f_mod(e, b=b, wv=wv):
                    last = None
                    for k in range(8):
                        last = e.matmul(PS[b][:], lhsT=condB[:, k, :], rhs=wv[:, k, :], start=(k == 0), stop=(k == 7))
                    return last
                add("pe", f_mod, reads=["olatn", wres], writes=[psn(b)])
                kind, idx, addc = VEC[v]
                if kind == "gate":
                    add("dve", lambda e, b=b, j=j, idx=idx, half=half: e.tensor_tensor(
                        out=gate_bc[:, idx, half * 512:(half + 1) * 512], in0=PS[b][:], in1=bb[j % 2], op=ALU.add),
                        reads=[psn(b), "xp%d" % (j % 2)], writes=["gate_bc%d_%d" % (idx, half)])
                else:
                    mt = modtmp[j % 2]
                    add("dve", lambda e, b=b, j=j, mt=mt: e.tensor_tensor(out=mt, in0=PS[b][:], in1=bb[j % 2], op=ALU.add),
                        reads=[psn(b), "xp%d" % (j % 2)], writes=["xr"])
                    b2 = rot()

                    def f_tr(e, b2=b2, mt=mt):
                        last = None
                        for q4 in range(4):
                            last = e.transpose(out=PS[b2][:, q4 * 128:(q4 + 1) * 128], in_=mt[:, q4 * 128:(q4 + 1) * 128], identity=identF)
                        return last
                    add("pe", f_tr, reads=["xr", "cst"], writes=[psn(b2)])
                    add("dve", lambda e, b2=b2, idx=idx, half=half, addc=addc: e.tensor_scalar(
                        out=modcol[:, idx, 4 * half:4 * half + 4], in0=PS[b2][:].rearrange("p (a b) -> p a b", a=4)[:, :, 0],
                        scalar1=addc, scalar2=None, op0=ALU.add),
                        reads=[psn(b2)], writes=["modcol%d_%d" % (idx, half)])
            for j in range(4):
                ada_chunk(j)
            MODC = ["modcol%d_%d" % (i, h) for i in range(4) for h in range(2)]
            MODC1 = ["modcol%d_%d" % (i, h) for i in range(2) for h in range(2)]
            GATE1 = ["gate_bc0_0", "gate_bc0_1"]
            GATE2 = ["gate_bc1_0", "gate_bc1_1"]
            dump("modcol", modcol[:], [128, 4, 8], F32, MODC)
            dump("gate_bc", gate_bc[:], [128, 2, D], F32, GATE1 + GATE2)

            if stop_after == "A":
                S.drain_dmas("sp")
                S.emit_phase()
                return nc, dbg_out

            xs = [xr, xp]
            hT = T(ph, "hT", [128, 8, 512], BF16)
            qT = T(ph, "qT", [128, 4, 512], BF16)
            qlatT = T(ph, "qlatT", [128, 8, 512], BF16)
            qiT = T(ph, "qiT", [128, 4, 512], BF16)
            kiT2 = T(ph, "kiT2", [128, 2, S_LEN], BF16)
            ckv_tok = T(ph, "ckv_tok", [128, NT, 128], BF16)
            ckvT = T(ph, "ckvT", [128, S_LEN], BF16)
            wi_tok = T(ph, "wi_tok", [128, 4, 8], F32)
            u_buf = T(ph, "u_buf", [128, 5, 512], BF16)
            pooledT = T(ph, "pooledT", [128, 4, 512], BF16)
            ypoolT = T(ph, "ypoolT", [128, 4, 512], BF16)
            yattnT = T(ph, "yattnT", [128, 4, 512], BF16)
            sgT = T(ph, "sgT", [128, 16, 512], BF16)
            ssq = T(ph, "ssq", [128, 4], F32)
            rstd4 = T(ph, "rstd4", [128, 4], F32)
            sqj = T(ph, "sqj", [128, 128], BF16)
            score = [T(ph, "score%d" % i, [128, S_LEN], F32) for i in range(2)]
            NMT = T(ph, "NMT", [128, NT, 128], BF16)
            Rb = [T(ph, "Rb%d" % i, [128, 512], BF16) for i in range(3)]
            PT = [T(ph, "PT%d" % i, [128, 512], BF16) for i in range(2)]
            rden = T(ph, "rden", [128, 512], F32)
            bis = T(ph, "bis", [128, 8], F32)
            wtab = T(ph, "wtab", [128, 16], F32)
            amax = [T(ph, "amax%d" % i, [128, 4], F32) for i in range(2)]
            t1 = T(ph, "t1", [128, 512], F32)
            t2 = T(ph, "t2", [128, 512], F32)
            x1t = T(ph, "x1t", [128, D], F32)
            h2T = T(ph, "h2T", [128, 8, 128], F32)
            lnst = T(ph, "lnst", [128, 2, 6], F32)
            lnmv = T(ph, "lnmv", [128, 4], F32)
            lg = T(ph, "lg", [128, NE], F32)
            top8 = T(ph, "top8", [128, 8], F32)
            rsm = T(ph, "rsm", [128, 8], F32)
            e4 = T(ph, "e4", [128, 4], F32)
            destf = T(ph, "destf", [128, NE], F32)
            destk = T(ph, "destk", [128, 4], F32)
            junk32 = T(ph, "junk32", [128, NE], F32)

            evac_flip = [0]

            def evac(out_ap, in_ap, reads, writes, eng=None):
                if eng is None:
                    eng = "act" if evac_flip[0] % 2 == 0 else "dve"
                    evac_flip[0] += 1
                if eng == "act":
                    add("act", lambda e: e.activation(out=out_ap, in_=in_ap, func=AF.Identity), reads=reads, writes=writes)
                else:
                    add("dve", lambda e: e.tensor_copy(out=out_ap, in_=in_ap), reads=reads, writes=writes)

            HT = ["hT0", "hT1", "hT2", "hT3"]

            def mm8(b, out_ap, lhs_fn, rhs_fn, reads, nk=8):
                def f(e):
                    last = None
                    for k in range(nk):
                        last = e.matmul(out_ap, lhsT=lhs_fn(k), rhs=rhs_fn(k), start=(k == 0), stop=(k == nk - 1))
                    return last
                add("pe", f, reads=reads, writes=[psn(b)])

            CH0 = 12
            for g in range(4):
                cbase = 4 if g == 0 else 24 + 12 * (g - 1)
                cb8 = cbase + (16 if g == 0 else 8)
                for t in range(4):
                    i = 4 * g + t
                    xb_ = xs[i % 2]
                    xsn = ["xr"] if i % 2 == 0 else ["xp0", "xp1"]
                    add("sp", lambda e, i=i, xb_=xb_: e.dma_start(out=xb_[:], in_=x_d[i * 128:(i + 1) * 128, :]),
                        writes=xsn, dma_key="xs%d" % (i % 2))
                    for hf in range(2):
                        b = rot()

                        def f_xt(e, b=b, xb_=xb_, hf=hf):
                            last = None
                            for c4 in range(4):
                                c = 4 * hf + c4
                                last = e.transpose(out=PS[b][:, c4 * 128:(c4 + 1) * 128], in_=xb_[:, c * 128:(c + 1) * 128], identity=identF)
                            return last
                        add("pe", f_xt, reads=xsn + ["cst"], writes=[psn(b)])
                        for c4 in range(4):
                            c = 4 * hf + c4
                            if c4 % 2 == 0:
                                add("act", lambda e, b=b, c=c, c4=c4, t=t: e.activation(
                                    out=hT[:, c, t * 128:(t + 1) * 128], in_=PS[b][:, c4 * 128:(c4 + 1) * 128], func=AF.Identity,
                                    scale=modcol[:, 0, c:c + 1], bias=modcol[:, 1, c:c + 1]),
                                    reads=[psn(b)] + MODC1, writes=["hT%d" % t])
                            else:
                                add("dve", lambda e, b=b, c=c, c4=c4, t=t: e.tensor_scalar(
                                    out=hT[:, c, t * 128:(t + 1) * 128], in0=PS[b][:, c4 * 128:(c4 + 1) * 128],
                                    scalar1=modcol[:, 0, c:c + 1], scalar2=modcol[:, 1, c:c + 1], op0=ALU.mult, op1=ALU.add),
                                    reads=[psn(b)] + MODC1, writes=["hT%d" % t])
                if g == 0:
                    zero_fill(100)
                    dump("hT", hT[:], [128, 8, 512], BF16, HT)

                wres, wv = wget(cbase + 0)
                for pr in range(4):
                    b = rot()
                    mm8(b, PS[b][:], lambda k, wv=wv, pr=pr: wv[:, k, pr * 128:(pr + 1) * 128], lambda k: hT[:, k, :], [wres] + HT)
                    evac(qT[:, pr, :], PS[b][:], [psn(b)], ["qT"])
                for h in range(8):
                    pr, hb = h // 2, (h % 2) * 64
                    b = rot()
                    add("pe", lambda e, b=b, pr=pr, hb=hb: e.matmul(PS[b][:], lhsT=wukT2[hb:hb + 64, pr, :], rhs=qT[hb:hb + 64, pr, :], start=True, stop=True),
                        reads=["wukT2", "qT"], writes=[psn(b)])
                    if h % 2 == 0:
                        add("act", lambda e, b=b, h=h: e.activation(out=qlatT[:, h, :], in_=PS[b][:], func=AF.Identity, scale=0.125),
                            reads=[psn(b)], writes=["qlatT"])
                    else:
                        add("dve", lambda e, b=b, h=h: e.tensor_scalar(out=qlatT[:, h, :], in0=PS[b][:], scalar1=0.125, scalar2=None, op0=ALU.mult),
                            reads=[psn(b)], writes=["qlatT"])
                if g == 0:
                    dump("qlatT", qlatT[:], [128, 8, 512], BF16, ["qlatT"])

                wres, wv = wget(cbase + 1)
                b = rot()
                for t in range(4):
                    mm8(b, PS[b][:, t * 128:(t + 1) * 128], lambda k, t=t: hT[:, k, t * 128:(t + 1) * 128], lambda k, wv=wv: wv[:, k, 0:128], [wres] + HT)
                for t in range(4):
                    add("act", lambda e, b=b, t=t: e.activation(out=sqj[:], in_=PS[b][:, t * 128:(t + 1) * 128], func=AF.Square, accum_out=ssq[:, t:t + 1]),
                        reads=[psn(b)], writes=["sqj", "ssq"])
                add("dve", lambda e: e.tensor_scalar(out=rstd4[:], in0=ssq[:], scalar1=1.0 / 128, scalar2=EPS, op0=ALU.mult, op1=ALU.add), reads=["ssq"], writes=["rstd4"])
                add("act", lambda e: e.activation(out=rstd4[:], in_=rstd4[:], func=AF.Sqrt), reads=["rstd4"], writes=["rstd4"])
                add("dve", lambda e: e.reciprocal(out=rstd4[:], in_=rstd4[:]), reads=["rstd4"], writes=["rstd4"])
                for t in range(4):
                    i = 4 * g + t
                    add("dve", lambda e, b=b, t=t, i=i: e.scalar_tensor_tensor(
                        out=ckv_tok[:, i, :], in0=PS[b][:, t * 128:(t + 1) * 128], scalar=rstd4[:, t:t + 1], in1=kvg_bc[:], op0=ALU.mult, op1=ALU.mult),
                        reads=[psn(b), "rstd4", "kvg_bc"], writes=["ckv_tok%d" % i])
                b2 = rot()
                psb2 = PS[b2][:].bitcast(BF16)

                def f_ckT(e, psb2=psb2, g=g):
                    last = None
                    for t in range(4):
                        last = e.transpose(out=psb2[:, t * 128:(t + 1) * 128], in_=ckv_tok[:, 4 * g + t, :], identity=identB)
                    return last
                add("pe", f_ckT, reads=["ckv_tok%d" % (4 * g + t) for t in range(4)] + ["cb"], writes=[psn(b2)])
                evac(ckvT[:, g * 512:(g + 1) * 512], psb2[:, 0:512], [psn(b2)], ["ckvT%d" % g])
                for pr in range(3):
                    b = rot()
                    mm8(b, PS[b][:], lambda k, wv=wv, pr=pr: wv[:, k, 128 + pr * 128:256 + pr * 128], lambda k: hT[:, k, :], [wres] + HT)
                    evac(qiT[:, pr, :], PS[b][:], [psn(b)], ["qiT"])
                wres, wv = wget(cbase + 2)
                b = rot()
                mm8(b, PS[b][:], lambda k, wv=wv: wv[:, k, 0:128], lambda k: hT[:, k, :], [wres] + HT)
                evac(qiT[:, 3, :], PS[b][:], [psn(b)], ["qiT"])
                b = rot()
                mm8(b, PS[b][0:64, :], lambda k, wv=wv: wv[:, k, 128:192], lambda k: hT[:, k, :], [wres] + HT)
                mm8(b, PS[b][64:128, :], lambda k, wv=wv: wv[:, k, 128:192], lambda k: hT[:, k, :], [wres] + HT)
                if g == 0:
                    add("pool", lambda e: e.memset(kiT2[:], 0.0), writes=["kiT2_z"])
                evac(kiT2[0:64, 0, g * 512:(g + 1) * 512], PS[b][0:64, :], [psn(b), "kiT2_z"], ["kiT2_%d" % g])
                evac(kiT2[64:128, 1, g * 512:(g + 1) * 512], PS[b][64:128, :], [psn(b), "kiT2_z"], ["kiT2_%d" % g])
                b = rot()
                for t in range(4):
                    mm8(b, PS[b][:, t * 8:(t + 1) * 8], lambda k, t=t: hT[:, k, t * 128:(t + 1) * 128], lambda k, wv=wv: wv[:, k, 192:200], [wres] + HT)
                evac(wi_tok[:].rearrange("p a b -> p (a b)"), PS[b][:, 0:32], [psn(b)], ["wi_tok"], eng="dve")
                if g == 0:
                    dump("ckv_tok", ckv_tok[:, 0:4, :], [128, 4, 128], BF16, ["ckv_tok%d" % t for t in range(4)])
                    dump("ckvT", ckvT[:, 0:512], [128, 512], BF16, ["ckvT0"])
                    dump("qiT", qiT[:], [128, 4, 512], BF16, ["qiT"])
                    dump("kiT2", kiT2[:, 0, 0:512], [128, 512], BF16, ["kiT2_0"])
                    dump("wi_tok", wi_tok[:], [128, 4, 8], F32, ["wi_tok"])

                def stage_B4sg(g=g, cbase=cbase):
                    wres, wv = wget(cbase + 3)
                    if g > 0:
                        add("pool", lambda e: e.tensor_copy(out=u_buf[:, 0, :], in_=u_buf[:, 4, :]), reads=["u4"], writes=["u0"])
                    for t in range(4):
                        b = rot()
                        mm8(b, PS[b][:], lambda k, t=t: hT[:, k, t * 128:(t + 1) * 128], lambda k, wv=wv: wv[:, k, :], [wres] + HT)
                        evac(u_buf[:, t + 1, :], PS[b][:], [psn(b)], ["u%d" % (t + 1)])
                    for t in range(4):
                        i = 4 * g + t
                        b = rot()

                        def f_pool(e, b=b, t=t, i=i):
                            last = None
                            for wg in range(4):
                                o = PS[b][:, wg * 128:(wg + 1) * 128]
                                if i == 0:
                                    last = e.matmul(o, lhsT=u_buf[:, t + 1, wg * 128:(wg + 1) * 128], rhs=band(wg, 2), start=True, stop=True)
                                else:
                                    e.matmul(o, lhsT=u_buf[:, t + 1, wg * 128:(wg + 1) * 128], rhs=band(wg, 0), start=True, stop=False)
                                    last = e.matmul(o, lhsT=u_buf[:, t, wg * 128:(wg + 1) * 128], rhs=band(wg, 1), start=False, stop=True)
                            return last
                        add("pe", f_pool, reads=["u%d" % t, "u%d" % (t + 1), "cb"], writes=[psn(b)])
                        evac(pooledT[:, :, t * 128:(t + 1) * 128], PS[b][:].rearrange("p (a b) -> p a b", a=4), [psn(b)], ["pooledT"])
                    for wg in range(4):
                        b = rot()
                        add("pe", lambda e, b=b, wg=wg: e.matmul(PS[b][:], lhsT=wpool[:, wg, :], rhs=pooledT[:, wg, :], start=True, stop=True),
                            reads=["wpool", "pooledT"], writes=[psn(b)])
                        add("act", lambda e, b=b, wg=wg: e.activation(out=ypoolT[:, wg, :], in_=PS[b][:], func=AF.Identity, scale=pscale[:, wg:wg + 1]),
                            reads=[psn(b), "pscale"], writes=["ypoolT"])
                    if g == 0:
                        dump("pooledT", pooledT[:], [128, 4, 512], BF16, ["pooledT"])
                        dump("ypoolT", ypoolT[:], [128, 4, 512], BF16, ["ypoolT"])

                    for m in range(4):
                        wres, wv = wget(cbase + 4 + m)
                        for q4 in range(4):
                            n = 4 * m + q4
                            b = rot()
                            mm8(b, PS[b][:], lambda k, wv=wv, q4=q4: wv[:, k, q4 * 128:(q4 + 1) * 128], lambda k: hT[:, k, :], [wres] + HT)
                            add("act", lambda e, b=b, n=n: e.activation(out=sgT[:, n, :], in_=PS[b][:], func=AF.Sigmoid), reads=[psn(b)], writes=["sgT"])
                    if g == 0:
                        dump("sgT", sgT[:], [128, 16, 512], BF16, ["sgT"])


                if stop_after == "B4":
                    S.drain_dmas("sp")
                    S.emit_phase()
                    return nc, dbg_out

                def build_diag(t, g=g):
                    i = 4 * g + t
                    dg = diag[i % 2]
                    dgn = "diag%d" % (i % 2)
                    for h in range(8):
                        add("dve", lambda e, h=h, t=t, dg=dg: e.tensor_scalar(out=dg[:, h, :], in0=identB, scalar1=wi_tok[:, t, h:h + 1], scalar2=None, op0=ALU.mult),
                            reads=["cb", "wi_tok"], writes=[dgn])

                def indexer(t, g=g):
                    i = 4 * g + t
                    sc = score[i % 2]
                    scn = "score%d" % (i % 2)
                    dg = diag[i % 2]
                    dgn = "diag%d" % (i % 2)
                    nch = i // 4 + 1
                    for c in range(nch):
                        N = 512 if c < nch - 1 else 128 * (i % 4 + 1)
                        bS = 1

                        def dots(h, c=c, N=N, t=t):
                            pr, hb = h // 2, (h % 2) * 64
                            bD = 6 + (h % 2)
                            R = Rb[h % 3]
                            add("pe", lambda e, bD=bD, pr=pr, hb=hb: e.matmul(
                                PS[bD][:, 0:N], lhsT=qiT[:, pr, t * 128:(t + 1) * 128], rhs=kiT2[:, hb // 64, c * 512:c * 512 + N], start=True, stop=True),
                                reads=["qiT", "kiT2_%d" % c], writes=[psn(bD)])
                            add("act", lambda e, bD=bD, R=R: e.activation(out=R[:, 0:N], in_=PS[bD][:, 0:N], func=AF.Relu),
                                reads=[psn(bD)], writes=["Rb%d" % (h % 3)])

                        def wsum(h, N=N, bS=bS):
                            R = Rb[h % 3]
                            add("pe", lambda e, h=h, R=R: e.matmul(PS[bS][:, 0:N], lhsT=dg[:, h, :], rhs=R[:, 0:N], start=(h == 0), stop=(h == 7)),
                                reads=[dgn, "Rb%d" % (h % 3)], writes=[psn(bS)], pe_chain=True)
                        dots(0)
                        dots(1)
                        for h in range(8):
                            wsum(h)
                            if h + 2 < 8:
                                dots(h + 2)
                        add("act", lambda e, bS=bS, c=c, sc=sc, N=N: e.activation(out=sc[:, c * 512:c * 512 + N], in_=PS[bS][:, 0:N], func=AF.Identity),
                            reads=[psn(bS)], writes=[scn])
                        add("dve", lambda e, N=N, c=c, i=i, sc=sc: e.tensor_reduce(out=amax[i % 2][:, c:c + 1], in_=sc[:, c * 512:c * 512 + N], axis=AX.X, op=ALU.max, apply_absolute_value=True),
                            reads=[scn], writes=["amax%d" % (i % 2)])
                        if c == nch - 1:
                            add("dve", lambda e, sc=sc, i=i: e.tensor_tensor(out=sc[:, i * 128:(i + 1) * 128], in0=sc[:, i * 128:(i + 1) * 128], in1=cmaskF, op=ALU.add),
                                reads=[scn, "cst"], writes=[scn])
                    return nch

                def threshold(t, nch, g=g):
                    i = 4 * g + t
                    sc = score[i % 2]
                    scn = "score%d" % (i % 2)
                    L = 128 * (i + 1)
                    A, mid, cnt, tmp, thr = (bis[:, k:k + 1] for k in range(5))
                    if i < 2:
                        add("dve", lambda e: e.memset(thr, -1.0e29), writes=["bis"])
                    else:
                        add("dve", lambda e: e.tensor_reduce(out=A, in_=amax[i % 2][:, 0:nch], axis=AX.X, op=ALU.max), reads=["amax%d" % (i % 2)], writes=["bis"])
                        add("dve", lambda e: e.tensor_scalar(out=wtab[:, 0:KBIS + 1], in0=cst[:, C_POW:C_POW + KBIS + 1], scalar1=A, scalar2=None, op0=ALU.mult),
                            reads=["bis", "cst"], writes=["wtab"])
                        add("dve", lambda e: e.memset(mid, 0.0), writes=["bis"])
                        for k in range(KBIS):
                            add("dve", lambda e: e.tensor_scalar(out=NM[:, 0:L], in0=sc[:, 0:L], scalar1=mid, scalar2=None, op0=ALU.is_ge, op1=ALU.add, accum_out=cnt),
                                reads=[scn, "bis"], writes=["NM", "bis"])
                            add("dve", lambda e: e.tensor_scalar(out=tmp, in0=cnt, scalar1=256.0, scalar2=-0.5, op0=ALU.is_ge, op1=ALU.add), reads=["bis"], writes=["bis"])
                            add("dve", lambda e, k=k: e.scalar_tensor_tensor(out=mid, in0=tmp, scalar=wtab[:, k:k + 1], in1=mid, op0=ALU.mult, op1=ALU.add),
                                reads=["bis", "wtab"], writes=["bis"])
                        add("dve", lambda e: e.tensor_tensor(out=thr, in0=mid, in1=wtab[:, KBIS:KBIS + 1], op=ALU.subtract), reads=["bis", "wtab"], writes=["bis"])
                    add("dve", lambda e: e.tensor_scalar(out=NM[:, 0:L], in0=sc[:, 0:L], scalar1=thr, scalar2=-30000.0, op0=ALU.is_lt, op1=ALU.mult),
                        reads=[scn, "bis"], writes=["NM"])
                    if g == 0 and t == 3:
                        dump("score3", sc[:, 0:512], [128, 512], F32, [scn])
                        dump("thr3", bis[:], [128, 8], F32, ["bis"])
                        dump("NM3", NM[:, 0:512], [128, 512], BF16, ["NM"])

                def attentionA(t, g=g):
                    i = 4 * g + t
                    for j0 in range(0, i + 1, 8):
                        nb = min(8, i + 1 - j0)
                        b = 6 + (j0 // 8) % 2
                        psb = PS[b][:].bitcast(BF16)

                        def f_nmt(e, psb=psb, j0=j0, nb=nb):
                            last = None
                            for jj in range(nb):
                                last = e.transpose(out=psb[:, jj * 128:(jj + 1) * 128], in_=NM[:, (j0 + jj) * 128:(j0 + jj + 1) * 128], identity=identB)
                            return last
                        add("pe", f_nmt, reads=["NM", "cb"], writes=[psn(b)])
                        evac(NMT[:, j0:j0 + nb, :].rearrange("p a b -> p (a b)"), psb[:, 0:nb * 128], [psn(b)], ["NMT"], eng="dve")

                def attentionB(t, g=g):
                    i = 4 * g + t
                    units = [(j, hh) for j in range(i + 1) for hh in range(2)]

                    LB = (0, 7)

                    def qk(n):
                        j, hh = units[n]
                        d = i - j
                        bl = LB[n % 2]
                        o3 = PS[bl][:].rearrange("p (a b) -> p a b", a=4)

                        def f(e):
                            e.matmul(o3, lhsT=ckvT[:, j * 128:(j + 1) * 128], rhs=qlatT[:, 4 * hh:4 * hh + 4, t * 128:(t + 1) * 128], start=True, stop=False)
                            e.matmul(o3, lhsT=alL[:, d * 128:(d + 1) * 128], rhs=alR[:].rearrange("p (a b) -> p a b", a=8)[:, 4 * hh:4 * hh + 4, :], start=False, stop=False)
                            return e.matmul(o3, lhsT=identB, rhs=NMT[:, j, :].unsqueeze(1).broadcast_to([128, 4, 128]), start=False, stop=True)
                        add("pe", f, reads=["ckvT%d" % (j // 4), "qlatT", "alL", "alR", "cb", "NMT"], writes=[psn(bl)])
                        add("act", lambda e, n=n, bl=bl: e.activation(out=PT[n % 2][:], in_=PS[bl][:], func=AF.Exp), reads=[psn(bl)], writes=["PT%d" % (n % 2)])

                    def pv(n):
                        j, hh = units[n]

                        def f(e):
                            e.matmul(PS[2 + hh][:], lhsT=ckv_tok[:, j, :], rhs=PT[n % 2][:], start=(j == 0), stop=(j == i))
                            return e.matmul(PS[4 + hh][:], lhsT=onesB, rhs=PT[n % 2][:], start=(j == 0), stop=(j == i))
                        add("pe", f, reads=["ckv_tok%d" % j, "PT%d" % (n % 2), "cb"], writes=[psn(2 + hh), psn(4 + hh)], pe_chain=True)
                    for n in range(len(units)):
                        qk(n)
                        if n > 0:
                            pv(n - 1)
                    pv(len(units) - 1)

                def attentionC(t, g=g):
                    i = 4 * g + t
                    for hh in range(2):
                        add("dve", lambda e, hh=hh: e.tensor_scalar(out=rden[:], in0=PS[4 + hh][:], scalar1=1.0e-30, scalar2=None, op0=ALU.max), reads=[psn(4 + hh)], writes=["rden"])
                        add("dve", lambda e: e.reciprocal(out=rden[:], in_=rden[:]), reads=["rden"], writes=["rden"])
                        add("dve", lambda e, hh=hh: e.tensor_tensor(out=olatn[:, 4 * hh:4 * hh + 4, :].rearrange("p a b -> p (a b)"), in0=PS[2 + hh][:], in1=rden[:], op=ALU.mult),
                            reads=[psn(2 + hh), "rden"], writes=["olatn"])

                def attentionD(t, g=g):
                    i = 4 * g + t
                    b = 7

                    def f_y(e):
                        last = None
                        for h in range(8):
                            pr, hb = h // 2, (h % 2) * 64
                            last = e.matmul(PS[b][hb:hb + 64, pr * 128:(pr + 1) * 128], lhsT=wuv[:, h * 64:(h + 1) * 64], rhs=olatn[:, h, :], start=True, stop=True)
                        return last
                    add("pe", f_y, reads=["wuv", "olatn"], writes=[psn(b)])
                    evac(yattnT[:, :, t * 128:(t + 1) * 128], PS[b][:].rearrange("p (a b) -> p a b", a=4), [psn(b)], ["yattnT"], eng="act")
                    if g == 0 and t == 3:
                        dump("olatn3", olatn[:], [128, 8, 128], BF16, ["olatn"])

                nchs = {}
                import os as _os
                if _os.environ.get("IDX1"):
                    indexer(int(_os.environ["IDX1"]))
                    S.drain_dmas("sp"); S.emit_phase(); return nc, dbg_out
                build_diag(0)
                build_diag(1)
                nchs[0] = indexer(0)
                threshold(0, nchs[0])
                stage_B4sg()
                if g == 0:
                    for j in range(4, 12):
                        ada_chunk(j)
                nchs[1] = indexer(1)
                for t in range(4):
                    attentionA(t)
                    if t + 2 < 4:
                        build_diag(t + 2)
                    if t + 1 < 4:
                        threshold(t + 1, nchs[t + 1])
                    attentionB(t)
                    attentionC(t)
                    if t + 2 < 4:
                        nchs[t + 2] = indexer(t + 2)
                    attentionD(t)
                if g == 0:
                    dump("yattnT", yattnT[:], [128, 4, 512], BF16, ["yattnT"])

                if stop_after == "B5":
                    S.drain_dmas("sp")
                    S.emit_phase()
                    return nc, dbg_out

                wresA, wvA = wget(cb8 + 0)
                wresP, wvP = wget(cb8 + 1)
                for n in range(8):
                    bA = rot()
                    mm8(bA, PS[bA][:], lambda k, n=n, wvA=wvA: wvA[:, k, n * 128:(n + 1) * 128], lambda k: yattnT[:, k, :], [wresA, "yattnT"], nk=4)
                    bP = rot()
                    mm8(bP, PS[bP][:], lambda k, n=n, wvP=wvP: wvP[:, k, n * 128:(n + 1) * 128], lambda k: ypoolT[:, k, :], [wresP, "ypoolT"], nk=4)
                    if n % 2 == 0:
                        t1_, t1n, t2_, t2n = t1[:, :], "t1", t2[:, :], "t2"
                    else:
                        t1_, t1n, t2_, t2n = rden[:, :], "rden", xp[:, 0:512], "xp0"
                    add("dve", lambda e, bA=bA, n=n, t1_=t1_: e.tensor_tensor(out=t1_, in0=PS[bA][:], in1=sgT[:, n, :], op=ALU.mult), reads=[psn(bA), "sgT"], writes=[t1n])
                    add("dve", lambda e, bP=bP, n=n, t2_=t2_: e.tensor_tensor(out=t2_, in0=PS[bP][:], in1=sgT[:, 8 + n, :], op=ALU.mult), reads=[psn(bP), "sgT"], writes=[t2n])
                    add("pool", lambda e, n=n, t1_=t1_, t2_=t2_: e.tensor_tensor(out=mixT[:, n, :], in0=t1_, in1=t2_, op=ALU.add), reads=[t1n, t2n], writes=["mixT"])
                if g == 0:
                    dump("mixT", mixT[:], [128, 8, 512], BF16, ["mixT"])
                wres0, wv0 = wget(cb8 + 2)
                wres1, wv1 = wget(cb8 + 3)
                def o_bufs(t, g=g):
                    i = 4 * g + t
                    if t % 2 == 0:
                        xp_, xpn = xp[:, :], ["xp0", "xp1"]
                        xr_, xrn = xr[:, :], "xr"
                        x1t_, x1tn = x1t[:, :], "x1t"
                        x1b_, x1bn = x1b[:, :], "x1b"
                        h2T_, h2Tn = h2T[:, :, :], "h2T"
                    else:
                        xp_, xpn = score[0][:, 0:1024], ["score0a", "score0a"]
                        x1t_, x1tn = score[0][:, 1024:2048], "score0b"
                        xr_, xrn = score[1][:, 0:1024], "score1a"
                        h2T_, h2Tn = score[1][:, 1024:2048].rearrange("p (a b) -> p a b", a=8), "score1b"
                        x1b_, x1bn = NM[:, 0:1024], "NM"
                    return i, xp_, xpn, xr_, xrn, x1t_, x1tn, x1b_, x1bn, h2T_, h2Tn

                def O_A(t):
                    i, xp_, xpn, xr_, xrn, x1t_, x1tn, x1b_, x1bn, h2T_, h2Tn = o_bufs(t)
                    add("sp", lambda e, i=i, xr_=xr_: e.dma_start(out=xr_, in_=x_d[i * 128:(i + 1) * 128, :]), writes=[xrn], dma_key="xrld%d" % (t % 2))
                    for hf, (wres_, wv_) in enumerate(((wres0, wv0), (wres1, wv1))):
                        b = rot()
                        mm8(b, PS[b][:], lambda k, t=t: mixT[:, k, t * 128:(t + 1) * 128], lambda k, wv_=wv_: wv_[:, k, :], [wres_, "mixT"])
                        add("dve", lambda e, b=b, hf=hf, xp_=xp_: e.tensor_tensor(out=xp_[:, hf * 512:(hf + 1) * 512], in0=PS[b][:], in1=gate_bc[:, 0, hf * 512:(hf + 1) * 512], op=ALU.mult),
                            reads=[psn(b)] + GATE1, writes=[xpn[hf]])
                    add("dve", lambda e, xp_=xp_, xr_=xr_: e.scalar_tensor_tensor(out=xp_, in0=xr_, scalar=ALPHA, in1=xp_, op0=ALU.mult, op1=ALU.add),
                        reads=[xrn] + xpn, writes=xpn)
                    for hf in range(2):
                        add("dve", lambda e, hf=hf, xp_=xp_: e.bn_stats(out=lnst[:, hf, :], in_=xp_[:, hf * 512:(hf + 1) * 512]), reads=xpn, writes=["lnst"])
                    add("dve", lambda e: e.bn_aggr(out=lnmv[:, 0:2], in_=lnst[:].rearrange("p a b -> p (a b)")), reads=["lnst"], writes=["lnmv"])
                    add("dve", lambda e: e.tensor_scalar(out=lnmv[:, 2:3], in0=lnmv[:, 1:2], scalar1=EPS, scalar2=None, op0=ALU.add), reads=["lnmv"], writes=["lnmv"])
                    add("act", lambda e: e.activation(out=lnmv[:, 2:3], in_=lnmv[:, 2:3], func=AF.Sqrt), reads=["lnmv"], writes=["lnmv"])
                    add("dve", lambda e: e.reciprocal(out=lnmv[:, 2:3], in_=lnmv[:, 2:3]), reads=["lnmv"], writes=["lnmv"])
                    add("dve", lambda e: e.scalar_tensor_tensor(out=lnmv[:, 3:4], in0=lnmv[:, 0:1], scalar=-1.0, in1=lnmv[:, 2:3], op0=ALU.mult, op1=ALU.mult), reads=["lnmv"], writes=["lnmv"])
                    add("act", lambda e, xp_=xp_, x1t_=x1t_: e.activation(out=x1t_, in_=xp_, func=AF.Identity, scale=lnmv[:, 2:3], bias=lnmv[:, 3:4]),
                        reads=xpn + ["lnmv"], writes=[x1tn])
                    add("pool", lambda e, x1t_=x1t_: e.tensor_tensor(out=x1t_, in0=x1t_, in1=ln1g_bc[:], op=ALU.mult), reads=[x1tn, "ln1g_bc"], writes=[x1tn])
                    add("pool", lambda e, x1t_=x1t_: e.tensor_tensor(out=x1t_, in0=x1t_, in1=ln1b_bc[:], op=ALU.add), reads=[x1tn, "ln1b_bc"], writes=[x1tn])

                def O_R(t):
                    i, xp_, xpn, xr_, xrn, x1t_, x1tn, x1b_, x1bn, h2T_, h2Tn = o_bufs(t)
                    add("sp", lambda e, i=i, x1t_=x1t_: e.dma_start(out=x1_d[i * 128:(i + 1) * 128, :], in_=x1t_), reads=[x1tn], writes=["x1d%d" % i], dma_key="x1d")
                    add("act", lambda e, x1t_=x1t_, x1b_=x1b_: e.activation(out=x1b_, in_=x1t_, func=AF.Identity), reads=[x1tn], writes=[x1bn])
                    for hf in range(2):
                        b = rot()

                        def f_x1t(e, b=b, hf=hf, x1t_=x1t_):
                            last = None
                            for c4 in range(4):
                                c = 4 * hf + c4
                                last = e.transpose(out=PS[b][:, c4 * 128:(c4 + 1) * 128], in_=x1t_[:, c * 128:(c + 1) * 128], identity=identF)
                            return last
                        add("pe", f_x1t, reads=[x1tn, "cst"], writes=[psn(b)])
                        for c4 in range(4):
                            c = 4 * hf + c4
                            add("act", lambda e, b=b, c=c, c4=c4, h2T_=h2T_: e.activation(out=h2T_[:, c, :], in_=PS[b][:, c4 * 128:(c4 + 1) * 128], func=AF.Identity,
                                                                                scale=modcol[:, 2, c:c + 1], bias=modcol[:, 3, c:c + 1]),
                                reads=[psn(b)] + MODC, writes=[h2Tn])
                    b = rot()
                    mm8(b, PS[b][:, 0:NE], lambda k, h2T_=h2T_: h2T_[:, k, :], lambda k: wrt[:, k, :], [h2Tn, "wrt"])
                    add("dve", lambda e, b=b: e.tensor_tensor(out=lg[:], in0=PS[b][:, 0:NE], in1=br_bc[:], op=ALU.add), reads=[psn(b), "br_bc"], writes=["lg"])
                    add("dve", lambda e: e.max(out=top8[:], in_=lg[:]), reads=["lg"], writes=["top8"])
                    add("dve", lambda e, i=i: e.tensor_scalar(out=Mall[:, i, :], in0=lg[:], scalar1=top8[:, 3:4], scalar2=None, op0=ALU.is_ge), reads=["lg", "top8"], writes=["Mall%d" % i])
                    add("dve", lambda e: e.tensor_scalar(out=rsm[:, 0:1], in0=top8[:, 0:1], scalar1=-1.0, scalar2=None, op0=ALU.mult), reads=["top8"], writes=["rsm"])
                    add("act", lambda e: e.activation(out=e4[:], in_=top8[:, 0:4], func=AF.Exp, bias=rsm[:, 0:1], scale=1.0, accum_out=rsm[:, 1:2]), reads=["top8", "rsm"], writes=["e4", "rsm"])
                    add("dve", lambda e: e.reciprocal(out=rsm[:, 2:3], in_=rsm[:, 1:2]), reads=["rsm"], writes=["rsm"])
                    add("dve", lambda e, i=i: e.tensor_scalar(out=gates[:, i, :], in0=e4[:], scalar1=rsm[:, 2:3], scalar2=None, op0=ALU.mult), reads=["e4", "rsm"], writes=["gates%d" % i])
                    b = rot()

                    def f_rank(e, b=b, i=i):
                        last = None
                        for j in range(i):
                            last = e.matmul(PS[b][:, 0:NE], lhsT=onesB, rhs=Mall[:, j, :], start=(j == 0), stop=False)
                        return e.matmul(PS[b][:, 0:NE], lhsT=triB, rhs=Mall[:, i, :], start=(i == 0), stop=True)
                    add("pe", f_rank, reads=["Mall%d" % j for j in range(i + 1)] + ["cb"], writes=[psn(b)])
                    add("dve", lambda e, b=b: e.tensor_scalar(out=destf[:], in0=PS[b][:, 0:NE], scalar1=float(CAP - 1), scalar2=None, op0=ALU.min), reads=[psn(b)], writes=["destf"])
                    add("dve", lambda e: e.tensor_tensor(out=destf[:], in0=destf[:], in1=cst[:, C_EB:C_EB + NE], op=ALU.add), reads=["destf", "cst"], writes=["destf"])
                    for k in range(4):
                        add("dve", lambda e, k=k: e.scalar_tensor_tensor(out=junk32[:], in0=lg[:], scalar=top8[:, k:k + 1], in1=destf[:], op0=ALU.is_equal, op1=ALU.mult, accum_out=destk[:, k:k + 1]),
                            reads=["lg", "top8", "destf"], writes=["junk32", "destk"])
                    add("dve", lambda e, i=i: e.tensor_copy(out=desti[:, i, :], in_=destk[:]), reads=["destk"], writes=["desti%d" % i])
                    for k in range(4):
                        add("pool", lambda e, i=i, k=k, x1b_=x1b_: e.indirect_dma_start(out=xpad_d, out_offset=bass.IndirectOffsetOnAxis(ap=desti[:, i, k:k + 1], axis=0), in_=x1b_, in_offset=None),
                            reads=[x1bn, "desti%d" % i] + xpadZ, writes=["xpadS%d_%d" % (i, k)], dma_key="xpadS")
                    if i == 0:
                        dump("x1t0", x1t[:], [128, D], F32, ["x1t"])
                        dump("lg0", lg[:], [128, NE], F32, ["lg"])
                        dump("destk0", destk[:], [128, 4], F32, ["destk"])


                O_A(0)
                for t in range(4):
                    if t + 1 < 4:
                        O_A(t + 1)
                    O_R(t)

            b = rot()

            def f_cnt(e, b=b):
                last = None
                for j in range(NT):
                    last = e.matmul(PS[b][:, 0:NE], lhsT=onesB, rhs=Mall[:, j, :], start=(j == 0), stop=(j == NT - 1))
                return last
            add("pe", f_cnt, reads=["Mall%d" % j for j in range(NT)] + ["cb"], writes=[psn(b)])
            add("dve", lambda e, b=b: e.tensor_copy(out=cnt_f[:], in_=PS[b][0:1, 0:NE]), reads=[psn(b)], writes=["cnt_f"])
            add("dve", lambda e: e.tensor_copy(out=cnt_i[:], in_=cnt_f[:]), reads=["cnt_f"], writes=["cnt_i"])
            dump("cnt_f", cnt_f[:], [1, NE], F32, ["cnt_f"])
            dump("gates", gates[:], [128, NT, 4], F32, ["gates%d" % i for i in range(NT)])
            dump("desti", desti[:], [128, NT, 4], U32, ["desti%d" % i for i in range(NT)])
            S.drain_dmas("sp")
            S.emit_phase()
        XPADS = ["xpadS%d_%d" % (i, k) for i in range(NT) for k in range(4)]
        if stop_after == "B":
            return nc, dbg_out

        with ExitStack() as ph:
            NSL = 8
            CPE = 3
            ring = [T(ph, "er%d" % i, [128, 8, 1024], BF16) for i in range(NSL)]
            wgu_v = wgu_d.rearrange("e (c p) n -> e p c n", p=128)
            wd_v = wd_d.rearrange("e (c p) n -> e p c n", p=128)
            loads = []
            for ex in range(NE):
                for m in range(2):
                    loads.append((wgu_v[ex][:, 4 * m:4 * m + 4, :], 4, 2048))
                loads.append((wd_v[ex][:, :, :], 8, 1024))
            issued = [0]
            LOOKC = 8

            def eprefetch(upto):
                while issued[0] <= min(upto, len(loads) - 1):
                    k = issued[0]
                    add("pool", lambda e, k=k: e.dma_start(out=ring[k % NSL][:].rearrange("p a b -> p (a b)").rearrange("p (c n) -> p c n", c=loads[k][1]), in_=loads[k][0]),
                        writes=["er%d" % (k % NSL)], dma_key="er%d" % (k % NSL))
                    issued[0] += 1

            def eget(n):
                assert n < issued[0]
                return "er%d" % (n % NSL), ring[n % NSL][:].rearrange("p a b -> p (a b)").rearrange("p (c n) -> p c n", c=loads[n][1])

            NTG = SG // 128
            xe_tok = [T(ph, "xe_tok%d" % i, [128, NTG, D], BF16) for i in range(3)]
            XeT = [T(ph, "XeT%d" % i, [128, 8, SG], BF16) for i in range(3)]
            actT = [T(ph, "actT%d" % i, [128, 8, SG], BF16) for i in range(3)]
            ye_tok = [T(ph, "ye_tok%d" % i, [128, NTG, D], BF16) for i in range(2)]
            bgu = T(ph, "bgu", [128, NE * 16], F32)
            bd16 = [T(ph, "bd16_%d" % i, [1, D], BF16) for i in range(2)]
            g1 = [T(ph, "g1_%d" % i, [128, SG], F32) for i in range(2)]
            l2 = [T(ph, "l2_%d" % i, [128, SG], F32) for i in range(2)]
            pp = [T(ph, "pp_%d" % i, [128, SG], F32) for i in range(2)]
            add("sp", lambda e: e.dma_start(out=bgu[:], in_=bgu_d), writes=["bgu"], dma_key="bgu")
            bgu3 = bgu[:].rearrange("p (e f) -> p e f", e=NE)
            add("dve", lambda e: e.tensor_scalar(out=bgu3[:, :, 8:16], in0=bgu3[:, :, 8:16], scalar1=1.0, scalar2=None, op0=ALU.add), reads=["bgu"], writes=["bgu"])
            bdsc = [T(ph, "bdsc_%d" % i, [128, D], BF16) for i in range(2)]
            for i2 in range(2):
                add("pool", lambda e, i2=i2: e.memset(bdsc[i2][:], 0.0), writes=["bdsc_%d" % i2])
            INV = 1.0 / 1.702
            g2s = T(ph, "g2s", [128, D], F32)
            add("dve", lambda e: e.tensor_scalar(out=g2s[:], in0=gate_bc[:, 1, :], scalar1=INV, scalar2=None, op0=ALU.mult), reads=GATE2, writes=["g2s"])
            groups = [(ex, sgi) for ex in range(NE) for sgi in range(NG)]

            class _Cond:
                def __init__(self, ex, sgi):
                    self.on = sgi > 0
                    self.ex, self.sgi = ex, sgi

                def __enter__(self):
                    if self.on:
                        S.begin_cond(cnt_i[0:1, self.ex:self.ex + 1], self.sgi * SG)

                def __exit__(self, *a):
                    if self.on:
                        S.end_cond()

            def bufi(gi):
                ex, sgi = groups[gi]
                return ex % 2 if sgi == 0 else 2

            def stage_load(gi):
                ex, sgi = groups[gi]
                par = bufi(gi)
                r0 = ex * CAP + sgi * SG
                xt = xe_tok[par]
                add("sp", lambda e, r0=r0, xt=xt: e.dma_start(out=xt[:], in_=xpad_d[r0:r0 + SG, :].rearrange("(a p) n -> p a n", p=128)),
                    reads=XPADS + xpadZ, writes=["xe_tok%d" % par], dma_key="xe_tok%d" % par)

            def stage_T(gi):
                ex, sgi = groups[gi]
                par = bufi(gi)
                xt, xtn, XT, XTn = xe_tok[par], "xe_tok%d" % par, XeT[par], "XeT%d" % par
                if True:
                    for k2 in range(4):
                        b = rot()
                        psb = PS[b][:].bitcast(BF16)

                        def f_xet(e, psb=psb, k2=k2, xt=xt):
                            last = None
                            for kk in range(2):
                                k = 2 * k2 + kk
                                for st in range(NTG):
                                    last = e.transpose(out=psb[:, kk * SG + st * 128:kk * SG + (st + 1) * 128], in_=xt[:, st, k * 128:(k + 1) * 128], identity=identB)
                            return last
                        add("pe", f_xet, reads=[xtn, "cb"], writes=[psn(b)])
                        for kk in range(2):
                            k = 2 * k2 + kk
                            add("act", lambda e, psb=psb, kk=kk, k=k, XT=XT: e.activation(out=XT[:, k, :], in_=psb[:, kk * SG:(kk + 1) * SG], func=AF.Identity,
                                                                                       scale=modcol[:, 2, k:k + 1], bias=modcol[:, 3, k:k + 1]),
                                reads=[psn(b)] + MODC, writes=[XTn])

            def stage_G(gi):
                ex, sgi = groups[gi]
                par = bufi(gi)
                XT, XTn, AT, ATn = XeT[par], "XeT%d" % par, actT[par], "actT%d" % par
                if sgi == 0:
                    if ex == 0:
                        add("pool", lambda e: e.dma_start(out=bd16[0][:], in_=bd_d[0:1, :]), writes=["bd16_0"], dma_key="bd16_0")
                    if ex + 1 < NE:
                        add("pool", lambda e, ex=ex: e.dma_start(out=bd16[(ex + 1) % 2][:], in_=bd_d[ex + 1:ex + 2, :]), writes=["bd16_%d" % ((ex + 1) % 2)], dma_key="bd16_%d" % ((ex + 1) % 2))
                    eprefetch(CPE * (ex - 1) + (CPE - 1) + NSL)
                    add("dve", lambda e, ex=ex: e.tensor_scalar(out=bdsc[ex % 2][0:1, :], in0=bd16[ex % 2][:], scalar1=1.702, scalar2=None, op0=ALU.mult),
                        reads=["bd16_%d" % (ex % 2)], writes=["bdsc_%d" % (ex % 2)])
                if True:
                    for fc in range(8):
                        res0, w0 = eget(CPE * ex + 0)
                        res1, w1 = eget(CPE * ex + 1)
                        wk = lambda k, col, w0=w0, w1=w1: (w0 if k < 4 else w1)[:, k % 4, col:col + 128]
                        bG = rot()
                        mm8(bG, PS[bG][:, 0:SG], lambda k, wk=wk, fc=fc: wk(k, fc * 128), lambda k, XT=XT: XT[:, k, :], [res0, res1, XTn])
                        bL = rot()
                        mm8(bL, PS[bL][:, 0:SG], lambda k, wk=wk, fc=fc: wk(k, 1024 + fc * 128), lambda k, XT=XT: XT[:, k, :], [res0, res1, XTn])
                        p2 = fc % 2
                        add("dve", lambda e, bG=bG, ex=ex, fc=fc, p2=p2: e.tensor_scalar(out=g1[p2][:], in0=PS[bG][:, 0:SG], scalar1=bgu[:, ex * 16 + fc:ex * 16 + fc + 1], scalar2=7.0, op0=ALU.add, op1=ALU.min),
                            reads=[psn(bG), "bgu"], writes=["g1_%d" % p2])
                        add("act", lambda e, p2=p2: e.activation(out=pp[p2][:], in_=g1[p2][:], func=AF.Silu, scale=1.702), reads=["g1_%d" % p2], writes=["pp_%d" % p2])
                        add("dve", lambda e, bL=bL, ex=ex, fc=fc, p2=p2: e.tensor_scalar(out=l2[p2][:], in0=PS[bL][:, 0:SG], scalar1=bgu[:, ex * 16 + 8 + fc:ex * 16 + 8 + fc + 1], scalar2=-6.0, op0=ALU.add, op1=ALU.max),
                            reads=[psn(bL), "bgu"], writes=["l2_%d" % p2])
                        add("dve", lambda e, p2=p2, fc=fc, AT=AT: e.scalar_tensor_tensor(out=AT[:, fc, :], in0=l2[p2][:], scalar=8.0, in1=pp[p2][:], op0=ALU.min, op1=ALU.mult),
                            reads=["l2_%d" % p2, "pp_%d" % p2], writes=[ATn])

            def stage_D(gi):
                ex, sgi = groups[gi]
                par = bufi(gi)
                AT, ATn = actT[par], "actT%d" % par
                ypar = 0 if sgi == 0 else 1
                yt, ytn = ye_tok[ypar], "ye_tok%d" % ypar
                r0 = ex * CAP + sgi * SG
                if True:
                    for hf in range(2):
                        resD, wD = eget(CPE * ex + 2)
                        for st in range(NTG):
                            b = rot()

                            def f_dn(e, b=b, wD=wD, st=st, ex=ex, hf=hf, AT=AT):
                                for k in range(8):
                                    e.matmul(PS[b][:], lhsT=AT[:, k, st * 128:(st + 1) * 128], rhs=wD[:, k, hf * 512:(hf + 1) * 512], start=(k == 0), stop=False)
                                return e.matmul(PS[b][:], lhsT=ones0B, rhs=bdsc[ex % 2][:, hf * 512:(hf + 1) * 512], start=False, stop=True)
                            add("pe", f_dn, reads=[resD, ATn, "cb", "bdsc_%d" % (ex % 2)], writes=[psn(b)])
                            add("dve", lambda e, b=b, st=st, hf=hf, yt=yt: e.tensor_tensor(out=yt[:, st, hf * 512:(hf + 1) * 512], in0=PS[b][:], in1=g2s[:, hf * 512:(hf + 1) * 512], op=ALU.mult),
                                reads=[psn(b), "g2s"], writes=[ytn])
                    add("sp", lambda e, r0=r0, yt=yt: e.dma_start(out=ypad_d[r0:r0 + SG, :].rearrange("(a p) n -> p a n", p=128), in_=yt[:]),
                        reads=[ytn], writes=["ypadS%d" % gi], dma_key="ypadS")

            g0 = lambda ex: ex * NG
            stage_load(g0(0))
            stage_load(g0(1))
            stage_T(g0(0))
            for ex in range(NE):
                stage_G(g0(ex))
                if ex + 1 < NE:
                    stage_T(g0(ex + 1))
                if ex + 2 < NE:
                    stage_load(g0(ex + 2))
                stage_load(g0(ex) + 1)
                stage_T(g0(ex) + 1)
                stage_D(g0(ex))
                for sgi in range(1, NG):
                    gi = g0(ex) + sgi
                    S.begin_cond(cnt_i[0:1, ex:ex + 1], sgi * SG)
                    if sgi > 1:
                        stage_load(gi)
                        stage_T(gi)
                    stage_G(gi)
                    stage_D(gi)
                for sgi in range(1, NG):
                    S.end_cond()
            S.drain_dmas("sp")
            S.emit_phase()
        YPADS = ["ypadS%d" % gi for gi in range(NE * NG)]

        with ExitStack() as ph:
            Yk = [[T(ph, "Yk%d_%d" % (s2, k), [128, D], BF16) for k in range(4)] for s2 in range(3)]
            x1r = [T(ph, "x1r%d" % i, [128, D], F32) for i in range(3)]
            acc = [T(ph, "acc%d" % i, [128, D], F32) for i in range(2)]
            ot = [T(ph, "ot%d" % i, [128, D], F32) for i in range(2)]
            ln2g_bc = T(ph, "ln2g_bc", [128, D], F32)
            ln2b_bc = T(ph, "ln2b_bc", [128, D], F32)
            add("sp", lambda e: e.dma_start(out=ln2g_bc[:], in_=ln2g_d.broadcast_to([128, D])), writes=["ln2g_bc"], dma_key="ln2g_bc")
            add("sp", lambda e: e.dma_start(out=ln2b_bc[:], in_=ln2b_d.broadcast_to([128, D])), writes=["ln2b_bc"], dma_key="ln2b_bc")
            dgk = [T(ph, "dgk%d" % i, [128, 4, 128], BF16) for i in range(2)]
            def d_fetch(i):
                s2 = i % 3
                for k in range(4):
                    add("pool", lambda e, i=i, k=k, s2=s2: e.indirect_dma_start(out=Yk[s2][k][:], out_offset=None, in_=ypad_d, in_offset=bass.IndirectOffsetOnAxis(ap=desti[:, i, k:k + 1], axis=0)),
                        reads=YPADS + ["desti%d" % i], writes=["Yk%d_%d" % (s2, k)], dma_key="Yk%d_%d" % (s2, k))
                add("sp", lambda e, i=i, s2=s2: e.dma_start(out=x1r[s2][:], in_=x1_d[i * 128:(i + 1) * 128, :]), reads=["x1d%d" % i], writes=["x1r%d" % s2], dma_key="x1r%d" % s2)

            lnst2 = [T(ph, "lnst2_%d" % i, [128, 2, 6], F32) for i in range(2)]
            lnmv2 = [T(ph, "lnmv2_%d" % i, [128, 4], F32) for i in range(2)]

            cmb_banks = {}

            def D_A1(i):
                s2, p2 = i % 3, i % 2
                for k in range(4):
                    add("dve", lambda e, k=k: e.tensor_scalar(out=dgk[p2][:, k, :], in0=identB, scalar1=gates[:, i, k:k + 1], scalar2=None, op0=ALU.mult),
                        reads=["cb", "gates%d" % i], writes=["dgk%d" % p2])
                cmb_banks[i] = []
                for hf in range(2):
                    b = rot()
                    cmb_banks[i].append(b)

                    def f_cmb(e, b=b, hf=hf):
                        last = None
                        for k in range(4):
                            last = e.matmul(PS[b][:], lhsT=dgk[p2][:, k, :], rhs=Yk[s2][k][:, hf * 512:(hf + 1) * 512], start=(k == 0), stop=(k == 3))
                        return last
                    add("pe", f_cmb, reads=["dgk%d" % p2] + ["Yk%d_%d" % (s2, k) for k in range(4)], writes=[psn(b)])

            def D_A2(i):
                s2, p2 = i % 3, i % 2
                a_, an = acc[p2], "acc%d" % p2
                st_, stn, mv_, mvn = lnst2[p2], "lnst2_%d" % p2, lnmv2[p2], "lnmv2_%d" % p2
                for hf in range(2):
                    b = cmb_banks[i][hf]
                    add("dve", lambda e, b=b, hf=hf: e.scalar_tensor_tensor(out=a_[:, hf * 512:(hf + 1) * 512], in0=x1r[s2][:, hf * 512:(hf + 1) * 512], scalar=ALPHA,
                                                                         in1=PS[b][:], op0=ALU.mult, op1=ALU.add),
                        reads=["x1r%d" % s2, psn(b)], writes=[an])
                for hf in range(2):
                    add("dve", lambda e, hf=hf: e.bn_stats(out=st_[:, hf, :], in_=a_[:, hf * 512:(hf + 1) * 512]), reads=[an], writes=[stn])
                add("dve", lambda e: e.bn_aggr(out=mv_[:, 0:2], in_=st_[:].rearrange("p a b -> p (a b)")), reads=[stn], writes=[mvn])

            def D_B1(i):
                p2 = i % 2
                a_, an = acc[p2], "acc%d" % p2
                mv_, mvn = lnmv2[p2], "lnmv2_%d" % p2
                o_, on = ot[p2], "ot%d" % p2
                add("dve", lambda e: e.tensor_scalar(out=mv_[:, 2:3], in0=mv_[:, 1:2], scalar1=EPS, scalar2=None, op0=ALU.add), reads=[mvn], writes=[mvn])
                add("act", lambda e: e.activation(out=mv_[:, 2:3], in_=mv_[:, 2:3], func=AF.Sqrt), reads=[mvn], writes=[mvn])
                add("dve", lambda e: e.reciprocal(out=mv_[:, 2:3], in_=mv_[:, 2:3]), reads=[mvn], writes=[mvn])
                add("dve", lambda e: e.scalar_tensor_tensor(out=mv_[:, 3:4], in0=mv_[:, 0:1], scalar=-1.0, in1=mv_[:, 2:3], op0=ALU.mult, op1=ALU.mult), reads=[mvn], writes=[mvn])
                add("act", lambda e: e.activation(out=o_[:], in_=a_[:], func=AF.Identity, scale=mv_[:, 2:3], bias=mv_[:, 3:4]), reads=[an, mvn], writes=[on])

            def D_B2(i):
                p2 = i % 2
                o_, on = ot[p2], "ot%d" % p2
                add("dve", lambda e: e.tensor_tensor(out=o_[:], in0=o_[:], in1=ln2g_bc[:], op=ALU.mult), reads=[on, "ln2g_bc"], writes=[on])
                add("dve", lambda e: e.tensor_tensor(out=o_[:], in0=o_[:], in1=ln2b_bc[:], op=ALU.add), reads=[on, "ln2b_bc"], writes=[on])
                add("sp", lambda e: e.dma_start(out=out_d[i * 128:(i + 1) * 128, :], in_=o_[:]), reads=[on], writes=["outd%d" % i], dma_key="outd")

            d_fetch(0)
            d_fetch(1)
            D_A1(0)
            D_A2(0)
            for i in range(NT):
                if i + 2 < NT:
                    d_fetch(i + 2)
                if i + 1 < NT:
                    D_A1(i + 1)
                D_B1(i)
                if i + 1 < NT:
                    D_A2(i + 1)
                D_B2(i)
            S.drain_dmas("sp")
            S.emit_phase()
    return nc, dbg_out


def make_in_maps(inputs, cores=range(8)):
    f = lambda a: np.ascontiguousarray(np.asarray(a, dtype=np.float32))
    consts, constsb, alL, alR = make_consts()
    shared = {
        "w_ada": f(inputs["w_ada"][0]),
        "b_ada": f(inputs["b_ada"][0]).reshape(1, -1),
        "w_in": f(inputs["w_in"][0]),
        "kv_norm_g": f(inputs["kv_norm_g"][0]).reshape(1, -1),
        "w_uk": f(inputs["w_uk"][0]).reshape(128, 512),
        "w_uv": f(inputs["w_uv"][0]).reshape(128, 512),
        "w_pool": f(inputs["w_pool_group"][0]),
        "pool_scale_col": f(np.asarray(inputs["pool_scale"][0]).reshape(4, 128).T),
        "w_ba": f(inputs["w_branch_attn"][0]),
        "w_bp": f(inputs["w_branch_pool"][0]),
        "w_out": f(inputs["w_out"][0]),
        "ln1_g": f(inputs["ln1_g"][0]).reshape(1, -1),
        "ln1_b": f(inputs["ln1_b"][0]).reshape(1, -1),
        "w_router": f(inputs["w_router"][0]),
        "b_router": f(inputs["b_router"][0]).reshape(1, -1),
        "w_gate_up": f(inputs["w_gate_up"][0]),
        "b_gu_col": f(np.asarray(inputs["b_gate_up"][0]).reshape(NE, 16, 128).transpose(2, 0, 1).reshape(128, NE * 16)),
        "w_down": f(inputs["w_down"][0]),
        "b_down": f(inputs["b_down"][0]),
        "ln2_g": f(inputs["ln2_g"][0]).reshape(1, -1),
        "ln2_b": f(inputs["ln2_b"][0]).reshape(1, -1),
        "consts": consts, "constsb": constsb, "alibiL": alL, "alibiR": alR,
    }
    maps = []
    for b in cores:
        m = dict(shared)
        m["x"] = f(inputs["x"][b])
        m["c_col"] = f(np.asarray(inputs["c"][b]).reshape(8, 128).T)
        maps.append(m)
    return maps


def kernel(**inputs):
    nc, _ = build_nc()
    maps = make_in_maps(inputs)
    res = run_bass_kernel_spmd(nc, maps, core_ids=list(range(8)))
    out = np.stack([np.asarray(r["out"], dtype=np.float32) for r in res.results], axis=0)
    return out
```
